# Optimizing a Trainium2 kernel written in Bass

```python
import math
import jax, jax.numpy as jnp
from jax import lax
import numpy as np

D_MODEL = 2048
BATCH = 8
SEQ = 2048
DEPTH = 1

GRID_W = 64
CTX_LEN = 256
EPS = 1e-6
NEG_INF = -1e30
DN_HEADS = 16
DN_DK = 128
DN_DV = 128
DN_W = DN_HEADS * DN_DV
DN_QKV_W = 2 * DN_HEADS * DN_DK + DN_W
CONV_K = 5
DN_CHUNK = 64
NA_HEADS = 16
NA_DH = 128
NA_W = NA_HEADS * NA_DH
NA_KH_MAX = 8
NA_KW = 16
ROPE_THETA = 10000.0
N_EXPERTS = 16
EC_CAPACITY_FACTOR = 2
EXPERT_FF = 1024
IN_SIZES = (DN_HEADS * DN_DK, DN_HEADS * DN_DK, DN_W, DN_W, 2 * DN_HEADS, 2 * DN_HEADS, NA_W, NA_W, NA_W, D_MODEL, D_MODEL)
IN_W = sum(IN_SIZES)

kernel_name = "hybrid_deltanet_natten_ecmoe_dit_block"


def rmsnorm(x, w):
    xf = x.astype(jnp.float32)
    y = xf * lax.rsqrt(jnp.mean(xf * xf, axis=-1, keepdims=True) + EPS)
    return (y * w.astype(jnp.float32)).astype(x.dtype)


def l2norm(x):
    xf = x.astype(jnp.float32)
    return xf * lax.rsqrt(jnp.sum(xf * xf, axis=-1, keepdims=True) + EPS)


def short_conv(u, w):
    y = lax.conv_general_dilated(u, w[:, None, :].astype(u.dtype), window_strides=(1,),
                                 padding=[(CONV_K // 2, CONV_K // 2)],
                                 dimension_numbers=('NWC', 'WIO', 'NWC'),
                                 feature_group_count=u.shape[-1])
    return jax.nn.silu(y)


def gated_delta_chunked(q, k, v, beta, g, s0):
    q, k, v, beta, g, s0 = [t.astype(jnp.float32) for t in (q, k, v, beta, g, s0)]
    B, H, L, dk = q.shape
    dv = v.shape[-1]
    n = L // DN_CHUNK
    q, k, v = [t.reshape(B, H, n, DN_CHUNK, t.shape[-1]) for t in (q, k, v)]
    beta, g = [t.reshape(B, H, n, DN_CHUNK) for t in (beta, g)]
    G = jnp.cumsum(g, axis=-1)
    idx = jnp.arange(DN_CHUNK)
    tril = idx[:, None] >= idx[None, :]
    strict = idx[:, None] > idx[None, :]
    decay = jnp.exp(jnp.where(tril, G[..., :, None] - G[..., None, :], -jnp.inf))
    kb = k * beta[..., None]
    lower = jnp.where(strict, jnp.einsum('bhncd,bhnsd->bhncs', kb, k) * decay, 0.0)
    rhs = jnp.concatenate([v * beta[..., None], kb * jnp.exp(G)[..., None]], axis=-1)
    sol = lax.linalg.triangular_solve(jnp.eye(DN_CHUNK, dtype=jnp.float32) + lower, rhs,
                                      left_side=True, lower=True, unit_diagonal=True)
    u, w = sol[..., :dv], sol[..., dv:]
    a_qk = jnp.where(tril, jnp.einsum('bhncd,bhnsd->bhncs', q, k) * decay, 0.0)
    g_last = G[..., -1]
    k_dec = k * jnp.exp(g_last[..., None] - G)[..., None]
    q_dec = q * jnp.exp(G)[..., None]

    def step(S, xs):
        q_i, k_i, u_i, w_i, a_i, gl_i = xs
        v_new = u_i - jnp.einsum('bhck,bhkv->bhcv', w_i, S)
        o_i = jnp.einsum('bhck,bhkv->bhcv', q_i, S) + jnp.einsum('bhcs,bhsv->bhcv', a_i, v_new)
        S = S * jnp.exp(gl_i)[..., None, None] + jnp.einsum('bhck,bhcv->bhkv', k_i, v_new)
        return S, o_i

    xs = tuple(jnp.moveaxis(t, 2, 0) for t in (q_dec, k_dec, u, w, a_qk, g_last))
    s_fin, o = lax.scan(step, s0, xs)
    return jnp.moveaxis(o, 0, 2).reshape(B, H, L, dv), s_fin


def delta_inputs(pq, pk, pv, pbeta, pa, conv_w, a_log, dt_bias):
    B, L, _ = pq.shape
    qkv = short_conv(jnp.concatenate([pq, pk, pv], axis=-1), conv_w)
    q, k, v = jnp.split(qkv, [DN_HEADS * DN_DK, 2 * DN_HEADS * DN_DK], axis=-1)
    q = l2norm(q.reshape(B, L, DN_HEADS, DN_DK)).transpose(0, 2, 1, 3) * DN_DK ** -0.5
    k = l2norm(k.reshape(B, L, DN_HEADS, DN_DK)).transpose(0, 2, 1, 3)
    v = v.reshape(B, L, DN_HEADS, DN_DV).transpose(0, 2, 1, 3)
    beta = jax.nn.sigmoid(pbeta.astype(jnp.float32)).transpose(0, 2, 1)
    g = -jnp.exp(a_log.reshape(-1).astype(jnp.float32))[:, None] * jax.nn.softplus(
        pa.astype(jnp.float32).transpose(0, 2, 1) + dt_bias.reshape(-1).astype(jnp.float32)[:, None])
    return q, k, v, beta, g


def delta_branch(p_lat, p_ctx, z, conv_w, a_log, dt_bias, norm_w):
    ql, kl, vl, bl, gl = delta_inputs(*p_lat, conv_w, a_log, dt_bias)
    qc, kc, vc, bc, gc = delta_inputs(*p_ctx, conv_w, a_log, dt_bias)
    H = DN_HEADS
    B, L, _ = z.shape
    s0 = jnp.zeros((B, H, DN_DK, DN_DV), jnp.float32)
    flip = lambda t: jnp.flip(t, axis=2)
    _, s_ctx_f = gated_delta_chunked(qc, kc, vc, bc[:, :H], gc[:, :H], s0)
    _, s_ctx_b = gated_delta_chunked(flip(qc), flip(kc), flip(vc), flip(bc[:, H:]), flip(gc[:, H:]), s0)
    o_f, _ = gated_delta_chunked(ql, kl, vl, bl[:, :H], gl[:, :H], s_ctx_f)
    o_b, _ = gated_delta_chunked(flip(ql), flip(kl), flip(vl), flip(bl[:, H:]), flip(gl[:, H:]), s_ctx_b)
    o = (o_f + flip(o_b)).transpose(0, 2, 1, 3)
    o = rmsnorm(o, norm_w) * jax.nn.silu(z.reshape(B, L, H, DN_DV).astype(jnp.float32))
    return o.reshape(B, L, DN_W).astype(z.dtype)


def axial_rope_tables(n_tokens):
    pos = jnp.arange(n_tokens)
    row = (pos // GRID_W).astype(jnp.float32)
    col = (pos % GRID_W).astype(jnp.float32)
    half = NA_DH // 2
    inv_freq = ROPE_THETA ** (-jnp.arange(0, half, 2, dtype=jnp.float32) / half)
    ang_r = row[:, None] * inv_freq[None, :]
    ang_c = col[:, None] * inv_freq[None, :]
    return (jnp.cos(ang_r)[:, None, :], jnp.sin(ang_r)[:, None, :],
            jnp.cos(ang_c)[:, None, :], jnp.sin(ang_c)[:, None, :])


def _rotate(x, cos, sin):
    x1, x2 = jnp.split(x, 2, axis=-1)
    return jnp.concatenate([x1 * cos - x2 * sin, x2 * cos + x1 * sin], axis=-1)


def apply_axial_rope(x, tabs):
    cr, sr, cc, sc = tabs
    xr, xc = jnp.split(x.astype(jnp.float32), 2, axis=-1)
    return jnp.concatenate([_rotate(xr, cr, sr), _rotate(xc, cc, sc)], axis=-1)


def neighbourhood_attention(q, k, v, k_ctx, v_ctx, rpb, rows):
    B, H, _, _, dh = q.shape
    kh = min(NA_KH_MAX, rows)
    n_cb = GRID_W // NA_KW
    kcb = 2 * NA_KW
    qcol = np.arange(GRID_W).reshape(n_cb, NA_KW)
    qstart = np.clip(qcol - NA_KW // 2, 0, GRID_W - NA_KW)
    kstart = np.clip(np.arange(n_cb) * NA_KW - NA_KW // 2, 0, GRID_W - kcb)
    kcol = kstart[:, None] + np.arange(kcb)[None, :]
    in_win = (kcol[:, None, :] >= qstart[:, :, None]) & (kcol[:, None, :] < qstart[:, :, None] + NA_KW)
    dc_idx = np.clip(kcol[:, None, :] - qcol[:, :, None] + NA_KW - 1, 0, 2 * NA_KW - 2)
    rpb_c = rpb[:, :, dc_idx].astype(jnp.float32)
    mask = jnp.asarray(in_win)[:, :, None, :]
    n_loc = kh * kcb

    def row_block(r):
        rs = jnp.clip(r - kh // 2, 0, rows - kh)
        kb = lax.dynamic_slice_in_dim(k, rs, kh, axis=2)[:, :, :, kcol]
        vb = lax.dynamic_slice_in_dim(v, rs, kh, axis=2)[:, :, :, kcol]
        qr = lax.dynamic_index_in_dim(q, r, axis=2, keepdims=False).reshape(B, H, n_cb, NA_KW, dh)
        dr_idx = rs + jnp.arange(kh) - r + NA_KH_MAX - 1
        bias = jnp.take(rpb_c, dr_idx, axis=1).transpose(0, 2, 3, 1, 4)
        s_loc = jnp.einsum('bhnqd,bhinkd->bhnqik', qr, kb).astype(jnp.float32) + bias
        s_loc = jnp.where(mask, s_loc, NEG_INF).reshape(B, H, n_cb, NA_KW, n_loc)
        s_ctx = jnp.einsum('bhnqd,bhcd->bhnqc', qr, k_ctx).astype(jnp.float32)
        p = jax.nn.softmax(jnp.concatenate([s_loc, s_ctx], axis=-1), axis=-1).astype(v.dtype)
        p_loc = p[..., :n_loc].reshape(B, H, n_cb, NA_KW, kh, kcb)
        o = (jnp.einsum('bhnqik,bhinkd->bhnqd', p_loc, vb)
             + jnp.einsum('bhnqc,bhcd->bhnqd', p[..., n_loc:], v_ctx))
        return o.reshape(B, H, GRID_W, dh)

    return lax.map(row_block, jnp.arange(rows))


def na_branch(pq, pk, pv, pk_ctx, pv_ctx, q_norm_w, k_norm_w, rpb, tabs):
    B, L, _ = pq.shape
    rows = L // GRID_W
    dtype = pv.dtype
    heads = lambda t: t.reshape(t.shape[0], t.shape[1], NA_HEADS, NA_DH)
    q = apply_axial_rope(rmsnorm(heads(pq), q_norm_w), tabs) * NA_DH ** -0.5
    k = apply_axial_rope(rmsnorm(heads(pk), k_norm_w), tabs)
    grid = lambda t: t.astype(dtype).reshape(B, rows, GRID_W, NA_HEADS, NA_DH).transpose(0, 3, 1, 2, 4)
    q, k, v = grid(q), grid(k), grid(heads(pv))
    k_ctx = rmsnorm(heads(pk_ctx), k_norm_w).transpose(0, 2, 1, 3)
    v_ctx = heads(pv_ctx).transpose(0, 2, 1, 3)
    out = neighbourhood_attention(q, k, v, k_ctx, v_ctx, rpb, rows)
    return out.transpose(1, 0, 3, 2, 4).reshape(B, L, NA_W)


def expert_choice_moe(h, w_router, w1, w3, w2):
    B, L, D = h.shape
    cap = EC_CAPACITY_FACTOR * L // N_EXPERTS
    aff = jax.nn.softmax((h @ w_router).astype(jnp.float32), axis=-1)
    gate, idx = lax.top_k(aff.transpose(0, 2, 1), cap)
    bidx = jnp.arange(B)[:, None, None]
    xg = h[bidx, idx]
    hid = jax.nn.silu(jnp.einsum('becd,edf->becf', xg, w1)) * jnp.einsum('becd,edf->becf', xg, w3)
    ye = jnp.einsum('becf,efd->becd', hid, w2) * gate[..., None].astype(h.dtype)
    return jnp.zeros_like(h).at[bidx, idx].add(ye)


def setup_inputs(seed: int = 0) -> dict:
    key = jax.random.key(seed)
    ks = jax.random.split(key, 24)
    f32 = jnp.float32

    def nrm(k, shape, scale):
        return jax.random.normal(k, shape, f32) * scale

    dt = jnp.exp(jax.random.uniform(ks[9], (DEPTH, 2, DN_HEADS), f32, minval=math.log(1e-3), maxval=math.log(1e-1)))
    return {
        "x": nrm(ks[0], (BATCH, SEQ, D_MODEL), 1.0),
        "c": nrm(ks[1], (BATCH, D_MODEL), 1.0),
        "ctx": nrm(ks[2], (BATCH, CTX_LEN, D_MODEL), 1.0),
        "c_ctx": nrm(ks[3], (D_MODEL,), 1.0),
        "ada_w": nrm(ks[4], (DEPTH, D_MODEL, 6 * D_MODEL), D_MODEL ** -0.5),
        "ada_b": nrm(ks[5], (DEPTH, 6 * D_MODEL), 0.01),
        "norm1_w": 1.0 + nrm(ks[6], (DEPTH, D_MODEL), 0.02),
        "w_in": nrm(ks[7], (DEPTH, D_MODEL, IN_W), D_MODEL ** -0.5),
        "conv_w": nrm(ks[8], (DEPTH, CONV_K, DN_QKV_W), CONV_K ** -0.5),
        "dn_a_log": jnp.log(jax.random.uniform(ks[10], (DEPTH, 2, DN_HEADS), f32, minval=1.0, maxval=16.0)),
        "dn_dt_bias": dt + jnp.log(-jnp.expm1(-dt)),
        "dn_norm_w": 1.0 + nrm(ks[11], (DEPTH, DN_DV), 0.02),
        "na_q_norm_w": 1.0 + nrm(ks[12], (DEPTH, NA_DH), 0.02),
        "na_k_norm_w": 1.0 + nrm(ks[13], (DEPTH, NA_DH), 0.02),
        "na_rpb": nrm(ks[14], (DEPTH, NA_HEADS, 2 * NA_KH_MAX - 1, 2 * NA_KW - 1), 0.1),
        "w_branch_a": nrm(ks[15], (DEPTH, DN_W, D_MODEL), DN_W ** -0.5),
        "w_branch_b": nrm(ks[16], (DEPTH, NA_W, D_MODEL), NA_W ** -0.5),
        "w_out": nrm(ks[17], (DEPTH, D_MODEL, D_MODEL), D_MODEL ** -0.5),
        "norm2_w": 1.0 + nrm(ks[18], (DEPTH, D_MODEL), 0.02),
        "w_router": nrm(ks[19], (DEPTH, D_MODEL, N_EXPERTS), D_MODEL ** -0.5),
        "expert_w1": nrm(ks[20], (DEPTH, N_EXPERTS, D_MODEL, EXPERT_FF), D_MODEL ** -0.5),
        "expert_w3": nrm(ks[21], (DEPTH, N_EXPERTS, D_MODEL, EXPERT_FF), D_MODEL ** -0.5),
        "expert_w2": nrm(ks[22], (DEPTH, N_EXPERTS, EXPERT_FF, D_MODEL), EXPERT_FF ** -0.5),
    }


def reference(x, c, ctx, c_ctx, ada_w, ada_b, norm1_w, w_in, conv_w, dn_a_log, dn_dt_bias, dn_norm_w,
              na_q_norm_w, na_k_norm_w, na_rpb, w_branch_a, w_branch_b, w_out, norm2_w, w_router,
              expert_w1, expert_w3, expert_w2):
    B, L, D = x.shape
    tabs = axial_rope_tables(L)
    split_at = np.cumsum(IN_SIZES)[:-1].tolist()
    for i in range(DEPTH):
        mod = jax.nn.silu(c) @ ada_w[i] + ada_b[i]
        sh1, sc1, g1, sh2, sc2, g2 = [m[:, None, :] for m in jnp.split(mod, 6, axis=-1)]
        mod_c = jax.nn.silu(c_ctx) @ ada_w[i] + ada_b[i]
        sh1c, sc1c = mod_c[:D], mod_c[D:2 * D]
        h = rmsnorm(x, norm1_w[i]) * (1 + sc1) + sh1
        hc = rmsnorm(ctx, norm1_w[i]) * (1 + sc1c) + sh1c
        (dq, dk, dv, dz, dbeta, da, nq, nk, nv, gate_a, gate_b) = jnp.split(h @ w_in[i], split_at, axis=-1)
        (cdq, cdk, cdv, _, cbeta, cda, _, cnk, cnv, _, _) = jnp.split(hc @ w_in[i], split_at, axis=-1)
        y_a = delta_branch((dq, dk, dv, dbeta, da), (cdq, cdk, cdv, cbeta, cda), dz, conv_w[i],
                           dn_a_log[i], dn_dt_bias[i], dn_norm_w[i]) @ w_branch_a[i]
        y_b = na_branch(nq, nk, nv, cnk, cnv, na_q_norm_w[i], na_k_norm_w[i], na_rpb[i], tabs) @ w_branch_b[i]
        y = jax.nn.sigmoid(gate_a) * y_a + jax.nn.sigmoid(gate_b) * y_b
        x = x + g1 * (y @ w_out[i])
        h2 = rmsnorm(x, norm2_w[i]) * (1 + sc2) + sh2
        x = x + g2 * expert_choice_moe(h2, w_router[i], expert_w1[i], expert_w3[i], expert_w2[i])
    return x
```

```python
import os
import numpy as np
from contextlib import ExitStack
import ml_dtypes
import concourse.bass as bass
import concourse.mybir as mybir
from concourse.bass_utils import run_bass_kernel_spmd

F32 = mybir.dt.float32
BF16 = mybir.dt.bfloat16
F32R = mybir.dt.float32r
ALU = mybir.AluOpType
AF = mybir.ActivationFunctionType
AX = mybir.AxisListType

D = 2048
L = 2048
CTX = 256
T = L + CTX
INW = 18496
NCH = T // 128
EPS = 1e-6
SEM_ROLL = 30000


class Buf:
    __slots__ = ("name", "lw", "rd", "excl")

    def __init__(self, name=""):
        self.name = name
        self.lw = None
        self.rd = []
        self.excl = False


class TL:
    def __init__(self, t, name=""):
        self.t = t
        self.b = Buf(name)

    def __getitem__(self, k):
        return self.t[k]


def _b(x):
    return x.b if isinstance(x, TL) else x


class MK:
    ENG = ("pe", "act", "dve", "pool", "sp")

    def __init__(self, nc, es):
        self.nc = nc
        self.es = es
        self.eo = {"pe": nc.tensor, "act": nc.scalar, "dve": nc.vector, "pool": nc.gpsimd, "sp": nc.sync}
        self.cnt = {e: 0 for e in self.ENG}
        self.semi = 0
        self.cur = {}
        for e in self.ENG:
            self.cur[e] = self._newsem()
        self.seen = {e: {} for e in self.ENG}
        self.dsem = {}
        self.dpos = {}
        for q in ("sp", "act", "pool"):
            n = 16 if q == "sp" else 4
            self.dsem[q] = [[self._newsem(), 0, None] for _ in range(n)]
            self.dpos[q] = 0
        self.ninst = 0

    def _newsem(self):
        self.semi += 1
        return self.es.enter_context(self.nc.semaphore("s%d" % self.semi))

    def _wait(self, e, ev):
        if ev is None:
            return
        sem, val, src = ev
        if src == e and e == "pe":
            return
        key = id(sem)
        if self.seen[e].get(key, 0) >= val:
            return
        self.seen[e][key] = val
        self.eo[e].wait_ge(sem, val)

    def _deps(self, e, reads, writes):
        for b in reads:
            self._wait(e, b.lw)
        for b in writes:
            self._wait(e, b.lw)
            for r in b.rd:
                self._wait(e, r)

    def _commit(self, ev, reads, writes):
        for b in writes:
            b.lw = ev
            b.rd = []
        for b in reads:
            if b not in writes:
                b.rd.append(ev)
                if len(b.rd) > 16:
                    d = {}
                    for r in b.rd:
                        k = id(r[0])
                        if k not in d or d[k][1] < r[1]:
                            d[k] = r
                    b.rd = list(d.values())

    def op(self, e, fn, reads=(), writes=()):
        reads = [_b(x) for x in reads]
        writes = [_b(x) for x in writes]
        writes = writes + [b for b in reads if b.excl and b not in writes]
        reads = [b for b in reads if not b.excl]
        self._deps(e, reads, writes)
        if self.cnt[e] >= SEM_ROLL:
            self.cur[e] = self._newsem()
            self.cnt[e] = 0
        self.cnt[e] += 1
        ev = (self.cur[e], self.cnt[e], e)
        fn(self.eo[e]).then_inc(ev[0], 1)
        self._commit(ev, reads, writes)
        self.ninst += 1
        return ev

    def dma(self, q, out, in_, reads=(), writes=(), **kw):
        reads = [_b(x) for x in reads]
        writes = [_b(x) for x in writes]
        self._deps(q, reads, writes)
        slot = self.dsem[q][self.dpos[q]]
        self.dpos[q] = (self.dpos[q] + 1) % len(self.dsem[q])
        if slot[2] is not None:
            self._wait(q, slot[2])
        slot[1] += 16
        ev = (slot[0], slot[1], "dma")
        slot[2] = ev
        self.eo[q].dma_start(out=out, in_=in_, **kw).then_inc(slot[0], 16)
        self._commit(ev, reads, writes)
        self.ninst += 1
        return ev

    def barrier(self):
        evs = []
        for e in self.ENG:
            if self.cnt[e] > 0:
                evs.append((self.cur[e], self.cnt[e], "x"))
        for q in self.dsem:
            for slot in self.dsem[q]:
                if slot[2] is not None:
                    evs.append(slot[2])
        for e in self.ENG:
            for ev in evs:
                self._wait(e, ev)


class Ctx:
    pass


_UID = [0]


def _alloc(nc, stack, kind, name, shape, dt):
    f = nc.sbuf_tensor if kind == "sb" else nc.psum_tensor
    _UID[0] += 1
    name = "%s_%s_%d" % (kind, name, _UID[0])
    tl = TL(stack.enter_context(f(name, list(shape), dt)), name)
    if kind == "ps":
        tl.b.excl = True
    return tl


def _consts():
    c = {}
    i = np.arange(128)
    c["ident"] = np.eye(128, dtype=np.float32)
    c["ones"] = np.ones((128, 128), np.float32)
    c["uf"] = (i[:, None] <= i[None, :]).astype(np.float32)
    c["ub"] = (i[:, None] >= i[None, :]).astype(np.float32)
    NEG = -1.0e6
    c["mf"] = np.where(i[None, :] >= i[:, None], 0.0, NEG).astype(np.float32)
    c["mb"] = np.where(i[None, :] <= i[:, None], 0.0, NEG).astype(np.float32)
    c["offd"] = (1.0 - np.eye(128)).astype(np.float32)
    R = np.zeros((128, 128), np.float32)
    for p in range(128):
        q = p + 32 if (p % 64) < 32 else p - 32
        R[q, p] = 1.0
    c["rot"] = R
    return c


CONST_NAMES = ["ident", "ones", "uf", "ub", "mf", "mb", "offd", "rot"]


def _rope_tables():
    pos = np.arange(L)
    row = (pos // 64).astype(np.float32)
    col = (pos % 64).astype(np.float32)
    half = 64
    inv = (np.float32(10000.0) ** (-np.arange(0, half, 2, dtype=np.float32) / np.float32(half))).astype(np.float32)
    ang_r = row[:, None] * inv[None, :]
    ang_c = col[:, None] * inv[None, :]
    cr, sr, cc, sc = np.cos(ang_r), np.sin(ang_r), np.cos(ang_c), np.sin(ang_c)
    COS = np.concatenate([cr, cr, cc, cc], axis=1).T.astype(np.float32)
    SIN = np.concatenate([-sr, sr, -sc, sc], axis=1).T.astype(np.float32)
    return np.ascontiguousarray(COS), np.ascontiguousarray(SIN)


NA_BLOCKS = [(0, 6), (4, 8), (12, 8), (20, 6)]


def _na_bias_table(rpb):
    H = rpb.shape[0]
    out = np.full((H, 28, 128, 512), -30000.0, np.float32)
    ti = 0
    qq = np.arange(512)
    qr_l, qc = qq // 64, qq % 64
    kk = np.arange(128)
    kr_l, kc = kk // 64, kk % 64
    qstart = np.clip(qc - 8, 0, 48)
    for m, (lo, nt) in enumerate(NA_BLOCKS):
        qr = 8 * m + qr_l
        rs = np.clip(qr - 4, 0, 24)
        for j in range(nt):
            kr = lo + 2 * j + kr_l
            okr = (kr[:, None] >= rs[None, :]) & (kr[:, None] < rs[None, :] + 8) & (kr[:, None] < 32)
            okc = (kc[:, None] >= qstart[None, :]) & (kc[:, None] < qstart[None, :] + 16)
            ok = okr & okc
            dr = np.clip(kr[:, None] - qr[None, :] + 7, 0, 14)
            dc = np.clip(kc[:, None] - qc[None, :] + 15, 0, 30)
            g = rpb[:, dr, dc]
            out[:, ti] = np.where(ok[None], g, np.float32(-30000.0))
            ti += 1
    return out


def build_nc(upto=99, dump=(), feed=(), run=None, heads=range(16)):
    nc = bass.Bass("TRN2", target_bir_lowering=False)
    g = Ctx()
    g.nc = nc

    def din(name, shape, dt=F32):
        return nc.dram_tensor(name, list(shape), dt, kind="ExternalInput").ap()

    def dscr(name, shape, dt=F32):
        kind = "ExternalOutput" if name in dump else ("ExternalInput" if name in feed else "Internal")
        return TL(nc.dram_tensor(name, list(shape), dt, kind=kind).ap(), name)

    I = Ctx()
    I.xT = din("xT", [16, 128, T])
    I.cs = din("cs", [128, 32])
    I.ada_w = din("ada_w", [D, 6 * D])
    I.ada_b = din("ada_b", [128, 96])
    I.n1w = din("n1w", [128, 16])
    I.n2w = din("n2w", [128, 16])
    I.w_in = din("w_in", [D, INW])
    I.conv_w = din("conv_w", [128, 48, 5])
    I.dn_sc = din("dn_sc", [128, 64])
    I.hw = din("hw", [128, 3])
    I.consts = din("consts", [len(CONST_NAMES), 128, 128])
    I.ropec = din("ropec", [128, L])
    I.ropes = din("ropes", [128, L])
    I.nab = din("nab", [16, 28, 128, 512])
    I.iota = din("iota", [128, 258])
    I.dnmask = din("dnmask", [4, 128, 128])
    I.w_a = din("w_a", [D, D])
    I.w_b = din("w_b", [D, D])
    I.w_out = din("w_out", [D, D])
    I.w_r = din("w_r", [D, 16])
    I.ew1 = din("ew1", [16, D, 1024])
    I.ew3 = din("ew3", [16, D, 1024])
    I.ew2 = din("ew2", [16, 1024, D])
    outT = TL(nc.dram_tensor("outT", [16, 128, L], F32, kind="ExternalOutput").ap(), "outT")

    S = Ctx()
    S.dn = dscr("s_dn", [48, 128, T])
    S.z = dscr("s_z", [16, 128, L])
    S.ba = dscr("s_ba", [T, 64])
    S.nq = dscr("s_nq", [16, 128, L])
    S.nk = dscr("s_nk", [16, 128, T])
    S.nv = dscr("s_nv", [T, D], BF16)
    S.ga = dscr("s_ga", [16, 128, L])
    S.gb = dscr("s_gb", [16, 128, L])
    S.odn = dscr("s_odn", [16, 128, L], BF16)
    S.ona = dscr("s_ona", [16, 128, L], BF16)
    S.y = dscr("s_y", [16, 128, L], BF16)
    S.x1 = dscr("s_x1", [16, 128, L])
    S.ye = dscr("s_ye", [32, 128, D], BF16)
    S.hT = dscr("s_hT", [16, 128, T], BF16)
    S.grow = dscr("s_grow", [32, T])
    S.brow = dscr("s_brow", [32, T])

    with ExitStack() as es:
        mk = MK(nc, es)
        g.mk = mk

        def sbt(stack, name, shape, dt=F32):
            return _alloc(nc, stack, "sb", name, shape, dt)

        def pst(stack, name, shape, dt=F32):
            return _alloc(nc, stack, "ps", name, shape, dt)

        K = {}
        for i, n in enumerate(CONST_NAMES):
            K[n] = sbt(es, "k_" + n, [128, 128])
            mk.dma("sp", K[n][:], I.consts[i], writes=[K[n]])
        onesb = sbt(es, "onesb", [128, 128], BF16)
        identb = sbt(es, "identb", [128, 128], BF16)
        mk.op("dve", lambda e: e.tensor_copy(onesb[:], K["ones"][:]), reads=[K["ones"]], writes=[onesb])
        mk.op("dve", lambda e: e.tensor_copy(identb[:], K["ident"][:]), reads=[K["ident"]], writes=[identb])
        epsT = sbt(es, "epsT", [128, 1])
        mk.op("dve", lambda e: e.memset(epsT[:], EPS), writes=[epsT])
        modsb = sbt(es, "modsb", [128, 96, 2])
        a1 = sbt(es, "a1", [128, 16]); a1c = sbt(es, "a1c", [128, 16]); a2 = sbt(es, "a2", [128, 16])
        n1w = sbt(es, "n1w", [128, 16]); n2w = sbt(es, "n2w", [128, 16])
        hw = sbt(es, "hw", [128, 3])
        mk.dma("sp", n1w[:], I.n1w, writes=[n1w])
        mk.dma("sp", n2w[:], I.n2w, writes=[n2w])
        mk.dma("sp", hw[:], I.hw, writes=[hw])

        def phase_mod():
            with ExitStack() as ps:
                cs = sbt(ps, "cs", [128, 32]); sc_ = sbt(ps, "silu_c", [128, 32])
                adab = sbt(ps, "adab", [128, 96])
                wb = [sbt(ps, "adaw%d" % i, [128, 16, 512]) for i in range(2)]
                pp = [pst(ps, "p0_%d" % i, [128, 512]) for i in range(2)]
                mk.dma("sp", cs[:], I.cs, writes=[cs])
                mk.dma("sp", adab[:], I.ada_b, writes=[adab])
                mk.op("act", lambda e: e.activation(sc_[:], cs[:], AF.Silu), reads=[cs], writes=[sc_])
                wv = I.ada_w.rearrange("(k p) n -> p k n", p=128)
                for nb in range(24):
                    w = wb[nb % 2]
                    mk.dma("sp", w[:], wv[:, :, nb * 512:(nb + 1) * 512], writes=[w])
                    for ct in range(4):
                        j = nb * 4 + ct
                        p = pp[j % 2]
                        for k in range(16):
                            mk.op("pe", lambda e, p=p, w=w, k=k, ct=ct: e.matmul(
                                p[:, 0:2], w[:, k, ct * 128:(ct + 1) * 128], sc_[:, 2 * k:2 * k + 2],
                                start=(k == 0), stop=(k == 15)), reads=[w, sc_], writes=[p])
                        mk.op("dve", lambda e, p=p, j=j: e.tensor_scalar(
                            modsb[:, j, :], p[:, 0:2], adab[:, j:j + 1], None, op0=ALU.add),
                            reads=[p, adab], writes=[modsb])
                mk.op("dve", lambda e: e.scalar_tensor_tensor(a1[:], modsb[:, 16:32, 0], 1.0, n1w[:], op0=ALU.add, op1=ALU.mult),
                      reads=[modsb, n1w], writes=[a1])
                mk.op("dve", lambda e: e.scalar_tensor_tensor(a1c[:], modsb[:, 16:32, 1], 1.0, n1w[:], op0=ALU.add, op1=ALU.mult),
                      reads=[modsb, n1w], writes=[a1c])
                mk.op("dve", lambda e: e.scalar_tensor_tensor(a2[:], modsb[:, 64:80, 0], 1.0, n2w[:], op0=ALU.add, op1=ALU.mult),
                      reads=[modsb, n2w], writes=[a2])
            mk.barrier()

        def norm_mod(ps_bank, xt, sq, rs, tmp2, w, a_t, bcol, out_fn, extra_reads=()):
            mk.op("act", lambda e: e.activation(sq[:, :, :w], xt[:, :, :w], AF.Square), reads=[xt], writes=[sq])
            for k in range(16):
                mk.op("pe", lambda e, k=k: e.matmul(ps_bank[:, :w], K["ones"][:], sq[:, k, :w], start=(k == 0), stop=(k == 15)),
                      reads=[K["ones"], sq], writes=[ps_bank])
            mk.op("act", lambda e: e.activation(rs[:, :w], ps_bank[:, :w], AF.Ln, bias=epsT[:, 0:1], scale=1.0 / D),
                  reads=[ps_bank, epsT], writes=[rs])
            mk.op("act", lambda e: e.activation(rs[:, :w], rs[:, :w], AF.Exp, scale=-0.5), reads=[rs], writes=[rs])
            for k in range(16):
                tm = tmp2[k % 2]
                mk.op("dve", lambda e, k=k, tm=tm: e.tensor_tensor(tm[:, :w], xt[:, k, :w], rs[:, :w], op=ALU.mult),
                      reads=[xt, rs], writes=[tm])
                o, ow = out_fn(k)
                mk.op("act", lambda e, k=k, tm=tm, o=o: e.activation(o, tm[:, :w], AF.Identity, bias=bcol(k), scale=a_t[:, k:k + 1]),
                      reads=[tm, a_t, modsb] + list(extra_reads), writes=[ow])

        TOK_TILES = [(0, 512), (512, 512), (1024, 512), (1536, 512), (2048, 256)]

        def phase_inproj():
            with ExitStack() as ps:
                hT = sbt(ps, "hT", [128, 16, T], BF16)
                pb = [pst(ps, "p1_%d" % i, [128, 512]) for i in range(8)]
                with ExitStack() as p1:
                    xts = [sbt(p1, "xt%d" % i, [128, 16, 512]) for i in range(2)]
                    sq = sbt(p1, "sq", [128, 16, 512])
                    rs = sbt(p1, "rs", [128, 512])
                    tmp2 = [sbt(p1, "tmp%d" % i, [128, 512]) for i in range(2)]
                    xv = I.xT.rearrange("k p t -> p k t")
                    for ti, (t0, w) in enumerate(TOK_TILES):
                        xt = xts[ti % 2]
                        mk.dma("sp", xt[:, :, :w], xv[:, :, t0:t0 + w], writes=[xt])
                        ctxp = (t0 >= L)
                        norm_mod(pb[ti % 2], xt, sq, rs, tmp2, w, a1c if ctxp else a1,
                                 (lambda k, c=(1 if ctxp else 0): modsb[:, k, c:c + 1]),
                                 lambda k, t0=t0, w=w: (hT[:, k, t0:t0 + w], hT))
                    if "s_hT" in dump:
                        mk.dma("sp", S.hT.t.rearrange("k p t -> p k t"), hT[:], reads=[hT], writes=[S.hT])
                mk.barrier()
                if upto < 2:
                    return
                stg = [sbt(ps, "wstg%d" % i, [128, 16, 256]) for i in range(2)]
                wbf = [sbt(ps, "wbf%d" % i, [128, 16, 256], BF16) for i in range(2)]
                ost = [sbt(ps, "ost%d" % i, [128, T]) for i in range(2)]
                vst = [sbt(ps, "vst%d" % i, [128, NCH, 256], BF16) for i in range(2)]
                bst = sbt(ps, "bst", [128, NCH, 64])
                wv = I.w_in.rearrange("(k p) n -> p k n", p=128)
                blocks = []
                for c0 in range(0, 6144, 256):
                    blocks.append((c0, 256, "fm", (S.dn, c0 // 128), T, "copy"))
                for c0 in range(6144, 8192, 256):
                    blocks.append((c0, 256, "fm", (S.z, (c0 - 6144) // 128), L, "silu"))
                blocks.append((8192, 64, "ba", None, T, None))
                for c0 in range(8256, 10304, 256):
                    blocks.append((c0, 256, "fm", (S.nq, (c0 - 8256) // 128), L, "copy"))
                for c0 in range(10304, 12352, 256):
                    blocks.append((c0, 256, "fm", (S.nk, (c0 - 10304) // 128), T, "copy"))
                for c0 in range(12352, 14400, 256):
                    blocks.append((c0, 256, "tm", c0 - 12352, T, None))
                for c0 in range(14400, 16448, 256):
                    blocks.append((c0, 256, "fm", (S.ga, (c0 - 14400) // 128), L, "sig"))
                for c0 in range(16448, 18496, 256):
                    blocks.append((c0, 256, "fm", (S.gb, (c0 - 16448) // 128), L, "sig"))
                st = {"pi": 0, "oi": 0}

                def load(bi):
                    c0, nc_, *_ = blocks[bi]
                    s_ = stg[bi % 2]; wb_ = wbf[bi % 2]
                    mk.dma("sp", s_[:, :, :nc_], wv[:, :, c0:c0 + nc_], writes=[s_])
                    mk.op("act", lambda e: e.copy(wb_[:, 0:8, :nc_], s_[:, 0:8, :nc_]), reads=[s_], writes=[wb_])
                    mk.op("dve", lambda e: e.tensor_copy(wb_[:, 8:16, :nc_], s_[:, 8:16, :nc_]), reads=[s_], writes=[wb_])

                load(0)
                for bi, (c0, nc_, kind, dst, thi, epi) in enumerate(blocks):
                    if bi + 1 < len(blocks):
                        load(bi + 1)
                    wb_ = wbf[bi % 2]
                    if kind == "fm":
                        for ctile in range(nc_ // 128):
                            o = ost[st["oi"] % 2]; st["oi"] += 1
                            for (t0, w) in TOK_TILES:
                                if t0 >= thi:
                                    continue
                                p = pb[st["pi"] % 8]; st["pi"] += 1
                                for k in range(16):
                                    mk.op("pe", lambda e, p=p, k=k, ctile=ctile, t0=t0, w=w: e.matmul(
                                        p[:, :w], wb_[:, k, ctile * 128:(ctile + 1) * 128], hT[:, k, t0:t0 + w],
                                        start=(k == 0), stop=(k == 15)), reads=[wb_, hT], writes=[p])
                                if epi == "copy":
                                    mk.op("dve", lambda e, p=p, o=o, t0=t0, w=w: e.tensor_copy(o[:, t0:t0 + w], p[:, :w]), reads=[p], writes=[o])
                                else:
                                    fn = AF.Silu if epi == "silu" else AF.Sigmoid
                                    mk.op("act", lambda e, p=p, o=o, t0=t0, w=w, fn=fn: e.activation(o[:, t0:t0 + w], p[:, :w], fn), reads=[p], writes=[o])
                            dt_, ti_ = dst
                            mk.dma("sp", dt_.t[ti_ + ctile][:, 0:thi], o[:, 0:thi], reads=[o], writes=[dt_])
                    elif kind == "tm":
                        vs = vst[st["oi"] % 2]; st["oi"] += 1
                        for ti in range(NCH):
                            p = pb[st["pi"] % 8]; st["pi"] += 1
                            for k in range(16):
                                mk.op("pe", lambda e, p=p, k=k, ti=ti: e.matmul(
                                    p[:, :256], hT[:, k, ti * 128:(ti + 1) * 128], wb_[:, k, :256],
                                    start=(k == 0), stop=(k == 15)), reads=[wb_, hT], writes=[p])
                            eng = "dve" if ti % 2 == 0 else "act"
                            if eng == "dve":
                                mk.op("dve", lambda e, p=p, ti=ti: e.tensor_copy(vs[:, ti, :], p[:, :256]), reads=[p], writes=[vs])
                            else:
                                mk.op("act", lambda e, p=p, ti=ti: e.copy(vs[:, ti, :], p[:, :256]), reads=[p], writes=[vs])
                        mk.dma("sp", S.nv.t.rearrange("(n p) c -> p n c", p=128)[:, :, dst:dst + 256], vs[:], reads=[vs], writes=[S.nv])
                    else:
                        for ti in range(NCH):
                            p = pb[st["pi"] % 8]; st["pi"] += 1
                            for k in range(16):
                                mk.op("pe", lambda e, p=p, k=k, ti=ti: e.matmul(
                                    p[:, :64], hT[:, k, ti * 128:(ti + 1) * 128], wb_[:, k, :64],
                                    start=(k == 0), stop=(k == 15)), reads=[wb_, hT], writes=[p])
                            mk.op("dve", lambda e, p=p, ti=ti: e.tensor_copy(bst[:, ti, :], p[:, :64]), reads=[p], writes=[bst])
                        mk.dma("sp", S.ba.t.rearrange("(n p) c -> p n c", p=128), bst[:], reads=[bst], writes=[S.ba])
            mk.barrier()

        g.phase_mod = phase_mod
        g.phase_inproj = phase_inproj
        phases_extra(g, I, S, K, mk, sbt, pst, es, outT, dict(
            modsb=modsb, a2=a2, hw=hw, epsT=epsT, onesb=onesb, identb=identb, norm_mod=norm_mod, dump=dump, upto=upto))

        if run is None:
            run = ("mod", "inproj", "dn", "na", "merge", "moe")
        if "mod" in run:
            phase_mod()
        if "inproj" in run:
            phase_inproj()
        if "dn" in run:
            g.phase_dn(heads)
        if "na" in run:
            g.phase_na(heads)
        if "merge" in run:
            g.phase_merge()
        if "moe" in run:
            g.phase_moe()
        if "modsb" in dump:
            md = nc.dram_tensor("modsb_o", [128, 192], F32, kind="ExternalOutput").ap()
            mk.dma("sp", md, modsb[:].rearrange("p a b -> p (a b)"), reads=[modsb])
        mk.barrier()
        g.ninst = mk.ninst
    return nc, g


def phases_extra(g, I, S, K, mk, sbt, pst, es, outT, X):
    nc = g.nc
    modsb = X["modsb"]; hw = X["hw"]; epsT = X["epsT"]; onesb = X["onesb"]; identb = X["identb"]
    dump = X["dump"]
    TOK5 = [(0, 512), (512, 512), (1024, 512), (1536, 512), (2048, 256)]
    ident = K["ident"]; ones = K["ones"]

    def phase_dn(heads=range(16)):
        with ExitStack() as ps:
            pw = [pst(ps, "dnw%d" % i, [128, 512]) for i in range(2)]
            pqb = [pst(ps, "dnq%d" % i, [128, 512]) for i in range(6)]
            class SlotV(TL):
                def __init__(self, bank, j):
                    self.t = bank.t[:, j * 128:(j + 1) * 128]
                    self.b = bank.b

            banks = [[SlotV(pqb[i], j) for j in range(4)] for i in range(6)]
            sl = {"i": 0, "w": 0}

            def nbank():
                sl["i"] += 1
                return banks[sl["i"] % 6]

            def nwide():
                sl["w"] += 1
                return pw[sl["w"] % 2]

            ba = sbt(ps, "ba", [128, NCH, 64]); dsc = sbt(ps, "dsc", [128, 64]); negexp = sbt(ps, "negexp", [128, 32])
            betaC = sbt(ps, "betaC", [128, NCH, 32]); gC = sbt(ps, "gC", [128, NCH, 32])
            cw = sbt(ps, "cw", [128, 48, 5])
            mk.dma("sp", ba[:], S.ba.t.rearrange("(n p) c -> p n c", p=128), reads=[S.ba], writes=[ba])
            mk.dma("sp", dsc[:], I.dn_sc, writes=[dsc])
            mk.dma("sp", cw[:], I.conv_w, writes=[cw])
            mk.op("act", lambda e: e.activation(negexp[:], dsc[:, 0:32], AF.Exp), reads=[dsc], writes=[negexp])
            mk.op("dve", lambda e: e.tensor_scalar(negexp[:], negexp[:], -1.0, None, op0=ALU.mult), reads=[negexp], writes=[negexp])
            mk.op("act", lambda e: e.activation(betaC[:], ba[:, :, 0:32], AF.Sigmoid), reads=[ba], writes=[betaC])
            for n in range(NCH):
                mk.op("dve", lambda e, n=n: e.tensor_tensor(gC[:, n, :], ba[:, n, 32:64], dsc[:, 32:64], op=ALU.add), reads=[ba, dsc], writes=[gC])
            mk.op("act", lambda e: e.activation(gC[:], gC[:], AF.Exp), reads=[gC], writes=[gC])
            mk.op("act", lambda e: e.activation(gC[:], gC[:], AF.Ln, bias=1.0), reads=[gC], writes=[gC])
            for n in range(NCH):
                mk.op("dve", lambda e, n=n: e.tensor_tensor(gC[:, n, :], gC[:, n, :], negexp[:], op=ALU.mult), reads=[gC, negexp], writes=[gC])

            with ExitStack() as p0:
                growT = [sbt(p0, "growT%d" % i, [16, NCH, 128]) for i in range(2)]
                browT = [sbt(p0, "browT%d" % i, [16, NCH, 128]) for i in range(2)]
                Um0 = [K["uf"], K["ub"]]
                for n in range(NCH):
                    bk = nbank()
                    for d in range(2):
                        mk.op("pe", lambda e, bk=bk, d=d, n=n: e.matmul(bk[d][0:16, :], gC[:, n, d * 16:(d + 1) * 16], Um0[d][:], start=True, stop=True), reads=[gC, Um0[d]], writes=[bk[d]])
                        mk.op("pe", lambda e, bk=bk, d=d, n=n: e.transpose(bk[2 + d][0:16, :], betaC[:, n, d * 16:(d + 1) * 16], ident[:]), reads=[betaC, ident], writes=[bk[2 + d]])
                    for d in range(2):
                        mk.op("dve", lambda e, bk=bk, d=d, n=n: e.tensor_copy(growT[d][:, n, :], bk[d][0:16, :]), reads=[bk[d]], writes=[growT[d]])
                        mk.op("act", lambda e, bk=bk, d=d, n=n: e.copy(browT[d][:, n, :], bk[2 + d][0:16, :]), reads=[bk[2 + d]], writes=[browT[d]])
                for d in range(2):
                    mk.dma("sp", S.grow.t[d * 16:(d + 1) * 16, :], growT[d][:].rearrange("p a b -> p (a b)"), reads=[growT[d]], writes=[S.grow])
                    mk.dma("sp", S.brow.t[d * 16:(d + 1) * 16, :], browT[d][:].rearrange("p a b -> p (a b)"), reads=[browT[d]], writes=[S.brow])
            GBt = [sbt(ps, "GBt%d" % i, [128, T]) for i in range(2)]
            BBt = [sbt(ps, "BBt%d" % i, [128, T]) for i in range(2)]
            raw = sbt(ps, "raw", [128, T]); sqb = raw; rsb = sbt(ps, "rsb", [128, T])
            cq = sbt(ps, "cq", [128, T]); ck = sbt(ps, "ck", [128, T]); cv = sbt(ps, "cv", [128, T])
            ktok = sbt(ps, "ktok", [128, NCH, 128]); vtok = sbt(ps, "vtok", [128, NCH, 128])
            oacc = sbt(ps, "oacc", [128, L]); zt = cv
            oaccB = [Buf("oacc%d" % n) for n in range(16)]
            NS = 8
            uS = sbt(ps, "uS", [128, NS, 128]); wS = sbt(ps, "wS", [128, NS, 128])
            aS = sbt(ps, "aS", [128, NS, 128]); qS = sbt(ps, "qS", [128, NS, 128])
            uV = [TL(uS.t[:, i, :]) for i in range(NS)]; wV = [TL(wS.t[:, i, :]) for i in range(NS)]
            aV = [TL(aS.t[:, i, :]) for i in range(NS)]; qV = [TL(qS.t[:, i, :]) for i in range(NS)]
            W = 4
            wb = {}
            for nm in ["A0", "A1", "B0", "B1", "P0", "P1", "ApI", "dm1", "dm2", "dec", "dec2", "eGb", "BM", "Af"]:
                wb[nm] = [sbt(ps, "w%s%d" % (nm, i), [128, 128]) for i in range(W)]
            wb["vb"] = [sbt(ps, "wvb%d" % i, [128, 128]) for i in range(W)]
            wb["kbg"] = [sbt(ps, "wkbg%d" % i, [128, 128]) for i in range(W)]
            bd16 = sbt(ps, "bd16", [128, 128]); msk = [sbt(ps, "msk%d" % i, [128, 128]) for i in range(3)]
            mk.dma("sp", bd16[:], I.dnmask[0], writes=[bd16])
            for i_ in range(3):
                mk.dma("sp", msk[i_][:], I.dnmask[1 + i_], writes=[msk[i_]])
            kdec = [sbt(ps, "kdec%d" % i, [128, 128]) for i in range(4)]
            vnew = [sbt(ps, "vnew%d" % i, [128, 128]) for i in range(4)]
            Sst = [[sbt(ps, "S%d_%d" % (d, i), [128, 128]) for i in range(2)] for d in range(2)]
            small = {}
            for nm in ["gc", "bc", "Gcol", "eGcol", "kbs", "kds", "eGl", "tmp"]:
                small[nm] = [sbt(ps, "sm%s%d" % (nm, d), [128, NCH]) for d in range(2)]
            Umat = [K["uf"], K["ub"]]; Mm = [K["mf"], K["mb"]]
            Mo = [sbt(ps, "mos%d" % i, [128, 128]) for i in range(2)]
            for i_, src_ in enumerate([K["mb"], K["mf"]]):
                mk.op("dve", lambda e, i_=i_, src_=src_: e.scalar_tensor_tensor(Mo[i_][:], ident[:], -1.0e6, src_[:], op0=ALU.mult, op1=ALU.add), reads=[ident, src_], writes=[Mo[i_]])
            offd = K["offd"]

            R = lambda ap: ap.bitcast(F32R)
            DN_STOP = int(os.environ.get("DN_STOP", "99"))
            PREP_STOP = int(os.environ.get("PREP_STOP", "99"))
            for h in heads:
                if DN_STOP <= 0:
                    break
                for idx, (acc, eng) in enumerate([(cq, "dve"), (ck, "dve"), (cv, "pool")]):
                    mk.dma("sp", raw[:], S.dn.t[idx * 16 + h], reads=[S.dn], writes=[raw])
                    t_ = idx * 16 + h
                    if eng == "dve":
                        mk.op(eng, lambda e, acc=acc, t_=t_: e.tensor_scalar(R(acc[:]), raw[:], cw[:, t_, 2:3], None, op0=ALU.mult),
                              reads=[raw, cw], writes=[acc])
                    else:
                        mk.op(eng, lambda e, acc=acc, t_=t_: e.tensor_tensor(R(acc[:]), raw[:], cw[:, t_, 2:3].to_broadcast([128, T]), op=ALU.mult),
                              reads=[raw, cw], writes=[acc])
                    for (s0, s1) in [(0, L), (L, T)]:
                        for jj in (0, 1, 3, 4):
                            sh = jj - 2
                            d0 = s0 + max(0, -sh); d1 = s1 - max(0, sh)
                            if eng == "dve":
                                mk.op(eng, lambda e, acc=acc, t_=t_, jj=jj, d0=d0, d1=d1, sh=sh: e.scalar_tensor_tensor(
                                    R(acc[:, d0:d1]), raw[:, d0 + sh:d1 + sh], cw[:, t_, jj:jj + 1], acc[:, d0:d1], op0=ALU.mult, op1=ALU.add),
                                    reads=[raw, cw, acc], writes=[acc])
                            else:
                                mk.op(eng, lambda e, t_=t_, jj=jj, d0=d0, d1=d1, sh=sh: e.tensor_tensor(
                                    rsb[:, d0:d1], raw[:, d0 + sh:d1 + sh], cw[:, t_, jj:jj + 1].to_broadcast([128, d1 - d0]), op=ALU.mult),
                                    reads=[raw, cw], writes=[rsb])
                                mk.op(eng, lambda e, acc=acc, d0=d0, d1=d1: e.tensor_tensor(
                                    R(acc[:, d0:d1]), acc[:, d0:d1], rsb[:, d0:d1], op=ALU.add),
                                    reads=[rsb, acc], writes=[acc])
                    mk.op("act", lambda e, acc=acc: e.activation(R(acc[:]), acc[:], AF.Silu), reads=[acc], writes=[acc])
                if DN_STOP <= 1:
                    break
                for acc, scl in [(cq, 128.0 ** -0.5), (ck, 1.0)]:
                    mk.op("act", lambda e, acc=acc: e.activation(sqb[:], acc[:], AF.Square), reads=[acc], writes=[sqb])
                    for (t0, w) in TOK5:
                        p = nwide()
                        mk.op("pe", lambda e, p=p, t0=t0, w=w: e.matmul(p[:, :w], ones[:], sqb[:, t0:t0 + w], start=True, stop=True),
                              reads=[ones, sqb], writes=[p])
                        mk.op("act", lambda e, p=p, t0=t0, w=w: e.activation(rsb[:, t0:t0 + w], p[:, :w], AF.Ln, bias=epsT[:, 0:1], scale=1.0),
                              reads=[p, epsT], writes=[rsb])
                    mk.op("act", lambda e: e.activation(rsb[:], rsb[:], AF.Exp, scale=-0.5), reads=[rsb], writes=[rsb])
                    mk.op("dve", lambda e, acc=acc, scl=scl: e.scalar_tensor_tensor(R(acc[:]), acc[:], scl, rsb[:], op0=ALU.mult, op1=ALU.mult),
                          reads=[acc, rsb], writes=[acc])
                if DN_STOP <= 2:
                    break
                for src, dst in [(ck, ktok), (cv, vtok)]:
                    for n0 in range(0, NCH, 4):
                        nn = min(4, NCH - n0)
                        p = nwide()
                        for q_ in range(nn):
                            n = n0 + q_
                            mk.op("pe", lambda e, p=p, q_=q_, n=n, src=src: e.transpose(p[:, q_ * 128:(q_ + 1) * 128], src[:, n * 128:(n + 1) * 128], ident[:]),
                                  reads=[src, ident], writes=[p])
                        mk.op("act", lambda e, p=p, n0=n0, nn=nn, dst=dst: e.copy(dst[:, n0:n0 + nn, :], p[:, :nn * 128].rearrange("p (a b) -> p a b", b=128)),
                              reads=[p], writes=[dst])
                if DN_STOP <= 3:
                    break
                mk.dma("sp", zt[:, :L], S.z.t[h], reads=[S.z], writes=[zt])
                for d in range(2):
                    ci = d * 16 + h
                    gc = small["gc"][d]; bc = small["bc"][d]
                    mk.op("dve", lambda e, gc=gc, ci=ci: e.tensor_copy(gc[:], gC[:, :, ci]), reads=[gC], writes=[gc])
                    mk.op("dve", lambda e, bc=bc, ci=ci: e.tensor_copy(bc[:], betaC[:, :, ci]), reads=[betaC], writes=[bc])
                    mk.dma("sp", GBt[d][:], S.grow.t[ci:ci + 1, :].partition_broadcast(128), reads=[S.grow], writes=[GBt[d]])
                    mk.dma("sp", BBt[d][:], S.brow.t[ci:ci + 1, :].partition_broadcast(128), reads=[S.brow], writes=[BBt[d]])
                    bk = nbank(); p1 = bk[0]; p2 = bk[1]
                    mk.op("pe", lambda e, p1=p1, d=d, gc=gc: e.matmul(p1[:, :NCH], Umat[d][:], gc[:], start=True, stop=True), reads=[Umat[d], gc], writes=[p1])
                    mk.op("pe", lambda e, p2=p2, gc=gc: e.matmul(p2[:, :NCH], ones[:], gc[:], start=True, stop=True), reads=[ones, gc], writes=[p2])
                    Gcol = small["Gcol"][d]; eGcol = small["eGcol"][d]; kbs = small["kbs"][d]; kds = small["kds"][d]; eGl = small["eGl"][d]; tmp = small["tmp"][d]
                    mk.op("dve", lambda e, p1=p1, Gcol=Gcol: e.tensor_copy(Gcol[:], p1[:, :NCH]), reads=[p1], writes=[Gcol])
                    mk.op("act", lambda e, p1=p1, eGcol=eGcol: e.activation(eGcol[:], p1[:, :NCH], AF.Exp), reads=[p1], writes=[eGcol])
                    mk.op("dve", lambda e, kbs=kbs, bc=bc, eGcol=eGcol: e.tensor_tensor(kbs[:], bc[:], eGcol[:], op=ALU.mult), reads=[bc, eGcol], writes=[kbs])
                    mk.op("dve", lambda e, tmp=tmp, p2=p2, Gcol=Gcol: e.tensor_tensor(tmp[:], p2[:, :NCH], Gcol[:], op=ALU.subtract), reads=[p2, Gcol], writes=[tmp])
                    mk.op("act", lambda e, kds=kds, tmp=tmp: e.activation(kds[:], tmp[:], AF.Exp), reads=[tmp], writes=[kds])
                    mk.op("act", lambda e, eGl=eGl, p2=p2: e.activation(eGl[:], p2[:, :NCH], AF.Exp), reads=[p2], writes=[eGl])
                    mk.op("dve", lambda e, d=d: e.tensor_scalar(R(Sst[d][0][:]), ident[:], 0.0, None, op0=ALU.mult), reads=[ident], writes=[Sst[d][0]])

                fo = [16, 17] + list(range(16)); bo = [17, 16] + list(range(15, -1, -1))
                seq = []
                for i_ in range(NCH):
                    seq.append((0, fo[i_])); seq.append((1, bo[i_]))
                spos = {0: 0, 1: 0}
                oinit = set()

                def prep(wave):
                    cds = [(wi, d, n, (wave_base + wi) % NS) for wi, (d, n) in enumerate(wave)]
                    P = {}
                    for wi, d, n, si in cds:
                        gc = small["gc"][d]; bc = small["bc"][d]; Gcol = small["Gcol"][d]
                        kc = ck[:, n * 128:(n + 1) * 128]; qc = cq[:, n * 128:(n + 1) * 128]
                        _b0, _b1, KKp, QKp = nbank()
                        MMS = "kq"
                        GBv = GBt[d][:, n * 128:(n + 1) * 128]; BBv = BBt[d][:, n * 128:(n + 1) * 128]
                        if "k" in MMS:
                          mk.op("pe", lambda e, KKp=KKp, kc=kc: e.matmul(KKp[:], R(kc), R(kc), start=True, stop=True), reads=[ck], writes=[KKp])
                        if "q" in MMS:
                          mk.op("pe", lambda e, QKp=QKp, kc=kc, qc=qc: e.matmul(QKp[:], R(kc), R(qc), start=True, stop=True), reads=[ck, cq], writes=[QKp])
                        if PREP_STOP <= 1:
                            continue
                        dm1 = wb["dm1"][wi]; dm2 = wb["dm2"][wi]; dec = wb["dec"][wi]; dec2 = wb["dec2"][wi]; eGb = wb["eGb"][wi]; BM = wb["BM"][wi]
                        mk.op("dve", lambda e, dm1=dm1, GBv=GBv, Gcol=Gcol, n=n, d=d: e.scalar_tensor_tensor(dm1[:], GBv, Gcol[:, n:n + 1], Mm[d][:], op0=ALU.subtract, op1=ALU.add), reads=[GBt[d], Gcol, Mm[d]], writes=[dm1])
                        mk.op("dve", lambda e, dm2=dm2, GBv=GBv, Gcol=Gcol, n=n, d=d: e.scalar_tensor_tensor(dm2[:], GBv, Gcol[:, n:n + 1], Mo[d][:], op0=ALU.subtract, op1=ALU.subtract), reads=[GBt[d], Gcol, Mo[d]], writes=[dm2])
                        mk.op("act", lambda e, dec=dec, dm1=dm1: e.activation(R(dec[:]), dm1[:], AF.Exp), reads=[dm1], writes=[dec])
                        mk.op("act", lambda e, dec2=dec2, dm2=dm2: e.activation(R(dec2[:]), dm2[:], AF.Exp, scale=-1.0), reads=[dm2], writes=[dec2])
                        mk.op("act", lambda e, eGb=eGb, GBv=GBv: e.activation(R(eGb[:]), GBv, AF.Exp), reads=[GBt[d]], writes=[eGb])
                        mk.op("pool", lambda e, BM=BM, BBv=BBv: e.tensor_tensor(BM[:], BBv, offd[:], op=ALU.mult), reads=[BBt[d], offd], writes=[BM])
                        mk.op("dve", lambda e, si=si, QKp=QKp, dec=dec: e.tensor_tensor(R(aV[si][:]), QKp[:], dec[:], op=ALU.mult), reads=[QKp, dec], writes=[aV[si]])
                        mk.op("pool", lambda e, BM=BM, dec=dec: e.tensor_tensor(BM[:], BM[:], dec[:], op=ALU.mult), reads=[BM, dec], writes=[BM])
                        A = wb["A0"][wi]; B = wb["B0"][wi]; Pm = wb["P0"][wi]; Af = wb["Af"][wi]
                        mk.op("dve", lambda e, KKp=KKp, BM=BM: e.tensor_tensor(BM[:], KKp[:], BM[:], op=ALU.mult), reads=[KKp, BM], writes=[BM])
                        mk.op("dve", lambda e, Af=Af, KKp=KKp, bc=bc, n=n, dec2=dec2: e.scalar_tensor_tensor(Af[:], KKp[:], bc[:, n:n + 1], dec2[:], op0=ALU.mult, op1=ALU.mult), reads=[KKp, bc, dec2], writes=[Af])
                        mk.op("pool", lambda e, si=si, qc=qc, eGb=eGb: e.tensor_tensor(R(qV[si][:]), qc, eGb[:], op=ALU.mult), reads=[cq, eGb], writes=[qV[si]])
                        mk.op("dve", lambda e, B=B, BM=BM: e.tensor_tensor(R(B[:]), BM[:], bd16[:], op=ALU.mult), reads=[BM, bd16], writes=[B])
                        mk.op("dve", lambda e, A=A, Af=Af: e.tensor_tensor(R(A[:]), Af[:], bd16[:], op=ALU.mult), reads=[Af, bd16], writes=[A])
                        mk.op("pool", lambda e, Pm=Pm, B=B: e.tensor_tensor(R(Pm[:]), ident[:], B[:], op=ALU.subtract), reads=[ident, B], writes=[Pm])
                        P[wi] = [A, B, Pm]
                    if PREP_STOP <= 2:
                        return
                    NLEV = 3
                    for lev in range(1, NLEV + 1):
                        nxt = "1" if lev % 2 == 1 else "0"
                        pend = {}
                        for wi, d, n, si in cds:
                            A, B, Pm = P[wi]
                            bk = nbank()
                            Ap = bk[0]
                            mk.op("pe", lambda e, Ap=Ap, A=A, B=B: e.matmul(Ap[:], R(B[:]), R(A[:]), start=True, stop=True), reads=[A, B], writes=[Ap])
                            Bp = None
                            if lev < NLEV:
                                Bp = bk[1]
                                mk.op("pe", lambda e, Bp=Bp, A=A, B=B: e.matmul(Bp[:], R(A[:]), R(B[:]), start=True, stop=True), reads=[A, B], writes=[Bp])
                            pend[wi] = (Ap, Bp, bk)
                        for wi, d, n, si in cds:
                            Ap, Bp, bk = pend[wi]
                            ApI = wb["ApI"][wi]
                            mk.op("dve", lambda e, ApI=ApI, Ap=Ap: e.tensor_tensor(R(ApI[:]), Ap[:], ident[:], op=ALU.add), reads=[Ap, ident], writes=[ApI])
                            if lev < NLEV:
                                An = wb["A" + nxt][wi]; Bn = wb["B" + nxt][wi]
                                mk.op("act", lambda e, An=An, Ap=Ap: e.copy(R(An[:]), Ap[:]), reads=[Ap], writes=[An])
                                if wi % 2 == 0:
                                    mk.op("act", lambda e, Bn=Bn, Bp=Bp: e.copy(R(Bn[:]), Bp[:]), reads=[Bp], writes=[Bn])
                                else:
                                    mk.op("dve", lambda e, Bn=Bn, Bp=Bp: e.tensor_copy(R(Bn[:]), Bp[:]), reads=[Bp], writes=[Bn])
                                P[wi][0] = An; P[wi][1] = Bn
                        pend2 = {}
                        for wi, d, n, si in cds:
                            Pm = P[wi][2]; ApI = wb["ApI"][wi]
                            Pp = pend[wi][2][2]
                            mk.op("pe", lambda e, Pp=Pp, ApI=ApI, Pm=Pm: e.matmul(Pp[:], R(ApI[:]), R(Pm[:]), start=True, stop=True), reads=[ApI, Pm], writes=[Pp])
                            pend2[wi] = Pp
                        for wi, d, n, si in cds:
                            Pn = wb["P" + nxt][wi]
                            Pp = pend2[wi]
                            if wi % 2 == 0:
                                mk.op("dve", lambda e, Pn=Pn, Pp=Pp: e.tensor_copy(R(Pn[:]), Pp[:]), reads=[Pp], writes=[Pn])
                            else:
                                mk.op("act", lambda e, Pn=Pn, Pp=Pp: e.copy(R(Pn[:]), Pp[:]), reads=[Pp], writes=[Pn])
                            P[wi][2] = Pn
                    for wi, d, n, si in cds:
                        Dr = P[wi][2]; Dl = wb["dec2"][wi]
                        bk = nbank()
                        mk.op("pe", lambda e, bk=bk, Dr=Dr: e.transpose(bk[0][:], Dr[:], ident[:]), reads=[Dr, ident], writes=[bk[0]])
                        mk.op("act", lambda e, bk=bk, Dl=Dl: e.copy(R(Dl[:]), bk[0][:]), reads=[bk[0]], writes=[Dl])
                    for bl in range(3):
                        Ms = msk[bl]
                        pend = {}
                        for wi, d, n, si in cds:
                            Dr = P[wi][2]; AM = wb["eGb"][wi]; Af = wb["Af"][wi]
                            mk.op("pool", lambda e, AM=AM, Af=Af, Ms=Ms: e.tensor_tensor(R(AM[:]), Af[:], Ms[:], op=ALU.mult), reads=[Af, Ms], writes=[AM])
                            bk = nbank()
                            mk.op("pe", lambda e, bk=bk, AM=AM, Dr=Dr: e.matmul(bk[0][:], R(AM[:]), R(Dr[:]), start=True, stop=True), reads=[AM, Dr], writes=[bk[0]])
                            pend[wi] = bk
                        for wi, d, n, si in cds:
                            bk = pend[wi]; Ysb = wb["dec"][wi]
                            if wi % 2 == 0:
                                mk.op("act", lambda e, bk=bk, Ysb=Ysb: e.copy(R(Ysb[:]), bk[0][:]), reads=[bk[0]], writes=[Ysb])
                            else:
                                mk.op("dve", lambda e, bk=bk, Ysb=Ysb: e.tensor_copy(R(Ysb[:]), bk[0][:]), reads=[bk[0]], writes=[Ysb])
                        for wi, d, n, si in cds:
                            bk = pend[wi]; Ysb = wb["dec"][wi]; Dl = wb["dec2"][wi]
                            mk.op("pe", lambda e, bk=bk, Dl=Dl, Ysb=Ysb: e.matmul(bk[1][:], R(Dl[:]), R(Ysb[:]), start=True, stop=True), reads=[Dl, Ysb], writes=[bk[1]])
                        for wi, d, n, si in cds:
                            bk = pend[wi]; Dr = P[wi][2]
                            Dn = wb["P1"][wi] if Dr is wb["P0"][wi] else wb["P0"][wi]
                            mk.op("dve", lambda e, bk=bk, Dr=Dr, Dn=Dn: e.tensor_tensor(R(Dn[:]), Dr[:], bk[1][:], op=ALU.subtract), reads=[Dr, bk[1]], writes=[Dn])
                            P[wi][2] = Dn
                        if bl < 2:
                            for wi, d, n, si in cds:
                                bk = pend[wi]; Dn = P[wi][2]; Dl = wb["dec2"][wi]
                                mk.op("pe", lambda e, bk=bk, Dn=Dn: e.transpose(bk[2][:], Dn[:], ident[:]), reads=[Dn, ident], writes=[bk[2]])
                                mk.op("act", lambda e, bk=bk, Dl=Dl: e.copy(R(Dl[:]), bk[2][:]), reads=[bk[2]], writes=[Dl])
                    if PREP_STOP <= 3:
                        return
                    for wi, d, n, si in cds:
                        TT = P[wi][2]
                        bc = small["bc"][d]; kbs = small["kbs"][d]
                        vb = wb["vb"][wi]; kbg = wb["kbg"][wi]
                        mk.op("dve", lambda e, vb=vb, n=n, bc=bc: e.tensor_scalar(R(vb[:]), vtok[:, n, :], bc[:, n:n + 1], None, op0=ALU.mult), reads=[vtok, bc], writes=[vb])
                        mk.op("dve", lambda e, kbg=kbg, n=n, kbs=kbs: e.tensor_scalar(R(kbg[:]), ktok[:, n, :], kbs[:, n:n + 1], None, op0=ALU.mult), reads=[ktok, kbs], writes=[kbg])
                        bk = nbank(); up = bk[0]; wp = bk[1]
                        mk.op("pe", lambda e, up=up, TT=TT, vb=vb: e.matmul(up[:], R(TT[:]), R(vb[:]), start=True, stop=True), reads=[TT, vb], writes=[up])
                        mk.op("pe", lambda e, wp=wp, TT=TT, kbg=kbg: e.matmul(wp[:], R(kbg[:]), R(TT[:]), start=True, stop=True), reads=[TT, kbg], writes=[wp])
                        mk.op("act", lambda e, si=si, up=up: e.copy(uV[si][:], up[:]), reads=[up], writes=[uV[si]])
                        mk.op("dve", lambda e, si=si, wp=wp: e.tensor_scalar(R(wV[si][:]), wp[:], -1.0, None, op0=ALU.mult), reads=[wp], writes=[wV[si]])

                def scan(wave):
                    for wi, (d, n) in enumerate(wave):
                        si = (wave_base + wi) % NS
                        kds = small["kds"][d]; eGl = small["eGl"][d]
                        Sc = Sst[d][spos[d] % 2]; Sn = Sst[d][(spos[d] + 1) % 2]; spos[d] += 1
                        kd = kdec[si % 4]; vn = vnew[si % 4]
                        mk.op("act", lambda e, kd=kd, n=n, kds=kds: e.activation(R(kd[:]), ktok[:, n, :], AF.Copy, scale=kds[:, n:n + 1]), reads=[ktok, kds], writes=[kd])
                        bk = nbank(); vp = bk[0]; sp_ = bk[1]; bk2 = nbank()
                        mk.op("pe", lambda e, vp=vp, si=si, Sc=Sc: e.matmul(vp[:], R(wV[si][:]), R(Sc[:]), start=True, stop=True), reads=[wV[si], Sc], writes=[vp])
                        mk.op("dve", lambda e, vn=vn, vp=vp, si=si: e.tensor_tensor(R(vn[:]), vp[:], uV[si][:], op=ALU.add), reads=[vp, uV[si]], writes=[vn])
                        if n < 16:
                            op_ = bk2[0]
                            mk.op("pe", lambda e, op_=op_, Sc=Sc, si=si: e.matmul(op_[:], R(Sc[:]), R(qV[si][:]), start=True, stop=False), reads=[Sc, qV[si]], writes=[op_])
                            mk.op("pe", lambda e, op_=op_, vn=vn, si=si: e.matmul(op_[:], R(vn[:]), R(aV[si][:]), start=False, stop=True), reads=[vn, aV[si]], writes=[op_])
                            if n not in oinit:
                                oinit.add(n)
                                mk.op("act", lambda e, op_=op_, n=n: e.copy(oacc[:, n * 128:(n + 1) * 128], op_[:]), reads=[op_], writes=[oaccB[n]])
                            else:
                                mk.op("dve", lambda e, op_=op_, n=n: e.tensor_tensor(oacc[:, n * 128:(n + 1) * 128], op_[:], oacc[:, n * 128:(n + 1) * 128], op=ALU.add), reads=[op_, oaccB[n]], writes=[oaccB[n]])
                        mk.op("pe", lambda e, sp_=sp_, kd=kd, vn=vn: e.matmul(sp_[:], R(kd[:]), R(vn[:]), start=True, stop=True), reads=[kd, vn], writes=[sp_])
                        mk.op("dve", lambda e, Sn=Sn, Sc=Sc, eGl=eGl, n=n, sp_=sp_: e.scalar_tensor_tensor(R(Sn[:]), Sc[:], eGl[:, n:n + 1], sp_[:], op0=ALU.mult, op1=ALU.add), reads=[Sc, eGl, sp_], writes=[Sn])

                waves = [seq[i_:i_ + W] for i_ in range(0, len(seq), W)]
                if DN_STOP <= 4:
                    break
                for wv_i, wave in enumerate(waves):
                    wave_base = wv_i * W
                    prep(wave)
                    if DN_STOP <= 5:
                        break
                    scan(wave)
                    if DN_STOP <= 6:
                        break
                if DN_STOP <= 6:
                    break
                mk.op("act", lambda e: e.activation(sqb[:, :L], oacc[:], AF.Square), reads=oaccB, writes=[sqb])
                for (t0, w) in TOK5[:4]:
                    p = nwide()
                    mk.op("pe", lambda e, p=p, t0=t0, w=w: e.matmul(p[:, :w], ones[:], sqb[:, t0:t0 + w], start=True, stop=True), reads=[ones, sqb], writes=[p])
                    mk.op("act", lambda e, p=p, t0=t0, w=w: e.activation(rsb[:, t0:t0 + w], p[:, :w], AF.Ln, bias=epsT[:, 0:1], scale=1.0 / 128.0), reads=[p, epsT], writes=[rsb])
                mk.op("act", lambda e: e.activation(rsb[:, :L], rsb[:, :L], AF.Exp, scale=-0.5), reads=[rsb], writes=[rsb])
                mk.op("dve", lambda e: e.tensor_tensor(sqb[:, :L], oacc[:], rsb[:, :L], op=ALU.mult), reads=oaccB + [rsb], writes=[sqb])
                mk.op("dve", lambda e: e.scalar_tensor_tensor(rsb[:, :1024].bitcast(BF16), sqb[:, :L], hw[:, 0:1], zt[:, :L], op0=ALU.mult, op1=ALU.mult), reads=[sqb, hw, zt], writes=[rsb])
                mk.dma("sp", S.odn.t[h], rsb[:, :1024].bitcast(BF16), reads=[rsb], writes=[S.odn])
        mk.barrier()

    g.phase_dn = phase_dn

    def phase_na(heads=range(16)):
        with ExitStack() as ps:
            pb = [pst(ps, "na%d" % i, [128, 512]) for i in range(8)]
            cnt = {"s": 0, "g": 0, "bb": 0}

            def sbank():
                cnt["s"] += 1
                return pb[cnt["s"] % 4]
            ropec = sbt(ps, "ropec", [128, L]); ropes = sbt(ps, "ropes", [128, L])
            mk.dma("sp", ropec[:], I.ropec, writes=[ropec]); mk.dma("sp", ropes[:], I.ropes, writes=[ropes])
            qraws = [sbt(ps, "qraw%d" % i, [128, L]) for i in range(2)]; kraws = [sbt(ps, "kraw%d" % i, [128, T]) for i in range(2)]
            sq = sbt(ps, "nsq", [128, T]); rs = sbt(ps, "nrs", [128, T])
            t1 = sbt(ps, "t1", [128, T]); qbf = sbt(ps, "qbf", [128, L], BF16); kbf = sbt(ps, "kbf", [128, T], BF16)
            vts = [sbt(ps, "vt%d" % i, [128, NCH, 128], BF16) for i in range(2)]
            bias = [sbt(ps, "bias%d" % i, [128, 512]) for i in range(8)]
            et = [sbt(ps, "et%d" % i, [128, 512]) for i in range(2)]
            pT = [sbt(ps, "pT%d" % i, [128, 512], BF16) for i in range(3)]
            t2 = [sbt(ps, "t2_%d" % i, [128, 512]) for i in range(2)]
            ost = sbt(ps, "nost", [128, L], BF16); rden = sbt(ps, "rden", [128, 512])
            rot = K["rot"]
            for hi_, h in enumerate(heads):
                qraw = qraws[hi_ % 2]; kraw = kraws[hi_ % 2]; vt = vts[hi_ % 2]
                mk.dma("sp", qraw[:], S.nq.t[h], reads=[S.nq], writes=[qraw])
                mk.dma("sp", kraw[:], S.nk.t[h], reads=[S.nk], writes=[kraw])
                mk.dma("sp", vt[:], S.nv.t.rearrange("(n p) c -> p n c", p=128)[:, :, h * 128:(h + 1) * 128], reads=[S.nv], writes=[vt])
                for raw, Wd, wc, scl, obf_ in [(qraw, L, 1, 128.0 ** -0.5, qbf), (kraw, T, 2, 1.0, kbf)]:
                    mk.op("act", lambda e, raw=raw, Wd=Wd: e.activation(sq[:, :Wd], raw[:, :Wd], AF.Square), reads=[raw], writes=[sq])
                    for (t0, w) in TOK5:
                        if t0 >= Wd:
                            continue
                        p = sbank()
                        mk.op("pe", lambda e, p=p, t0=t0, w=w: e.matmul(p[:, :w], ones[:], sq[:, t0:t0 + w], start=True, stop=True), reads=[ones, sq], writes=[p])
                        mk.op("act", lambda e, p=p, t0=t0, w=w: e.activation(rs[:, t0:t0 + w], p[:, :w], AF.Ln, bias=epsT[:, 0:1], scale=1.0 / 128.0), reads=[p, epsT], writes=[rs])
                    mk.op("act", lambda e, Wd=Wd: e.activation(rs[:, :Wd], rs[:, :Wd], AF.Exp, scale=-0.5), reads=[rs], writes=[rs])
                    mk.op("dve", lambda e, raw=raw, Wd=Wd: e.tensor_tensor(t1[:, :Wd], raw[:, :Wd], rs[:, :Wd], op=ALU.mult), reads=[raw, rs], writes=[t1])
                    mk.op("dve", lambda e, Wd=Wd, wc=wc, scl=scl: e.tensor_scalar(t1[:, :Wd], t1[:, :Wd], hw[:, wc:wc + 1], scl, op0=ALU.mult, op1=ALU.mult), reads=[t1, hw], writes=[t1])
                    for ti in range(4):
                        t0 = ti * 512
                        p = sbank(); tt = t2[ti % 2]
                        mk.op("pe", lambda e, p=p, t0=t0: e.matmul(p[:, :512], rot[:], t1[:, t0:t0 + 512], start=True, stop=True), reads=[rot, t1], writes=[p])
                        mk.op("dve", lambda e, p=p, t0=t0, tt=tt: e.tensor_tensor(tt[:], p[:, :512], ropes[:, t0:t0 + 512], op=ALU.mult), reads=[p, ropes], writes=[tt])
                        mk.op("pool", lambda e, t0=t0: e.tensor_tensor(sq[:, t0:t0 + 512], t1[:, t0:t0 + 512], ropec[:, t0:t0 + 512], op=ALU.mult), reads=[t1, ropec], writes=[sq])
                        mk.op("pool", lambda e, t0=t0, tt=tt, obf_=obf_: e.tensor_tensor(obf_[:, t0:t0 + 512], sq[:, t0:t0 + 512], tt[:], op=ALU.add), reads=[sq, tt], writes=[obf_])
                    if Wd > L:
                        mk.op("act", lambda e: e.copy(kbf[:, L:T], t1[:, L:T]), reads=[t1], writes=[kbf])
                bi = 0
                for m, (lo, nt) in enumerate(NA_BLOCKS):
                    qs = qbf[:, m * 512:(m + 1) * 512]
                    obank = pb[4 + m % 2]; dbank = pb[6 + m % 2]
                    tiles = [("l", lo + 2 * j) for j in range(nt)] + [("c", 0), ("c", 1)]
                    for ti, (kind, kr0) in enumerate(tiles):
                        sb_ = sbank()
                        if kind == "l":
                            keys = kbf[:, kr0 * 64:kr0 * 64 + 128]; V = vt[:, kr0 // 2, :]
                        else:
                            keys = kbf[:, L + kr0 * 128:L + (kr0 + 1) * 128]; V = vt[:, 16 + kr0, :]
                        mk.op("pe", lambda e, sb_=sb_, keys=keys, qs=qs: e.matmul(sb_[:, :512], keys, qs, start=True, stop=True), reads=[kbf, qbf], writes=[sb_])
                        cnt["g"] += 1
                        pt_ = pT[cnt["g"] % 3]
                        if kind == "l":
                            bt = bias[cnt["bb"] % 8]; cnt["bb"] += 1; e_ = et[cnt["g"] % 2]
                            mk.dma("sp", bt[:], I.nab[h, bi], writes=[bt])
                            bi += 1
                            mk.op("dve", lambda e, e_=e_, sb_=sb_, bt=bt: e.tensor_tensor(e_[:], sb_[:, :512], bt[:], op=ALU.add), reads=[sb_, bt], writes=[e_])
                            mk.op("act", lambda e, pt_=pt_, e_=e_: e.activation(pt_[:], e_[:], AF.Exp), reads=[e_], writes=[pt_])
                        else:
                            mk.op("act", lambda e, pt_=pt_, sb_=sb_: e.activation(pt_[:], sb_[:, :512], AF.Exp), reads=[sb_], writes=[pt_])
                        first = (ti == 0); last = (ti == len(tiles) - 1)
                        mk.op("pe", lambda e, obank=obank, V=V, pt_=pt_, first=first, last=last: e.matmul(obank[:, :512], V, pt_[:], start=first, stop=last), reads=[vt, pt_], writes=[obank])
                        mk.op("pe", lambda e, dbank=dbank, pt_=pt_, first=first, last=last: e.matmul(dbank[:, :512], onesb[:], pt_[:], start=first, stop=last), reads=[onesb, pt_], writes=[dbank])
                    mk.op("act", lambda e, dbank=dbank: e.activation(rden[:], dbank[:, :512], AF.Ln), reads=[dbank], writes=[rden])
                    mk.op("act", lambda e: e.activation(rden[:], rden[:], AF.Exp, scale=-1.0), reads=[rden], writes=[rden])
                    mk.op("dve", lambda e, obank=obank, m=m: e.tensor_tensor(ost[:, m * 512:(m + 1) * 512], obank[:, :512], rden[:], op=ALU.mult), reads=[obank, rden], writes=[ost])
                mk.dma("pool", S.ona.t[h], ost[:], reads=[ost], writes=[S.ona])
        mk.barrier()

    g.phase_na = phase_na
    a2 = X["a2"]; norm_mod = X["norm_mod"]

    def make_wloader(ps, nbuf=4):
        stg = [sbt(ps, "stg%d" % i, [128, 4096]) for i in range(2)]
        wbf = [sbt(ps, "wld%d" % i, [128, 4096], BF16) for i in range(nbuf)]
        st = {"i": 0}

        def load(view, a, b):
            i = st["i"]; st["i"] += 1
            s_ = stg[i % 2]; w_ = wbf[i % nbuf]
            mk.dma("sp", s_[:].rearrange("p (a b) -> p a b", b=b), view, writes=[s_])
            mk.op("act", lambda e: e.copy(w_[:, 0:2048], s_[:, 0:2048]), reads=[s_], writes=[w_])
            mk.op("dve", lambda e: e.tensor_copy(w_[:, 2048:4096], s_[:, 2048:4096]), reads=[s_], writes=[w_])
            return w_
        return load

    def pipelined(n, loadfn, computefn):
        cur = loadfn(0)
        for i in range(n):
            nxt = loadfn(i + 1) if i + 1 < n else None
            computefn(i, cur)
            cur = nxt

    def phase_merge():
        with ExitStack() as ps:
            pb = [pst(ps, "mg%d" % i, [128, 512]) for i in range(8)]
            cnt = {"b": 0}

            def bank():
                cnt["b"] += 1
                return pb[cnt["b"] % 8]
            load = make_wloader(ps, nbuf=6)
            odn = sbt(ps, "odn", [128, 16, 1024], BF16); ona = sbt(ps, "ona", [128, 16, 1024], BF16)
            sa = [sbt(ps, "sa%d" % i, [128, 1024]) for i in range(2)]; sb_ = [sbt(ps, "sbb%d" % i, [128, 1024]) for i in range(2)]
            ta = [sbt(ps, "ta%d" % i, [128, 512]) for i in range(2)]; tb = [sbt(ps, "tb%d" % i, [128, 512]) for i in range(2)]
            yst = [sbt(ps, "yst%d" % i, [128, 1024], BF16) for i in range(2)]
            wav = I.w_a.rearrange("(k p) n -> p k n", p=128); wbv = I.w_b.rearrange("(k p) n -> p k n", p=128)
            for half in range(2):
                th0 = half * 1024
                mk.dma("sp", odn[:], S.odn.t.rearrange("h p t -> p h t")[:, :, th0:th0 + 1024], reads=[S.odn], writes=[odn])
                mk.dma("sp", ona[:], S.ona.t.rearrange("h p t -> p h t")[:, :, th0:th0 + 1024], reads=[S.ona], writes=[ona])

                def ld(cb):
                    return (load(wav[:, :, cb * 256:(cb + 1) * 256], 16, 256), load(wbv[:, :, cb * 256:(cb + 1) * 256], 16, 256))

                def comp(cb, ws):
                    wa_, wb_ = ws
                    for ct in range(2):
                        dt = cb * 2 + ct
                        sA = sa[dt % 2]; sB = sb_[dt % 2]; ys = yst[dt % 2]
                        mk.dma("sp", sA[:], S.ga.t[dt][:, th0:th0 + 1024], reads=[S.ga], writes=[sA])
                        mk.dma("sp", sB[:], S.gb.t[dt][:, th0:th0 + 1024], reads=[S.gb], writes=[sB])
                        for tq in range(2):
                            t0 = tq * 512
                            pA = bank(); pB = bank()
                            for k in range(16):
                                mk.op("pe", lambda e, pA=pA, k=k, ct=ct, t0=t0: e.matmul(pA[:, :512], wa_[:, k * 256 + ct * 128:k * 256 + (ct + 1) * 128], odn[:, k, t0:t0 + 512], start=(k == 0), stop=(k == 15)), reads=[wa_, odn], writes=[pA])
                            for k in range(16):
                                mk.op("pe", lambda e, pB=pB, k=k, ct=ct, t0=t0: e.matmul(pB[:, :512], wb_[:, k * 256 + ct * 128:k * 256 + (ct + 1) * 128], ona[:, k, t0:t0 + 512], start=(k == 0), stop=(k == 15)), reads=[wb_, ona], writes=[pB])
                            a_ = ta[tq]; b_ = tb[tq]
                            mk.op("dve", lambda e, a_=a_, pA=pA, sA=sA, t0=t0: e.tensor_tensor(a_[:], pA[:, :512], sA[:, t0:t0 + 512], op=ALU.mult), reads=[pA, sA], writes=[a_])
                            mk.op("dve", lambda e, b_=b_, pB=pB, sB=sB, t0=t0: e.tensor_tensor(b_[:], pB[:, :512], sB[:, t0:t0 + 512], op=ALU.mult), reads=[pB, sB], writes=[b_])
                            mk.op("pool", lambda e, a_=a_, b_=b_, ys=ys, t0=t0: e.tensor_tensor(ys[:, t0:t0 + 512], a_[:], b_[:], op=ALU.add), reads=[a_, b_], writes=[ys])
                        mk.dma("sp", S.y.t[dt][:, th0:th0 + 1024], ys[:], reads=[ys], writes=[S.y])
                pipelined(8, ld, comp)
            mk.barrier()
            yT = odn
            xt = [sbt(ps, "mxt%d" % i, [128, 1024]) for i in range(2)]
            xst = [sbt(ps, "mxs%d" % i, [128, 1024]) for i in range(2)]
            wov = I.w_out.rearrange("(k p) n -> p k n", p=128)
            for half in range(2):
                th0 = half * 1024
                mk.dma("sp", yT[:], S.y.t.rearrange("h p t -> p h t")[:, :, th0:th0 + 1024], reads=[S.y], writes=[yT])

                def ld2(cb):
                    return load(wov[:, :, cb * 256:(cb + 1) * 256], 16, 256)

                def comp2(cb, wo_):
                    for ct in range(2):
                        dt = cb * 2 + ct
                        x_ = xt[dt % 2]; xs = xst[dt % 2]
                        mk.dma("sp", x_[:], I.xT[dt][:, th0:th0 + 1024], writes=[x_])
                        for tq in range(2):
                            t0 = tq * 512
                            p = bank()
                            for k in range(16):
                                mk.op("pe", lambda e, p=p, k=k, ct=ct, t0=t0: e.matmul(p[:, :512], wo_[:, k * 256 + ct * 128:k * 256 + (ct + 1) * 128], yT[:, k, t0:t0 + 512], start=(k == 0), stop=(k == 15)), reads=[wo_, yT], writes=[p])
                            mk.op("dve", lambda e, p=p, xs=xs, x_=x_, dt=dt, t0=t0: e.scalar_tensor_tensor(xs[:, t0:t0 + 512], p[:, :512], modsb[:, 32 + dt, 0:1], x_[:, t0:t0 + 512], op0=ALU.mult, op1=ALU.add), reads=[p, modsb, x_], writes=[xs])
                        mk.dma("sp", S.x1.t[dt][:, th0:th0 + 1024], xs[:], reads=[xs], writes=[S.x1])
                pipelined(8, ld2, comp2)
        mk.barrier()

    g.phase_merge = phase_merge

    def phase_moe():
        with ExitStack() as ps:
            pb = [pst(ps, "me%d" % i, [128, 512]) for i in range(6)]
            pbt = [pst(ps, "met%d" % i, [128, 1024], BF16) for i in range(2)]
            cnt = {"b": 0, "t": 0}

            def bank():
                cnt["b"] += 1
                return pb[cnt["b"] % 6]
            h2tok = sbt(ps, "h2tok", [128, 16, D], BF16)
            logits = sbt(ps, "logits", [128, 16, 16])
            wr = sbt(ps, "wr", [128, 16, 16])
            iota = sbt(ps, "iota", [128, 258])
            mk.dma("sp", wr[:], I.w_r.rearrange("(k p) e -> p k e", p=128), writes=[wr])
            mk.dma("sp", iota[:], I.iota, writes=[iota])
            rkmT = sbt(ps, "rkmT", [16, L]); gwT = sbt(ps, "gwT", [16, L])
            rkm = sbt(ps, "rkm", [128, 16, 16])
            with ExitStack() as p1:
                affT = sbt(p1, "affT", [16, L]); work = sbt(p1, "work", [16, L])
                xts = [sbt(p1, "mx%d" % i, [128, 16, 512]) for i in range(1)]
                sq = sbt(p1, "msq", [128, 16, 512]); rs = sbt(p1, "mrs", [128, 512])
                tmp2 = [sbt(p1, "mtmp%d" % i, [128, 512]) for i in range(2)]
                h2f = sq; h2b = sbt(p1, "h2b", [128, 16, 512], BF16)
                xv = S.x1.t.rearrange("k p t -> p k t")
                for tt in range(4):
                    t0 = tt * 512
                    xt = xts[0]
                    mk.dma("sp", xt[:], xv[:, :, t0:t0 + 512], reads=[S.x1], writes=[xt])
                    norm_mod(bank(), xt, sq, rs, tmp2, 512, a2, (lambda k: modsb[:, 48 + k, 0:1]), lambda k: (h2f[:, k, :], h2f))
                    for tc in range(4):
                        p = bank()
                        for k in range(16):
                            mk.op("pe", lambda e, p=p, k=k, tc=tc: e.matmul(p[:, :16], h2f[:, k, tc * 128:(tc + 1) * 128], wr[:, k, :], start=(k == 0), stop=(k == 15)), reads=[h2f, wr], writes=[p])
                        mk.op("dve", lambda e, p=p, tt=tt, tc=tc: e.tensor_copy(logits[:, tt * 4 + tc, :], p[:, :16]), reads=[p], writes=[logits])
                    mk.op("act", lambda e: e.copy(h2b[:], h2f[:]), reads=[h2f], writes=[h2b])
                    for tc in range(4):
                        for kg in range(2):
                            cnt["t"] += 1
                            pt = pbt[cnt["t"] % 2]
                            for kk in range(8):
                                k = kg * 8 + kk
                                mk.op("pe", lambda e, pt=pt, kk=kk, k=k, tc=tc: e.transpose(pt[:, kk * 128:(kk + 1) * 128], h2b[:, k, tc * 128:(tc + 1) * 128], identb[:]), reads=[h2b, identb], writes=[pt])
                            eng = "dve" if (tc + kg) % 2 == 0 else "act"
                            dst = h2tok[:, tt * 4 + tc, kg * 1024:(kg + 1) * 1024]
                            if eng == "dve":
                                mk.op("dve", lambda e, pt=pt, dst=dst: e.tensor_copy(dst, pt[:]), reads=[pt], writes=[h2tok])
                            else:
                                mk.op("act", lambda e, pt=pt, dst=dst: e.copy(dst, pt[:]), reads=[pt], writes=[h2tok])
                mx = sbt(p1, "mx_", [128, 16]); sm = sbt(p1, "sm_", [128, 16])
                for tc in range(16):
                    mk.op("dve", lambda e, tc=tc: e.reduce_max(mx[:, tc:tc + 1], logits[:, tc, :], axis=AX.X), reads=[logits], writes=[mx])
                mk.op("dve", lambda e: e.tensor_scalar(mx[:], mx[:], -1.0, None, op0=ALU.mult), reads=[mx], writes=[mx])
                for tc in range(16):
                    mk.op("act", lambda e, tc=tc: e.activation(logits[:, tc, :], logits[:, tc, :], AF.Exp, bias=mx[:, tc:tc + 1], scale=1.0, accum_out=sm[:, tc:tc + 1]), reads=[logits, mx], writes=[logits, sm])
                mk.op("dve", lambda e: e.reciprocal(sm[:], sm[:]), reads=[sm], writes=[sm])
                for tc in range(16):
                    mk.op("dve", lambda e, tc=tc: e.tensor_scalar(logits[:, tc, :], logits[:, tc, :], sm[:, tc:tc + 1], None, op0=ALU.mult), reads=[logits, sm], writes=[logits])
                for g4 in range(4):
                    p = bank()
                    for q_ in range(4):
                        tc = g4 * 4 + q_
                        mk.op("pe", lambda e, p=p, q_=q_, tc=tc: e.transpose(p[0:16, q_ * 128:(q_ + 1) * 128], logits[:, tc, :], ident[:]), reads=[logits, ident], writes=[p])
                    mk.op("dve", lambda e, p=p, g4=g4: e.tensor_copy(affT[:, g4 * 512:(g4 + 1) * 512], p[0:16, :512]), reads=[p], writes=[affT])
                mk.op("dve", lambda e: e.tensor_copy(work[:], affT[:]), reads=[affT], writes=[work])
                m8 = sbt(p1, "m8", [16, 8])
                for r_ in range(32):
                    mk.op("dve", lambda e: e.max(m8[:], work[:]), reads=[work], writes=[m8])
                    if r_ < 31:
                        mk.op("dve", lambda e: e.match_replace(work[:], m8[:], work[:], -1.0), reads=[work, m8], writes=[work])
                mk.op("dve", lambda e: e.tensor_scalar(work[:], affT[:], m8[:, 7:8], None, op0=ALU.is_ge), reads=[affT, m8], writes=[work])
                mk.op("dve", lambda e: e.tensor_tensor(gwT[:], affT[:], work[:], op=ALU.mult), reads=[affT, work], writes=[gwT])
                onesr = sbt(p1, "onesr", [16, L])
                mk.op("pool", lambda e: e.memset(onesr[:], 1.0), writes=[onesr])
                mk.op("dve", lambda e: e.tensor_tensor_scan(rkmT[:], onesr[:], work[:], 0.0, op0=ALU.mult, op1=ALU.add), reads=[onesr, work], writes=[rkmT])
                mk.op("dve", lambda e: e.tensor_tensor(rkmT[:], rkmT[:], work[:], op=ALU.mult), reads=[rkmT, work], writes=[rkmT])
                mk.op("dve", lambda e: e.tensor_scalar(rkmT[:], rkmT[:], -1.0, None, op0=ALU.add), reads=[rkmT], writes=[rkmT])
                for g4 in range(4):
                    p = bank()
                    for q_ in range(4):
                        tc = g4 * 4 + q_
                        mk.op("pe", lambda e, p=p, q_=q_, tc=tc: e.transpose(p[:, q_ * 16:(q_ + 1) * 16], rkmT[:, tc * 128:(tc + 1) * 128], ident[0:16, 0:16]), reads=[rkmT, ident], writes=[p])
                    mk.op("dve", lambda e, p=p, g4=g4: e.tensor_copy(rkm[:, g4 * 4:(g4 + 1) * 4, :], p[:, :64].rearrange("p (a b) -> p a b", b=16)), reads=[p], writes=[rkm])
            mk.barrier()
            with ExitStack() as p2:
                load = make_wloader(p2, nbuf=4)
                sel = [sbt(p2, "sel%d" % i, [128, 16, 256], BF16) for i in range(2)]
                xg = sbt(p2, "xg", [128, 16, 256], BF16)
                hid = sbt(p2, "hid", [128, 8, 256], BF16)
                st_ = [sbt(p2, "sil%d" % i, [128, 512]) for i in range(2)]
                hidtok = sbt(p2, "hidtok", [128, 2, 1024], BF16)
                yest = [sbt(p2, "yest%d" % i, [128, D], BF16) for i in range(2)]
                for ex in range(16):
                    sl_ = sel[ex % 2]
                    for tc in range(16):
                        eng = "dve"
                        mk.op(eng, lambda e, sl_=sl_, tc=tc, ex=ex: e.tensor_scalar(sl_[:, tc, :], iota[:, 0:256], rkm[:, tc, ex:ex + 1], None, op0=ALU.is_equal), reads=[iota, rkm], writes=[sl_])
                    for Dc in range(16):
                        p = bank()
                        for tc in range(16):
                            mk.op("pe", lambda e, p=p, tc=tc, Dc=Dc, sl_=sl_: e.matmul(p[:, :256], h2tok[:, tc, Dc * 128:(Dc + 1) * 128], sl_[:, tc, :], start=(tc == 0), stop=(tc == 15)), reads=[h2tok, sl_], writes=[p])
                        if Dc % 2 == 0:
                            mk.op("dve", lambda e, p=p, Dc=Dc: e.tensor_copy(xg[:, Dc, :], p[:, :256]), reads=[p], writes=[xg])
                        else:
                            mk.op("act", lambda e, p=p, Dc=Dc: e.copy(xg[:, Dc, :], p[:, :256]), reads=[p], writes=[xg])
                    w1v = I.ew1[ex].rearrange("(k p) f -> p k f", p=128); w3v = I.ew3[ex].rearrange("(k p) f -> p k f", p=128)

                    jobs = [(fb, kb) for fb in range(2) for kb in range(2)]

                    def ld(j, w1v=w1v, w3v=w3v):
                        fb, kb = jobs[j]
                        return (load(w1v[:, kb * 8:(kb + 1) * 8, fb * 512:(fb + 1) * 512], 8, 512),
                                load(w3v[:, kb * 8:(kb + 1) * 8, fb * 512:(fb + 1) * 512], 8, 512))

                    def comp(j, ws):
                        fb, kb = jobs[j]
                        w1_, w3_ = ws
                        if kb == 0:
                            ffb[0] = [bank(), bank(), bank(), bank()]
                        pp = ffb[0]
                        for s2 in range(2):
                            for wi_, w_ in enumerate((w1_, w3_)):
                                p = pp[s2 * 2 + wi_]
                                for kk in range(8):
                                    k = kb * 8 + kk
                                    mk.op("pe", lambda e, p=p, k=k, kk=kk, s2=s2, w_=w_: e.matmul(
                                        p[:, :512], xg[:, k, s2 * 128:(s2 + 1) * 128], w_[:, kk * 512:(kk + 1) * 512],
                                        start=(k == 0), stop=(k == 15)), reads=[w_, xg], writes=[p])
                        if kb == 1:
                            for s2 in range(2):
                                s_ = st_[s2]
                                p1_ = pp[s2 * 2]; p3_ = pp[s2 * 2 + 1]
                                mk.op("act", lambda e, s_=s_, p1_=p1_: e.activation(s_[:], p1_[:, :512], AF.Silu), reads=[p1_], writes=[s_])
                                mk.op("dve", lambda e, s_=s_, p3_=p3_, fb=fb, s2=s2: e.tensor_tensor(hidtok[:, s2, fb * 512:(fb + 1) * 512], s_[:], p3_[:, :512], op=ALU.mult), reads=[s_, p3_], writes=[hidtok])
                    ffb = [None]
                    pipelined(4, ld, comp)
                    for s2 in range(2):
                        cnt["t"] += 1
                        pt = pbt[cnt["t"] % 2]
                        for ft in range(8):
                            mk.op("pe", lambda e, pt=pt, ft=ft, s2=s2: e.transpose(pt[:, ft * 128:(ft + 1) * 128], hidtok[:, s2, ft * 128:(ft + 1) * 128], identb[:]), reads=[hidtok, identb], writes=[pt])
                        if s2 == 0:
                            mk.op("dve", lambda e, pt=pt, s2=s2: e.tensor_copy(hid[:, :, s2 * 128:(s2 + 1) * 128], pt[:].rearrange("p (a b) -> p a b", b=128)), reads=[pt], writes=[hid])
                        else:
                            mk.op("act", lambda e, pt=pt, s2=s2: e.copy(hid[:, :, s2 * 128:(s2 + 1) * 128], pt[:].rearrange("p (a b) -> p a b", b=128)), reads=[pt], writes=[hid])
                    w2v = I.ew2[ex].rearrange("(k p) n -> p k n", p=128)

                    def ld2(nb, w2v=w2v):
                        return load(w2v[:, :, nb * 512:(nb + 1) * 512], 8, 512)

                    def comp2(nb, w2_):
                        for s2 in range(2):
                            p = bank(); ys = yest[s2]
                            for f in range(8):
                                mk.op("pe", lambda e, p=p, f=f, s2=s2: e.matmul(p[:, :512], hid[:, f, s2 * 128:(s2 + 1) * 128], w2_[:, f * 512:(f + 1) * 512], start=(f == 0), stop=(f == 7)), reads=[hid, w2_], writes=[p])
                            if s2 == 0:
                                mk.op("dve", lambda e, p=p, ys=ys, nb=nb: e.tensor_copy(ys[:, nb * 512:(nb + 1) * 512], p[:, :512]), reads=[p], writes=[ys])
                            else:
                                mk.op("act", lambda e, p=p, ys=ys, nb=nb: e.copy(ys[:, nb * 512:(nb + 1) * 512], p[:, :512]), reads=[p], writes=[ys])
                    pipelined(4, ld2, comp2)
                    for s2 in range(2):
                        mk.dma("sp", S.ye.t[ex * 2 + s2], yest[s2][:], reads=[yest[s2]], writes=[S.ye])
            mk.barrier()
            with ExitStack() as p3:
                selT = sbt(p3, "selT", [16, 16, 128])
                for ex in range(16):
                    mk.op("dve", lambda e, ex=ex: e.tensor_copy(selT[:, ex, :], ident[0:16, ex:ex + 1].to_broadcast([16, 128])), reads=[ident], writes=[selT])
                SGT = sbt(p3, "SGT", [128, 32, 512], BF16)
                gwB = [sbt(p3, "gwB%d" % i, [128, 512]) for i in range(2)]
                yeD = [sbt(p3, "yeD%d" % i, [128, 32, 128], BF16) for i in range(2)]
                x1t = [sbt(p3, "x1t%d" % i, [128, 512]) for i in range(2)]
                ot = [sbt(p3, "ot%d" % i, [128, 512]) for i in range(2)]
                yev = S.ye.t.rearrange("q p d -> p q d")
                for tt in range(4):
                    t0 = tt * 512
                    for ex in range(16):
                        pR = bank(); pG = bank(); gb_ = gwB[ex % 2]
                        mk.op("pe", lambda e, pR=pR, ex=ex, t0=t0: e.matmul(pR[:, :512], selT[:, ex, :], rkmT[:, t0:t0 + 512], start=True, stop=True), reads=[selT, rkmT], writes=[pR])
                        mk.op("pe", lambda e, pG=pG, ex=ex, t0=t0: e.matmul(pG[:, :512], selT[:, ex, :], gwT[:, t0:t0 + 512], start=True, stop=True), reads=[selT, gwT], writes=[pG])
                        mk.op("act", lambda e, gb_=gb_, pG=pG: e.copy(gb_[:], pG[:, :512]), reads=[pG], writes=[gb_])
                        for s2 in range(2):
                            mk.op("dve", lambda e, pR=pR, gb_=gb_, ex=ex, s2=s2: e.scalar_tensor_tensor(SGT[:, ex * 2 + s2, :], pR[:, :512], iota[:, 256 + s2:257 + s2], gb_[:], op0=ALU.is_equal, op1=ALU.mult), reads=[pR, iota, gb_], writes=[SGT])
                    for Dc in range(16):
                        yd = yeD[Dc % 2]; x_ = x1t[Dc % 2]; o_ = ot[Dc % 2]
                        mk.dma("sp", yd[:], yev[:, :, Dc * 128:(Dc + 1) * 128], reads=[S.ye], writes=[yd])
                        mk.dma("sp", x_[:], S.x1.t[Dc][:, t0:t0 + 512], reads=[S.x1], writes=[x_])
                        p = bank()
                        for q_ in range(32):
                            mk.op("pe", lambda e, p=p, q_=q_, yd=yd: e.matmul(p[:, :512], yd[:, q_, :], SGT[:, q_, :], start=(q_ == 0), stop=(q_ == 31)), reads=[yd, SGT], writes=[p])
                        mk.op("dve", lambda e, p=p, o_=o_, x_=x_, Dc=Dc: e.scalar_tensor_tensor(o_[:], p[:, :512], modsb[:, 80 + Dc, 0:1], x_[:], op0=ALU.mult, op1=ALU.add), reads=[p, modsb, x_], writes=[o_])
                        mk.dma("sp", outT.t[Dc][:, t0:t0 + 512], o_[:], reads=[o_], writes=[outT])
        mk.barrier()

    g.phase_moe = phase_moe


def prep_shared(inp):
    f = np.float32
    sh = {}
    sh["ada_w"] = np.ascontiguousarray(inp["ada_w"][0], f)
    sh["ada_b"] = np.ascontiguousarray(inp["ada_b"][0].reshape(96, 128).T, f)
    sh["n1w"] = np.ascontiguousarray(inp["norm1_w"][0].reshape(16, 128).T, f)
    sh["n2w"] = np.ascontiguousarray(inp["norm2_w"][0].reshape(16, 128).T, f)
    sh["w_in"] = np.ascontiguousarray(inp["w_in"][0], f)
    sh["conv_w"] = np.ascontiguousarray(inp["conv_w"][0].T.reshape(48, 128, 5).transpose(1, 0, 2), f)
    sc = np.concatenate([inp["dn_a_log"][0].reshape(-1), inp["dn_dt_bias"][0].reshape(-1)]).astype(f)
    sh["dn_sc"] = np.ascontiguousarray(np.broadcast_to(sc[None, :], (128, 64)), f)
    sh["hw"] = np.ascontiguousarray(np.stack([inp["dn_norm_w"][0], inp["na_q_norm_w"][0], inp["na_k_norm_w"][0]], axis=1), f)
    c = _consts()
    sh["consts"] = np.ascontiguousarray(np.stack([c[n] for n in CONST_NAMES]), f)
    sh["ropec"], sh["ropes"] = _rope_tables()
    sh["nab"] = _na_bias_table(np.asarray(inp["na_rpb"][0], f))
    io = np.zeros((128, 258), f)
    io[:, :256] = np.arange(256, dtype=f)[None, :]
    io[:, 256] = np.arange(128, dtype=f)
    io[:, 257] = np.arange(128, dtype=f) + 128
    sh["iota"] = io
    ii = np.arange(128)
    mks = [(ii[:, None] // 16 == ii[None, :] // 16)]
    for b_ in (16, 32, 64):
        same = (ii[:, None] // (2 * b_) == ii[None, :] // (2 * b_))
        hi = (ii % (2 * b_)) >= b_
        mks.append(same & (hi[:, None] != hi[None, :]))
    sh["dnmask"] = np.ascontiguousarray(np.stack(mks).astype(f))
    sh["w_a"] = np.ascontiguousarray(inp["w_branch_a"][0], f)
    sh["w_b"] = np.ascontiguousarray(inp["w_branch_b"][0], f)
    sh["w_out"] = np.ascontiguousarray(inp["w_out"][0], f)
    sh["w_r"] = np.ascontiguousarray(inp["w_router"][0], f)
    sh["ew1"] = np.ascontiguousarray(inp["expert_w1"][0], f)
    sh["ew3"] = np.ascontiguousarray(inp["expert_w3"][0], f)
    sh["ew2"] = np.ascontiguousarray(inp["expert_w2"][0], f)
    return sh


def prep_core(inp, b):
    f = np.float32
    m = {}
    xc = np.concatenate([inp["x"][b], inp["ctx"][b]], axis=0)
    m["xT"] = np.ascontiguousarray(xc.T.reshape(16, 128, T), f)
    cs = np.stack([inp["c"][b].reshape(16, 128), inp["c_ctx"].reshape(16, 128)], axis=-1)
    m["cs"] = np.ascontiguousarray(cs.transpose(1, 0, 2).reshape(128, 32), f)
    return m


_CACHE = {}


def kernel(**inputs):
    inp = {k: np.asarray(v) for k, v in inputs.items()}
    if "nc" not in _CACHE:
        _CACHE["nc"] = build_nc()[0]
    nc = _CACHE["nc"]
    sh = prep_shared(inp)
    in_maps = []
    for b in range(8):
        m = dict(sh)
        m.update(prep_core(inp, b))
        in_maps.append(m)
    res = run_bass_kernel_spmd(nc, in_maps, core_ids=list(range(8)))
    out = np.stack([np.asarray(r["outT"], np.float32).reshape(D, L).T for r in res.results], axis=0)
    return np.ascontiguousarray(out, np.float32)
```

```python
import os
import numpy as np
from contextlib import ExitStack
import ml_dtypes
import concourse.bass as bass
import concourse.mybir as mybir
from concourse.bass_utils import run_bass_kernel_spmd

F32 = mybir.dt.float32
BF16 = mybir.dt.bfloat16
F32R = mybir.dt.float32r
ALU = mybir.AluOpType
AF = mybir.ActivationFunctionType
AX = mybir.AxisListType

D = 2048
L = 2048
CTX = 256
T = L + CTX
INW = 18496
NCH = T // 128
EPS = 1e-6
SEM_ROLL = 30000


class Buf:
    __slots__ = ("name", "lw", "rd", "excl")

    def __init__(self, name=""):
        self.name = name
        self.lw = None
        self.rd = []
        self.excl = False


class TL:
    def __init__(self, t, name=""):
        self.t = t
        self.b = Buf(name)

    def __getitem__(self, k):
        return self.t[k]


def _b(x):
    return x.b if isinstance(x, TL) else x


class MK:
    ENG = ("pe", "act", "dve", "pool", "sp")

    def __init__(self, nc, es):
        self.nc = nc
        self.es = es
        self.eo = {"pe": nc.tensor, "act": nc.scalar, "dve": nc.vector, "pool": nc.gpsimd, "sp": nc.sync}
        self.cnt = {e: 0 for e in self.ENG}
        self.semi = 0
        self.cur = {}
        for e in self.ENG:
            self.cur[e] = self._newsem()
        self.seen = {e: {} for e in self.ENG}
        self.dsem = {}
        self.dpos = {}
        for q in ("sp", "act", "pool"):
            n = 16 if q == "sp" else 4
            self.dsem[q] = [[self._newsem(), 0, None] for _ in range(n)]
            self.dpos[q] = 0
        self.ninst = 0

    def _newsem(self):
        self.semi += 1
        return self.es.enter_context(self.nc.semaphore("s%d" % self.semi))

    def _wait(self, e, ev):
        if ev is None:
            return
        sem, val, src = ev
        if src == e and e == "pe":
            return
        key = id(sem)
        if self.seen[e].get(key, 0) >= val:
            return
        self.seen[e][key] = val
        self.eo[e].wait_ge(sem, val)

    def _deps(self, e, reads, writes):
        for b in reads:
            self._wait(e, b.lw)
        for b in writes:
            self._wait(e, b.lw)
            for r in b.rd:
                self._wait(e, r)

    def _commit(self, ev, reads, writes):
        for b in writes:
            b.lw = ev
            b.rd = []
        for b in reads:
            if b not in writes:
                b.rd.append(ev)
                if len(b.rd) > 16:
                    d = {}
                    for r in b.rd:
                        k = id(r[0])
                        if k not in d or d[k][1] < r[1]:
                            d[k] = r
                    b.rd = list(d.values())

    def op(self, e, fn, reads=(), writes=()):
        reads = [_b(x) for x in reads]
        writes = [_b(x) for x in writes]
        writes = writes + [b for b in reads if b.excl and b not in writes]
        reads = [b for b in reads if not b.excl]
        self._deps(e, reads, writes)
        if self.cnt[e] >= SEM_ROLL:
            self.cur[e] = self._newsem()
            self.cnt[e] = 0
        self.cnt[e] += 1
        ev = (self.cur[e], self.cnt[e], e)
        fn(self.eo[e]).then_inc(ev[0], 1)
        self._commit(ev, reads, writes)
        self.ninst += 1
        return ev

    def dma(self, q, out, in_, reads=(), writes=(), **kw):
        reads = [_b(x) for x in reads]
        writes = [_b(x) for x in writes]
        self._deps(q, reads, writes)
        slot = self.dsem[q][self.dpos[q]]
        self.dpos[q] = (self.dpos[q] + 1) % len(self.dsem[q])
        if slot[2] is not None:
            self._wait(q, slot[2])
        slot[1] += 16
        ev = (slot[0], slot[1], "dma")
        slot[2] = ev
        self.eo[q].dma_start(out=out, in_=in_, **kw).then_inc(slot[0], 16)
        self._commit(ev, reads, writes)
        self.ninst += 1
        return ev

    def barrier(self):
        evs = []
        for e in self.ENG:
            if self.cnt[e] > 0:
                evs.append((self.cur[e], self.cnt[e], "x"))
        for q in self.dsem:
            for slot in self.dsem[q]:
                if slot[2] is not None:
                    evs.append(slot[2])
        for e in self.ENG:
            for ev in evs:
                self._wait(e, ev)


class Ctx:
    pass


_UID = [0]


def _alloc(nc, stack, kind, name, shape, dt):
    f = nc.sbuf_tensor if kind == "sb" else nc.psum_tensor
    _UID[0] += 1
    name = "%s_%s_%d" % (kind, name, _UID[0])
    tl = TL(stack.enter_context(f(name, list(shape), dt)), name)
    if kind == "ps":
        tl.b.excl = True
    return tl


def _consts():
    c = {}
    i = np.arange(128)
    c["ident"] = np.eye(128, dtype=np.float32)
    c["ones"] = np.ones((128, 128), np.float32)
    c["uf"] = (i[:, None] <= i[None, :]).astype(np.float32)
    c["ub"] = (i[:, None] >= i[None, :]).astype(np.float32)
    NEG = -1.0e6
    c["mf"] = np.where(i[None, :] >= i[:, None], 0.0, NEG).astype(np.float32)
    c["mb"] = np.where(i[None, :] <= i[:, None], 0.0, NEG).astype(np.float32)
    c["offd"] = (1.0 - np.eye(128)).astype(np.float32)
    R = np.zeros((128, 128), np.float32)
    for p in range(128):
        q = p + 32 if (p % 64) < 32 else p - 32
        R[q, p] = 1.0
    c["rot"] = R
    return c


CONST_NAMES = ["ident", "ones", "uf", "ub", "mf", "mb", "offd", "rot"]


def _rope_tables():
    pos = np.arange(L)
    row = (pos // 64).astype(np.float32)
    col = (pos % 64).astype(np.float32)
    half = 64
    inv = (np.float32(10000.0) ** (-np.arange(0, half, 2, dtype=np.float32) / np.float32(half))).astype(np.float32)
    ang_r = row[:, None] * inv[None, :]
    ang_c = col[:, None] * inv[None, :]
    cr, sr, cc, sc = np.cos(ang_r), np.sin(ang_r), np.cos(ang_c), np.sin(ang_c)
    COS = np.concatenate([cr, cr, cc, cc], axis=1).T.astype(np.float32)
    SIN = np.concatenate([-sr, sr, -sc, sc], axis=1).T.astype(np.float32)
    return np.ascontiguousarray(COS), np.ascontiguousarray(SIN)


NA_BLOCKS = [(0, 6), (4, 8), (12, 8), (20, 6)]


def _na_bias_table(rpb):
    H = rpb.shape[0]
    out = np.full((H, 28, 128, 512), -30000.0, np.float32)
    ti = 0
    qq = np.arange(512)
    qr_l, qc = qq // 64, qq % 64
    kk = np.arange(128)
    kr_l, kc = kk // 64, kk % 64
    qstart = np.clip(qc - 8, 0, 48)
    for m, (lo, nt) in enumerate(NA_BLOCKS):
        qr = 8 * m + qr_l
        rs = np.clip(qr - 4, 0, 24)
        for j in range(nt):
            kr = lo + 2 * j + kr_l
            okr = (kr[:, None] >= rs[None, :]) & (kr[:, None] < rs[None, :] + 8) & (kr[:, None] < 32)
            okc = (kc[:, None] >= qstart[None, :]) & (kc[:, None] < qstart[None, :] + 16)
            ok = okr & okc
            dr = np.clip(kr[:, None] - qr[None, :] + 7, 0, 14)
            dc = np.clip(kc[:, None] - qc[None, :] + 15, 0, 30)
            g = rpb[:, dr, dc]
            out[:, ti] = np.where(ok[None], g, np.float32(-30000.0))
            ti += 1
    return out


def build_nc(upto=99, dump=(), feed=(), run=None, heads=range(16)):
    nc = bass.Bass("TRN2", target_bir_lowering=False)
    g = Ctx()
    g.nc = nc

    def din(name, shape, dt=F32):
        return nc.dram_tensor(name, list(shape), dt, kind="ExternalInput").ap()

    def dscr(name, shape, dt=F32):
        kind = "ExternalOutput" if name in dump else ("ExternalInput" if name in feed else "Internal")
        return TL(nc.dram_tensor(name, list(shape), dt, kind=kind).ap(), name)

    I = Ctx()
    I.xT = din("xT", [16, 128, T])
    I.cs = din("cs", [128, 32])
    I.ada_w = din("ada_w", [D, 6 * D])
    I.ada_b = din("ada_b", [128, 96])
    I.n1w = din("n1w", [128, 16])
    I.n2w = din("n2w", [128, 16])
    I.w_in = din("w_in", [D, INW])
    I.conv_w = din("conv_w", [128, 48, 5])
    I.dn_sc = din("dn_sc", [128, 64])
    I.hw = din("hw", [128, 3])
    I.consts = din("consts", [len(CONST_NAMES), 128, 128])
    I.ropec = din("ropec", [128, L])
    I.ropes = din("ropes", [128, L])
    I.nab = din("nab", [16, 28, 128, 512])
    I.iota = din("iota", [128, 258])
    I.dnmask = din("dnmask", [4, 128, 128])
    I.w_a = din("w_a", [D, D])
    I.w_b = din("w_b", [D, D])
    I.w_out = din("w_out", [D, D])
    I.w_r = din("w_r", [D, 16])
    I.ew1 = din("ew1", [16, D, 1024])
    I.ew3 = din("ew3", [16, D, 1024])
    I.ew2 = din("ew2", [16, 1024, D])
    outT = TL(nc.dram_tensor("outT", [16, 128, L], F32, kind="ExternalOutput").ap(), "outT")

    S = Ctx()
    S.dn = dscr("s_dn", [48, 128, T])
    S.z = dscr("s_z", [16, 128, L])
    S.ba = dscr("s_ba", [T, 64])
    S.nq = dscr("s_nq", [16, 128, L])
    S.nk = dscr("s_nk", [16, 128, T])
    S.nv = dscr("s_nv", [T, D], BF16)
    S.ga = dscr("s_ga", [16, 128, L])
    S.gb = dscr("s_gb", [16, 128, L])
    S.odn = dscr("s_odn", [16, 128, L], BF16)
    S.ona = dscr("s_ona", [16, 128, L], BF16)
    S.y = dscr("s_y", [16, 128, L], BF16)
    S.x1 = dscr("s_x1", [16, 128, L])
    S.ye = dscr("s_ye", [32, 128, D], BF16)
    S.hT = dscr("s_hT", [16, 128, T], BF16)
    S.grow = dscr("s_grow", [32, T])
    S.brow = dscr("s_brow", [32, T])

    with ExitStack() as es:
        mk = MK(nc, es)
        g.mk = mk

        def sbt(stack, name, shape, dt=F32):
            return _alloc(nc, stack, "sb", name, shape, dt)

        def pst(stack, name, shape, dt=F32):
            return _alloc(nc, stack, "ps", name, shape, dt)

        K = {}
        for i, n in enumerate(CONST_NAMES):
            K[n] = sbt(es, "k_" + n, [128, 128])
            mk.dma("sp", K[n][:], I.consts[i], writes=[K[n]])
        onesb = sbt(es, "onesb", [128, 128], BF16)
        identb = sbt(es, "identb", [128, 128], BF16)
        mk.op("dve", lambda e: e.tensor_copy(onesb[:], K["ones"][:]), reads=[K["ones"]], writes=[onesb])
        mk.op("dve", lambda e: e.tensor_copy(identb[:], K["ident"][:]), reads=[K["ident"]], writes=[identb])
        epsT = sbt(es, "epsT", [128, 1])
        mk.op("dve", lambda e: e.memset(epsT[:], EPS), writes=[epsT])
        modsb = sbt(es, "modsb", [128, 96, 2])
        a1 = sbt(es, "a1", [128, 16]); a1c = sbt(es, "a1c", [128, 16]); a2 = sbt(es, "a2", [128, 16])
        n1w = sbt(es, "n1w", [128, 16]); n2w = sbt(es, "n2w", [128, 16])
        hw = sbt(es, "hw", [128, 3])
        mk.dma("sp", n1w[:], I.n1w, writes=[n1w])
        mk.dma("sp", n2w[:], I.n2w, writes=[n2w])
        mk.dma("sp", hw[:], I.hw, writes=[hw])

        def phase_mod():
            with ExitStack() as ps:
                cs = sbt(ps, "cs", [128, 32]); sc_ = sbt(ps, "silu_c", [128, 32])
                adab = sbt(ps, "adab", [128, 96])
                wb = [sbt(ps, "adaw%d" % i, [128, 16, 512]) for i in range(2)]
                pp = [pst(ps, "p0_%d" % i, [128, 512]) for i in range(2)]
                mk.dma("sp", cs[:], I.cs, writes=[cs])
                mk.dma("sp", adab[:], I.ada_b, writes=[adab])
                mk.op("act", lambda e: e.activation(sc_[:], cs[:], AF.Silu), reads=[cs], writes=[sc_])
                wv = I.ada_w.rearrange("(k p) n -> p k n", p=128)
                for nb in range(24):
                    w = wb[nb % 2]
                    mk.dma("sp", w[:], wv[:, :, nb * 512:(nb + 1) * 512], writes=[w])
                    for ct in range(4):
                        j = nb * 4 + ct
                        p = pp[j % 2]
                        for k in range(16):
                            mk.op("pe", lambda e, p=p, w=w, k=k, ct=ct: e.matmul(
                                p[:, 0:2], w[:, k, ct * 128:(ct + 1) * 128], sc_[:, 2 * k:2 * k + 2],
                                start=(k == 0), stop=(k == 15)), reads=[w, sc_], writes=[p])
                        mk.op("dve", lambda e, p=p, j=j: e.tensor_scalar(
                            modsb[:, j, :], p[:, 0:2], adab[:, j:j + 1], None, op0=ALU.add),
                            reads=[p, adab], writes=[modsb])
                mk.op("dve", lambda e: e.scalar_tensor_tensor(a1[:], modsb[:, 16:32, 0], 1.0, n1w[:], op0=ALU.add, op1=ALU.mult),
                      reads=[modsb, n1w], writes=[a1])
                mk.op("dve", lambda e: e.scalar_tensor_tensor(a1c[:], modsb[:, 16:32, 1], 1.0, n1w[:], op0=ALU.add, op1=ALU.mult),
                      reads=[modsb, n1w], writes=[a1c])
                mk.op("dve", lambda e: e.scalar_tensor_tensor(a2[:], modsb[:, 64:80, 0], 1.0, n2w[:], op0=ALU.add, op1=ALU.mult),
                      reads=[modsb, n2w], writes=[a2])
            mk.barrier()

        def norm_mod(ps_bank, xt, sq, rs, tmp2, w, a_t, bcol, out_fn, extra_reads=()):
            mk.op("act", lambda e: e.activation(sq[:, :, :w], xt[:, :, :w], AF.Square), reads=[xt], writes=[sq])
            for k in range(16):
                mk.op("pe", lambda e, k=k: e.matmul(ps_bank[:, :w], K["ones"][:], sq[:, k, :w], start=(k == 0), stop=(k == 15)),
                      reads=[K["ones"], sq], writes=[ps_bank])
            mk.op("act", lambda e: e.activation(rs[:, :w], ps_bank[:, :w], AF.Ln, bias=epsT[:, 0:1], scale=1.0 / D),
                  reads=[ps_bank, epsT], writes=[rs])
            mk.op("act", lambda e: e.activation(rs[:, :w], rs[:, :w], AF.Exp, scale=-0.5), reads=[rs], writes=[rs])
            for k in range(16):
                tm = tmp2[k % 2]
                mk.op("dve", lambda e, k=k, tm=tm: e.tensor_tensor(tm[:, :w], xt[:, k, :w], rs[:, :w], op=ALU.mult),
                      reads=[xt, rs], writes=[tm])
                o, ow = out_fn(k)
                mk.op("act", lambda e, k=k, tm=tm, o=o: e.activation(o, tm[:, :w], AF.Identity, bias=bcol(k), scale=a_t[:, k:k + 1]),
                      reads=[tm, a_t, modsb] + list(extra_reads), writes=[ow])

        TOK_TILES = [(0, 512), (512, 512), (1024, 512), (1536, 512), (2048, 256)]

        def phase_inproj():
            with ExitStack() as ps:
                hT = sbt(ps, "hT", [128, 16, T], BF16)
                pb = [pst(ps, "p1_%d" % i, [128, 512]) for i in range(8)]
                with ExitStack() as p1:
                    xts = [sbt(p1, "xt%d" % i, [128, 16, 512]) for i in range(2)]
                    sq = sbt(p1, "sq", [128, 16, 512])
                    rs = sbt(p1, "rs", [128, 512])
                    tmp2 = [sbt(p1, "tmp%d" % i, [128, 512]) for i in range(2)]
                    xv = I.xT.rearrange("k p t -> p k t")
                    for ti, (t0, w) in enumerate(TOK_TILES):
                        xt = xts[ti % 2]
                        mk.dma("sp", xt[:, :, :w], xv[:, :, t0:t0 + w], writes=[xt])
                        ctxp = (t0 >= L)
                        norm_mod(pb[ti % 2], xt, sq, rs, tmp2, w, a1c if ctxp else a1,
                                 (lambda k, c=(1 if ctxp else 0): modsb[:, k, c:c + 1]),
                                 lambda k, t0=t0, w=w: (hT[:, k, t0:t0 + w], hT))
                    if "s_hT" in dump:
                        mk.dma("sp", S.hT.t.rearrange("k p t -> p k t"), hT[:], reads=[hT], writes=[S.hT])
                mk.barrier()
                if upto < 2:
                    return
                stg = [sbt(ps, "wstg%d" % i, [128, 16, 256]) for i in range(2)]
                wbf = [sbt(ps, "wbf%d" % i, [128, 16, 256], BF16) for i in range(2)]
                ost = [sbt(ps, "ost%d" % i, [128, T]) for i in range(2)]
                vst = [sbt(ps, "vst%d" % i, [128, NCH, 256], BF16) for i in range(2)]
                bst = sbt(ps, "bst", [128, NCH, 64])
                wv = I.w_in.rearrange("(k p) n -> p k n", p=128)
                blocks = []
                for c0 in range(0, 6144, 256):
                    blocks.append((c0, 256, "fm", (S.dn, c0 // 128), T, "copy"))
                for c0 in range(6144, 8192, 256):
                    blocks.append((c0, 256, "fm", (S.z, (c0 - 6144) // 128), L, "silu"))
                blocks.append((8192, 64, "ba", None, T, None))
                for c0 in range(8256, 10304, 256):
                    blocks.append((c0, 256, "fm", (S.nq, (c0 - 8256) // 128), L, "copy"))
                for c0 in range(10304, 12352, 256):
                    blocks.append((c0, 256, "fm", (S.nk, (c0 - 10304) // 128), T, "copy"))
                for c0 in range(12352, 14400, 256):
                    blocks.append((c0, 256, "tm", c0 - 12352, T, None))
                for c0 in range(14400, 16448, 256):
                    blocks.append((c0, 256, "fm", (S.ga, (c0 - 14400) // 128), L, "sig"))
                for c0 in range(16448, 18496, 256):
                    blocks.append((c0, 256, "fm", (S.gb, (c0 - 16448) // 128), L, "sig"))
                st = {"pi": 0, "oi": 0}

                def load(bi):
                    c0, nc_, *_ = blocks[bi]
                    s_ = stg[bi % 2]; wb_ = wbf[bi % 2]
                    mk.dma("sp", s_[:, :, :nc_], wv[:, :, c0:c0 + nc_], writes=[s_])
                    mk.op("act", lambda e: e.copy(wb_[:, 0:8, :nc_], s_[:, 0:8, :nc_]), reads=[s_], writes=[wb_])
                    mk.op("dve", lambda e: e.tensor_copy(wb_[:, 8:16, :nc_], s_[:, 8:16, :nc_]), reads=[s_], writes=[wb_])

                load(0)
                for bi, (c0, nc_, kind, dst, thi, epi) in enumerate(blocks):
                    if bi + 1 < len(blocks):
                        load(bi + 1)
                    wb_ = wbf[bi % 2]
                    if kind == "fm":
                        for ctile in range(nc_ // 128):
                            o = ost[st["oi"] % 2]; st["oi"] += 1
                            for (t0, w) in TOK_TILES:
                                if t0 >= thi:
                                    continue
                                p = pb[st["pi"] % 8]; st["pi"] += 1
                                for k in range(16):
                                    mk.op("pe", lambda e, p=p, k=k, ctile=ctile, t0=t0, w=w: e.matmul(
                                        p[:, :w], wb_[:, k, ctile * 128:(ctile + 1) * 128], hT[:, k, t0:t0 + w],
                                        start=(k == 0), stop=(k == 15)), reads=[wb_, hT], writes=[p])
                                if epi == "copy":
                                    mk.op("dve", lambda e, p=p, o=o, t0=t0, w=w: e.tensor_copy(o[:, t0:t0 + w], p[:, :w]), reads=[p], writes=[o])
                                else:
                                    fn = AF.Silu if epi == "silu" else AF.Sigmoid
                                    mk.op("act", lambda e, p=p, o=o, t0=t0, w=w, fn=fn: e.activation(o[:, t0:t0 + w], p[:, :w], fn), reads=[p], writes=[o])
                            dt_, ti_ = dst
                            mk.dma("sp", dt_.t[ti_ + ctile][:, 0:thi], o[:, 0:thi], reads=[o], writes=[dt_])
                    elif kind == "tm":
                        vs = vst[st["oi"] % 2]; st["oi"] += 1
                        for ti in range(NCH):
                            p = pb[st["pi"] % 8]; st["pi"] += 1
                            for k in range(16):
                                mk.op("pe", lambda e, p=p, k=k, ti=ti: e.matmul(
                                    p[:, :256], hT[:, k, ti * 128:(ti + 1) * 128], wb_[:, k, :256],
                                    start=(k == 0), stop=(k == 15)), reads=[wb_, hT], writes=[p])
                            eng = "dve" if ti % 2 == 0 else "act"
                            if eng == "dve":
                                mk.op("dve", lambda e, p=p, ti=ti: e.tensor_copy(vs[:, ti, :], p[:, :256]), reads=[p], writes=[vs])
                            else:
                                mk.op("act", lambda e, p=p, ti=ti: e.copy(vs[:, ti, :], p[:, :256]), reads=[p], writes=[vs])
                        mk.dma("sp", S.nv.t.rearrange("(n p) c -> p n c", p=128)[:, :, dst:dst + 256], vs[:], reads=[vs], writes=[S.nv])
                    else:
                        for ti in range(NCH):
                            p = pb[st["pi"] % 8]; st["pi"] += 1
                            for k in range(16):
                                mk.op("pe", lambda e, p=p, k=k, ti=ti: e.matmul(
                                    p[:, :64], hT[:, k, ti * 128:(ti + 1) * 128], wb_[:, k, :64],
                                    start=(k == 0), stop=(k == 15)), reads=[wb_, hT], writes=[p])
                            mk.op("dve", lambda e, p=p, ti=ti: e.tensor_copy(bst[:, ti, :], p[:, :64]), reads=[p], writes=[bst])
                        mk.dma("sp", S.ba.t.rearrange("(n p) c -> p n c", p=128), bst[:], reads=[bst], writes=[S.ba])
            mk.barrier()

        g.phase_mod = phase_mod
        g.phase_inproj = phase_inproj
        phases_extra(g, I, S, K, mk, sbt, pst, es, outT, dict(
            modsb=modsb, a2=a2, hw=hw, epsT=epsT, onesb=onesb, identb=identb, norm_mod=norm_mod, dump=dump, upto=upto))

        if run is None:
            run = ("mod", "inproj", "dn", "na", "merge", "moe")
        if "mod" in run:
            phase_mod()
        if "inproj" in run:
            phase_inproj()
        if "dn" in run:
            g.phase_dn(heads)
        if "na" in run:
            g.phase_na(heads)
        if "merge" in run:
            g.phase_merge()
        if "moe" in run:
            g.phase_moe()
        if "modsb" in dump:
            md = nc.dram_tensor("modsb_o", [128, 192], F32, kind="ExternalOutput").ap()
            mk.dma("sp", md, modsb[:].rearrange("p a b -> p (a b)"), reads=[modsb])
        mk.barrier()
        g.ninst = mk.ninst
    return nc, g


def phases_extra(g, I, S, K, mk, sbt, pst, es, outT, X):
    nc = g.nc
    modsb = X["modsb"]; hw = X["hw"]; epsT = X["epsT"]; onesb = X["onesb"]; identb = X["identb"]
    dump = X["dump"]
    TOK5 = [(0, 512), (512, 512), (1024, 512), (1536, 512), (2048, 256)]
    ident = K["ident"]; ones = K["ones"]

    def phase_dn(heads=range(16)):
        with ExitStack() as ps:
            pw = [pst(ps, "dnw%d" % i, [128, 512]) for i in range(2)]
            pqb = [pst(ps, "dnq%d" % i, [128, 512]) for i in range(6)]
            class SlotV(TL):
                def __init__(self, bank, j):
                    self.t = bank.t[:, j * 128:(j + 1) * 128]
                    self.b = bank.b

            banks = [[SlotV(pqb[i], j) for j in range(4)] for i in range(6)]
            sl = {"i": 0, "w": 0}

            def nbank():
                sl["i"] += 1
                return banks[sl["i"] % 6]

            def nwide():
                sl["w"] += 1
                return pw[sl["w"] % 2]

            ba = sbt(ps, "ba", [128, NCH, 64]); dsc = sbt(ps, "dsc", [128, 64]); negexp = sbt(ps, "negexp", [128, 32])
            betaC = sbt(ps, "betaC", [128, NCH, 32]); gC = sbt(ps, "gC", [128, NCH, 32])
            cw = sbt(ps, "cw", [128, 48, 5])
            mk.dma("sp", ba[:], S.ba.t.rearrange("(n p) c -> p n c", p=128), reads=[S.ba], writes=[ba])
            mk.dma("sp", dsc[:], I.dn_sc, writes=[dsc])
            mk.dma("sp", cw[:], I.conv_w, writes=[cw])
            mk.op("act", lambda e: e.activation(negexp[:], dsc[:, 0:32], AF.Exp), reads=[dsc], writes=[negexp])
            mk.op("dve", lambda e: e.tensor_scalar(negexp[:], negexp[:], -1.0, None, op0=ALU.mult), reads=[negexp], writes=[negexp])
            mk.op("act", lambda e: e.activation(betaC[:], ba[:, :, 0:32], AF.Sigmoid), reads=[ba], writes=[betaC])
            for n in range(NCH):
                mk.op("dve", lambda e, n=n: e.tensor_tensor(gC[:, n, :], ba[:, n, 32:64], dsc[:, 32:64], op=ALU.add), reads=[ba, dsc], writes=[gC])
            mk.op("act", lambda e: e.activation(gC[:], gC[:], AF.Exp), reads=[gC], writes=[gC])
            mk.op("act", lambda e: e.activation(gC[:], gC[:], AF.Ln, bias=1.0), reads=[gC], writes=[gC])
            for n in range(NCH):
                mk.op("dve", lambda e, n=n: e.tensor_tensor(gC[:, n, :], gC[:, n, :], negexp[:], op=ALU.mult), reads=[gC, negexp], writes=[gC])

            with ExitStack() as p0:
                growT = [sbt(p0, "growT%d" % i, [16, NCH, 128]) for i in range(2)]
                browT = [sbt(p0, "browT%d" % i, [16, NCH, 128]) for i in range(2)]
                Um0 = [K["uf"], K["ub"]]
                for n in range(NCH):
                    bk = nbank()
                    for d in range(2):
                        mk.op("pe", lambda e, bk=bk, d=d, n=n: e.matmul(bk[d][0:16, :], gC[:, n, d * 16:(d + 1) * 16], Um0[d][:], start=True, stop=True), reads=[gC, Um0[d]], writes=[bk[d]])
                        mk.op("pe", lambda e, bk=bk, d=d, n=n: e.transpose(bk[2 + d][0:16, :], betaC[:, n, d * 16:(d + 1) * 16], ident[:]), reads=[betaC, ident], writes=[bk[2 + d]])
                    for d in range(2):
                        mk.op("dve", lambda e, bk=bk, d=d, n=n: e.tensor_copy(growT[d][:, n, :], bk[d][0:16, :]), reads=[bk[d]], writes=[growT[d]])
                        mk.op("act", lambda e, bk=bk, d=d, n=n: e.copy(browT[d][:, n, :], bk[2 + d][0:16, :]), reads=[bk[2 + d]], writes=[browT[d]])
                for d in range(2):
                    mk.dma("sp", S.grow.t[d * 16:(d + 1) * 16, :], growT[d][:].rearrange("p a b -> p (a b)"), reads=[growT[d]], writes=[S.grow])
                    mk.dma("sp", S.brow.t[d * 16:(d + 1) * 16, :], browT[d][:].rearrange("p a b -> p (a b)"), reads=[browT[d]], writes=[S.brow])
            GBt = [sbt(ps, "GBt%d" % i, [128, T]) for i in range(2)]
            BBt = [sbt(ps, "BBt%d" % i, [128, T]) for i in range(2)]
            raw = sbt(ps, "raw", [128, T]); sqb = raw; rsb = sbt(ps, "rsb", [128, T])
            cq = sbt(ps, "cq", [128, T]); ck = sbt(ps, "ck", [128, T]); cv = sbt(ps, "cv", [128, T])
            ktok = sbt(ps, "ktok", [128, NCH, 128]); vtok = sbt(ps, "vtok", [128, NCH, 128])
            oacc = sbt(ps, "oacc", [128, L]); zt = cv
            oaccB = [Buf("oacc%d" % n) for n in range(16)]
            NS = 8
            uS = sbt(ps, "uS", [128, NS, 128]); wS = sbt(ps, "wS", [128, NS, 128])
            aS = sbt(ps, "aS", [128, NS, 128]); qS = sbt(ps, "qS", [128, NS, 128])
            uV = [TL(uS.t[:, i, :]) for i in range(NS)]; wV = [TL(wS.t[:, i, :]) for i in range(NS)]
            aV = [TL(aS.t[:, i, :]) for i in range(NS)]; qV = [TL(qS.t[:, i, :]) for i in range(NS)]
            W = 4
            wb = {}
            for nm in ["A0", "A1", "B0", "B1", "P0", "P1", "ApI", "dm1", "dm2", "dec", "dec2", "eGb", "BM", "Af"]:
                wb[nm] = [sbt(ps, "w%s%d" % (nm, i), [128, 128]) for i in range(W)]
            wb["vb"] = [sbt(ps, "wvb%d" % i, [128, 128]) for i in range(W)]
            wb["kbg"] = [sbt(ps, "wkbg%d" % i, [128, 128]) for i in range(W)]
            bd16 = sbt(ps, "bd16", [128, 128]); msk = [sbt(ps, "msk%d" % i, [128, 128]) for i in range(3)]
            mk.dma("sp", bd16[:], I.dnmask[0], writes=[bd16])
            for i_ in range(3):
                mk.dma("sp", msk[i_][:], I.dnmask[1 + i_], writes=[msk[i_]])
            kdec = [sbt(ps, "kdec%d" % i, [128, 128]) for i in range(4)]
            vnew = [sbt(ps, "vnew%d" % i, [128, 128]) for i in range(4)]
            Sst = [[sbt(ps, "S%d_%d" % (d, i), [128, 128]) for i in range(2)] for d in range(2)]
            small = {}
            for nm in ["gc", "bc", "Gcol", "eGcol", "kbs", "kds", "eGl", "tmp"]:
                small[nm] = [sbt(ps, "sm%s%d" % (nm, d), [128, NCH]) for d in range(2)]
            Umat = [K["uf"], K["ub"]]; Mm = [K["mf"], K["mb"]]; Mo = [K["mb"], K["mf"]]
            offd = K["offd"]

            R = lambda ap: ap.bitcast(F32R)
            DN_STOP = int(os.environ.get("DN_STOP", "99"))
            PREP_STOP = int(os.environ.get("PREP_STOP", "99"))
            for h in heads:
                if DN_STOP <= 0:
                    break
                for idx, (acc, eng) in enumerate([(cq, "dve"), (ck, "dve"), (cv, "pool")]):
                    mk.dma("sp", raw[:], S.dn.t[idx * 16 + h], reads=[S.dn], writes=[raw])
                    t_ = idx * 16 + h
                    if eng == "dve":
                        mk.op(eng, lambda e, acc=acc, t_=t_: e.tensor_scalar(R(acc[:]), raw[:], cw[:, t_, 2:3], None, op0=ALU.mult),
                              reads=[raw, cw], writes=[acc])
                    else:
                        mk.op(eng, lambda e, acc=acc, t_=t_: e.tensor_tensor(R(acc[:]), raw[:], cw[:, t_, 2:3].to_broadcast([128, T]), op=ALU.mult),
                              reads=[raw, cw], writes=[acc])
                    for (s0, s1) in [(0, L), (L, T)]:
                        for jj in (0, 1, 3, 4):
                            sh = jj - 2
                            d0 = s0 + max(0, -sh); d1 = s1 - max(0, sh)
                            if eng == "dve":
                                mk.op(eng, lambda e, acc=acc, t_=t_, jj=jj, d0=d0, d1=d1, sh=sh: e.scalar_tensor_tensor(
                                    R(acc[:, d0:d1]), raw[:, d0 + sh:d1 + sh], cw[:, t_, jj:jj + 1], acc[:, d0:d1], op0=ALU.mult, op1=ALU.add),
                                    reads=[raw, cw, acc], writes=[acc])
                            else:
                                mk.op(eng, lambda e, t_=t_, jj=jj, d0=d0, d1=d1, sh=sh: e.tensor_tensor(
                                    rsb[:, d0:d1], raw[:, d0 + sh:d1 + sh], cw[:, t_, jj:jj + 1].to_broadcast([128, d1 - d0]), op=ALU.mult),
                                    reads=[raw, cw], writes=[rsb])
                                mk.op(eng, lambda e, acc=acc, d0=d0, d1=d1: e.tensor_tensor(
                                    R(acc[:, d0:d1]), acc[:, d0:d1], rsb[:, d0:d1], op=ALU.add),
                                    reads=[rsb, acc], writes=[acc])
                    mk.op("act", lambda e, acc=acc: e.activation(R(acc[:]), acc[:], AF.Silu), reads=[acc], writes=[acc])
                if DN_STOP <= 1:
                    break
                for acc, scl in [(cq, 128.0 ** -0.5), (ck, 1.0)]:
                    mk.op("act", lambda e, acc=acc: e.activation(sqb[:], acc[:], AF.Square), reads=[acc], writes=[sqb])
                    for (t0, w) in TOK5:
                        p = nwide()
                        mk.op("pe", lambda e, p=p, t0=t0, w=w: e.matmul(p[:, :w], ones[:], sqb[:, t0:t0 + w], start=True, stop=True),
                              reads=[ones, sqb], writes=[p])
                        mk.op("act", lambda e, p=p, t0=t0, w=w: e.activation(rsb[:, t0:t0 + w], p[:, :w], AF.Ln, bias=epsT[:, 0:1], scale=1.0),
                              reads=[p, epsT], writes=[rsb])
                    mk.op("act", lambda e: e.activation(rsb[:], rsb[:], AF.Exp, scale=-0.5), reads=[rsb], writes=[rsb])
                    mk.op("dve", lambda e, acc=acc, scl=scl: e.scalar_tensor_tensor(R(acc[:]), acc[:], scl, rsb[:], op0=ALU.mult, op1=ALU.mult),
                          reads=[acc, rsb], writes=[acc])
                if DN_STOP <= 2:
                    break
                for src, dst in [(ck, ktok), (cv, vtok)]:
                    for n0 in range(0, NCH, 4):
                        nn = min(4, NCH - n0)
                        p = nwide()
                        for q_ in range(nn):
                            n = n0 + q_
                            mk.op("pe", lambda e, p=p, q_=q_, n=n, src=src: e.transpose(p[:, q_ * 128:(q_ + 1) * 128], src[:, n * 128:(n + 1) * 128], ident[:]),
                                  reads=[src, ident], writes=[p])
                        mk.op("act", lambda e, p=p, n0=n0, nn=nn, dst=dst: e.copy(dst[:, n0:n0 + nn, :], p[:, :nn * 128].rearrange("p (a b) -> p a b", b=128)),
                              reads=[p], writes=[dst])
                if DN_STOP <= 3:
                    break
                mk.dma("sp", zt[:, :L], S.z.t[h], reads=[S.z], writes=[zt])
                for d in range(2):
                    ci = d * 16 + h
                    gc = small["gc"][d]; bc = small["bc"][d]
                    mk.op("dve", lambda e, gc=gc, ci=ci: e.tensor_copy(gc[:], gC[:, :, ci]), reads=[gC], writes=[gc])
                    mk.op("dve", lambda e, bc=bc, ci=ci: e.tensor_copy(bc[:], betaC[:, :, ci]), reads=[betaC], writes=[bc])
                    mk.dma("sp", GBt[d][:], S.grow.t[ci:ci + 1, :].partition_broadcast(128), reads=[S.grow], writes=[GBt[d]])
                    mk.dma("sp", BBt[d][:], S.brow.t[ci:ci + 1, :].partition_broadcast(128), reads=[S.brow], writes=[BBt[d]])
                    bk = nbank(); p1 = bk[0]; p2 = bk[1]
                    mk.op("pe", lambda e, p1=p1, d=d, gc=gc: e.matmul(p1[:, :NCH], Umat[d][:], gc[:], start=True, stop=True), reads=[Umat[d], gc], writes=[p1])
                    mk.op("pe", lambda e, p2=p2, gc=gc: e.matmul(p2[:, :NCH], ones[:], gc[:], start=True, stop=True), reads=[ones, gc], writes=[p2])
                    Gcol = small["Gcol"][d]; eGcol = small["eGcol"][d]; kbs = small["kbs"][d]; kds = small["kds"][d]; eGl = small["eGl"][d]; tmp = small["tmp"][d]
                    mk.op("dve", lambda e, p1=p1, Gcol=Gcol: e.tensor_copy(Gcol[:], p1[:, :NCH]), reads=[p1], writes=[Gcol])
                    mk.op("act", lambda e, p1=p1, eGcol=eGcol: e.activation(eGcol[:], p1[:, :NCH], AF.Exp), reads=[p1], writes=[eGcol])
                    mk.op("dve", lambda e, kbs=kbs, bc=bc, eGcol=eGcol: e.tensor_tensor(kbs[:], bc[:], eGcol[:], op=ALU.mult), reads=[bc, eGcol], writes=[kbs])
                    mk.op("dve", lambda e, tmp=tmp, p2=p2, Gcol=Gcol: e.tensor_tensor(tmp[:], p2[:, :NCH], Gcol[:], op=ALU.subtract), reads=[p2, Gcol], writes=[tmp])
                    mk.op("act", lambda e, kds=kds, tmp=tmp: e.activation(kds[:], tmp[:], AF.Exp), reads=[tmp], writes=[kds])
                    mk.op("act", lambda e, eGl=eGl, p2=p2: e.activation(eGl[:], p2[:, :NCH], AF.Exp), reads=[p2], writes=[eGl])
                    mk.op("dve", lambda e, d=d: e.tensor_scalar(R(Sst[d][0][:]), ident[:], 0.0, None, op0=ALU.mult), reads=[ident], writes=[Sst[d][0]])

                fo = [16, 17] + list(range(16)); bo = [17, 16] + list(range(15, -1, -1))
                seq = []
                for i_ in range(NCH):
                    seq.append((0, fo[i_])); seq.append((1, bo[i_]))
                spos = {0: 0, 1: 0}
                oinit = set()

                def prep(wave):
                    cds = [(wi, d, n, (wave_base + wi) % NS) for wi, (d, n) in enumerate(wave)]
                    P = {}
                    for wi, d, n, si in cds:
                        gc = small["gc"][d]; bc = small["bc"][d]; Gcol = small["Gcol"][d]
                        kc = ck[:, n * 128:(n + 1) * 128]; qc = cq[:, n * 128:(n + 1) * 128]
                        _b0, _b1, KKp, QKp = nbank()
                        MMS = "kq"
                        GBv = GBt[d][:, n * 128:(n + 1) * 128]; BBv = BBt[d][:, n * 128:(n + 1) * 128]
                        if "k" in MMS:
                          mk.op("pe", lambda e, KKp=KKp, kc=kc: e.matmul(KKp[:], R(kc), R(kc), start=True, stop=True), reads=[ck], writes=[KKp])
                        if "q" in MMS:
                          mk.op("pe", lambda e, QKp=QKp, kc=kc, qc=qc: e.matmul(QKp[:], R(kc), R(qc), start=True, stop=True), reads=[ck, cq], writes=[QKp])
                        if PREP_STOP <= 1:
                            continue
                        dm1 = wb["dm1"][wi]; dm2 = wb["dm2"][wi]; dec = wb["dec"][wi]; dec2 = wb["dec2"][wi]; eGb = wb["eGb"][wi]; BM = wb["BM"][wi]
                        mk.op("dve", lambda e, dm1=dm1, GBv=GBv, Gcol=Gcol, n=n, d=d: e.scalar_tensor_tensor(dm1[:], GBv, Gcol[:, n:n + 1], Mm[d][:], op0=ALU.subtract, op1=ALU.add), reads=[GBt[d], Gcol, Mm[d]], writes=[dm1])
                        mk.op("dve", lambda e, dm2=dm2, GBv=GBv, Gcol=Gcol, n=n, d=d: e.scalar_tensor_tensor(dm2[:], GBv, Gcol[:, n:n + 1], Mo[d][:], op0=ALU.subtract, op1=ALU.subtract), reads=[GBt[d], Gcol, Mo[d]], writes=[dm2])
                        mk.op("act", lambda e, dec=dec, dm1=dm1: e.activation(R(dec[:]), dm1[:], AF.Exp), reads=[dm1], writes=[dec])
                        mk.op("act", lambda e, dec2=dec2, dm2=dm2: e.activation(R(dec2[:]), dm2[:], AF.Exp, scale=-1.0), reads=[dm2], writes=[dec2])
                        mk.op("act", lambda e, eGb=eGb, GBv=GBv: e.activation(R(eGb[:]), GBv, AF.Exp), reads=[GBt[d]], writes=[eGb])
                        mk.op("pool", lambda e, BM=BM, BBv=BBv: e.tensor_tensor(BM[:], BBv, offd[:], op=ALU.mult), reads=[BBt[d], offd], writes=[BM])
                        mk.op("dve", lambda e, si=si, QKp=QKp, dec=dec: e.tensor_tensor(R(aV[si][:]), QKp[:], dec[:], op=ALU.mult), reads=[QKp, dec], writes=[aV[si]])
                        mk.op("pool", lambda e, BM=BM, dec=dec: e.tensor_tensor(BM[:], BM[:], dec[:], op=ALU.mult), reads=[BM, dec], writes=[BM])
                        mk.op("pool", lambda e, dec2=dec2: e.tensor_tensor(R(dec2[:]), dec2[:], offd[:], op=ALU.mult), reads=[dec2, offd], writes=[dec2])
                        A = wb["A0"][wi]; B = wb["B0"][wi]; Pm = wb["P0"][wi]; Af = wb["Af"][wi]
                        mk.op("dve", lambda e, KKp=KKp, BM=BM: e.tensor_tensor(BM[:], KKp[:], BM[:], op=ALU.mult), reads=[KKp, BM], writes=[BM])
                        mk.op("dve", lambda e, Af=Af, KKp=KKp, bc=bc, n=n, dec2=dec2: e.scalar_tensor_tensor(Af[:], KKp[:], bc[:, n:n + 1], dec2[:], op0=ALU.mult, op1=ALU.mult), reads=[KKp, bc, dec2], writes=[Af])
                        mk.op("pool", lambda e, si=si, qc=qc, eGb=eGb: e.tensor_tensor(R(qV[si][:]), qc, eGb[:], op=ALU.mult), reads=[cq, eGb], writes=[qV[si]])
                        mk.op("pool", lambda e, B=B, BM=BM: e.tensor_tensor(R(B[:]), BM[:], bd16[:], op=ALU.mult), reads=[BM, bd16], writes=[B])
                        mk.op("pool", lambda e, A=A, Af=Af: e.tensor_tensor(R(A[:]), Af[:], bd16[:], op=ALU.mult), reads=[Af, bd16], writes=[A])
                        mk.op("pool", lambda e, Pm=Pm, B=B: e.tensor_tensor(R(Pm[:]), ident[:], B[:], op=ALU.subtract), reads=[ident, B], writes=[Pm])
                        P[wi] = [A, B, Pm]
                    if PREP_STOP <= 2:
                        return
                    NLEV = 3
                    for lev in range(1, NLEV + 1):
                        nxt = "1" if lev % 2 == 1 else "0"
                        pend = {}
                        for wi, d, n, si in cds:
                            A, B, Pm = P[wi]
                            bk = nbank()
                            Ap = bk[0]
                            mk.op("pe", lambda e, Ap=Ap, A=A, B=B: e.matmul(Ap[:], R(B[:]), R(A[:]), start=True, stop=True), reads=[A, B], writes=[Ap])
                            Bp = None
                            if lev < NLEV:
                                Bp = bk[1]
                                mk.op("pe", lambda e, Bp=Bp, A=A, B=B: e.matmul(Bp[:], R(A[:]), R(B[:]), start=True, stop=True), reads=[A, B], writes=[Bp])
                            pend[wi] = (Ap, Bp, bk)
                        for wi, d, n, si in cds:
                            Ap, Bp, bk = pend[wi]
                            ApI = wb["ApI"][wi]
                            mk.op("dve", lambda e, ApI=ApI, Ap=Ap: e.tensor_tensor(R(ApI[:]), Ap[:], ident[:], op=ALU.add), reads=[Ap, ident], writes=[ApI])
                            if lev < NLEV:
                                An = wb["A" + nxt][wi]; Bn = wb["B" + nxt][wi]
                                mk.op("act", lambda e, An=An, Ap=Ap: e.copy(R(An[:]), Ap[:]), reads=[Ap], writes=[An])
                                mk.op("act", lambda e, Bn=Bn, Bp=Bp: e.copy(R(Bn[:]), Bp[:]), reads=[Bp], writes=[Bn])
                                P[wi][0] = An; P[wi][1] = Bn
                        pend2 = {}
                        for wi, d, n, si in cds:
                            Pm = P[wi][2]; ApI = wb["ApI"][wi]
                            Pp = pend[wi][2][2]
                            mk.op("pe", lambda e, Pp=Pp, ApI=ApI, Pm=Pm: e.matmul(Pp[:], R(ApI[:]), R(Pm[:]), start=True, stop=True), reads=[ApI, Pm], writes=[Pp])
                            pend2[wi] = Pp
                        for wi, d, n, si in cds:
                            Pn = wb["P" + nxt][wi]
                            Pp = pend2[wi]
                            if wi % 2 == 0:
                                mk.op("dve", lambda e, Pn=Pn, Pp=Pp: e.tensor_copy(R(Pn[:]), Pp[:]), reads=[Pp], writes=[Pn])
                            else:
                                mk.op("act", lambda e, Pn=Pn, Pp=Pp: e.copy(R(Pn[:]), Pp[:]), reads=[Pp], writes=[Pn])
                            P[wi][2] = Pn
                    for wi, d, n, si in cds:
                        Dr = P[wi][2]; Dl = wb["dec2"][wi]
                        bk = nbank()
                        mk.op("pe", lambda e, bk=bk, Dr=Dr: e.transpose(bk[0][:], Dr[:], ident[:]), reads=[Dr, ident], writes=[bk[0]])
                        mk.op("act", lambda e, bk=bk, Dl=Dl: e.copy(R(Dl[:]), bk[0][:]), reads=[bk[0]], writes=[Dl])
                    for bl in range(3):
                        Ms = msk[bl]
                        pend = {}
                        for wi, d, n, si in cds:
                            Dr = P[wi][2]; AM = wb["eGb"][wi]; Af = wb["Af"][wi]
                            mk.op("pool", lambda e, AM=AM, Af=Af, Ms=Ms: e.tensor_tensor(R(AM[:]), Af[:], Ms[:], op=ALU.mult), reads=[Af, Ms], writes=[AM])
                            bk = nbank()
                            mk.op("pe", lambda e, bk=bk, AM=AM, Dr=Dr: e.matmul(bk[0][:], R(AM[:]), R(Dr[:]), start=True, stop=True), reads=[AM, Dr], writes=[bk[0]])
                            pend[wi] = bk
                        for wi, d, n, si in cds:
                            bk = pend[wi]; Ysb = wb["dec"][wi]
                            mk.op("act", lambda e, bk=bk, Ysb=Ysb: e.copy(R(Ysb[:]), bk[0][:]), reads=[bk[0]], writes=[Ysb])
                        for wi, d, n, si in cds:
                            bk = pend[wi]; Ysb = wb["dec"][wi]; Dl = wb["dec2"][wi]
                            mk.op("pe", lambda e, bk=bk, Dl=Dl, Ysb=Ysb: e.matmul(bk[1][:], R(Dl[:]), R(Ysb[:]), start=True, stop=True), reads=[Dl, Ysb], writes=[bk[1]])
                        for wi, d, n, si in cds:
                            bk = pend[wi]; Dr = P[wi][2]
                            Dn = wb["P1"][wi] if Dr is wb["P0"][wi] else wb["P0"][wi]
                            mk.op("dve", lambda e, bk=bk, Dr=Dr, Dn=Dn: e.tensor_tensor(R(Dn[:]), Dr[:], bk[1][:], op=ALU.subtract), reads=[Dr, bk[1]], writes=[Dn])
                            P[wi][2] = Dn
                        if bl < 2:
                            for wi, d, n, si in cds:
                                bk = pend[wi]; Dn = P[wi][2]; Dl = wb["dec2"][wi]
                                mk.op("pe", lambda e, bk=bk, Dn=Dn: e.transpose(bk[2][:], Dn[:], ident[:]), reads=[Dn, ident], writes=[bk[2]])
                                mk.op("act", lambda e, bk=bk, Dl=Dl: e.copy(R(Dl[:]), bk[2][:]), reads=[bk[2]], writes=[Dl])
                    if PREP_STOP <= 3:
                        return
                    for wi, d, n, si in cds:
                        TT = P[wi][2]
                        bc = small["bc"][d]; kbs = small["kbs"][d]
                        vb = wb["vb"][wi]; kbg = wb["kbg"][wi]
                        mk.op("dve", lambda e, vb=vb, n=n, bc=bc: e.tensor_scalar(R(vb[:]), vtok[:, n, :], bc[:, n:n + 1], None, op0=ALU.mult), reads=[vtok, bc], writes=[vb])
                        mk.op("dve", lambda e, kbg=kbg, n=n, kbs=kbs: e.tensor_scalar(R(kbg[:]), ktok[:, n, :], kbs[:, n:n + 1], None, op0=ALU.mult), reads=[ktok, kbs], writes=[kbg])
                        bk = nbank(); up = bk[0]; wp = bk[1]
                        mk.op("pe", lambda e, up=up, TT=TT, vb=vb: e.matmul(up[:], R(TT[:]), R(vb[:]), start=True, stop=True), reads=[TT, vb], writes=[up])
                        mk.op("pe", lambda e, wp=wp, TT=TT, kbg=kbg: e.matmul(wp[:], R(kbg[:]), R(TT[:]), start=True, stop=True), reads=[TT, kbg], writes=[wp])
                        mk.op("act", lambda e, si=si, up=up: e.copy(uV[si][:], up[:]), reads=[up], writes=[uV[si]])
                        mk.op("dve", lambda e, si=si, wp=wp: e.tensor_scalar(R(wV[si][:]), wp[:], -1.0, None, op0=ALU.mult), reads=[wp], writes=[wV[si]])

                def scan(wave):
                    for wi, (d, n) in enumerate(wave):
                        si = (wave_base + wi) % NS
                        kds = small["kds"][d]; eGl = small["eGl"][d]
                        Sc = Sst[d][spos[d] % 2]; Sn = Sst[d][(spos[d] + 1) % 2]; spos[d] += 1
                        kd = kdec[si % 4]; vn = vnew[si % 4]
                        mk.op("act", lambda e, kd=kd, n=n, kds=kds: e.activation(R(kd[:]), ktok[:, n, :], AF.Copy, scale=kds[:, n:n + 1]), reads=[ktok, kds], writes=[kd])
                        bk = nbank(); vp = bk[0]; sp_ = bk[1]; bk2 = nbank()
                        mk.op("pe", lambda e, vp=vp, si=si, Sc=Sc: e.matmul(vp[:], R(wV[si][:]), R(Sc[:]), start=True, stop=True), reads=[wV[si], Sc], writes=[vp])
                        mk.op("dve", lambda e, vn=vn, vp=vp, si=si: e.tensor_tensor(R(vn[:]), vp[:], uV[si][:], op=ALU.add), reads=[vp, uV[si]], writes=[vn])
                        if n < 16:
                            op_ = bk2[0]
                            mk.op("pe", lambda e, op_=op_, Sc=Sc, si=si: e.matmul(op_[:], R(Sc[:]), R(qV[si][:]), start=True, stop=False), reads=[Sc, qV[si]], writes=[op_])
                            mk.op("pe", lambda e, op_=op_, vn=vn, si=si: e.matmul(op_[:], R(vn[:]), R(aV[si][:]), start=False, stop=True), reads=[vn, aV[si]], writes=[op_])
                            if n not in oinit:
                                oinit.add(n)
                                mk.op("act", lambda e, op_=op_, n=n: e.copy(oacc[:, n * 128:(n + 1) * 128], op_[:]), reads=[op_], writes=[oaccB[n]])
                            else:
                                mk.op("dve", lambda e, op_=op_, n=n: e.tensor_tensor(oacc[:, n * 128:(n + 1) * 128], op_[:], oacc[:, n * 128:(n + 1) * 128], op=ALU.add), reads=[op_, oaccB[n]], writes=[oaccB[n]])
                        mk.op("pe", lambda e, sp_=sp_, kd=kd, vn=vn: e.matmul(sp_[:], R(kd[:]), R(vn[:]), start=True, stop=True), reads=[kd, vn], writes=[sp_])
                        mk.op("dve", lambda e, Sn=Sn, Sc=Sc, eGl=eGl, n=n, sp_=sp_: e.scalar_tensor_tensor(R(Sn[:]), Sc[:], eGl[:, n:n + 1], sp_[:], op0=ALU.mult, op1=ALU.add), reads=[Sc, eGl, sp_], writes=[Sn])

                waves = [seq[i_:i_ + W] for i_ in range(0, len(seq), W)]
                if DN_STOP <= 4:
                    break
                for wv_i, wave in enumerate(waves):
                    wave_base = wv_i * W
                    prep(wave)
                    if DN_STOP <= 5:
                        break
                    scan(wave)
                    if DN_STOP <= 6:
                        break
                if DN_STOP <= 6:
                    break
                mk.op("act", lambda e: e.activation(sqb[:, :L], oacc[:], AF.Square), reads=oaccB, writes=[sqb])
                for (t0, w) in TOK5[:4]:
                    p = nwide()
                    mk.op("pe", lambda e, p=p, t0=t0, w=w: e.matmul(p[:, :w], ones[:], sqb[:, t0:t0 + w], start=True, stop=True), reads=[ones, sqb], writes=[p])
                    mk.op("act", lambda e, p=p, t0=t0, w=w: e.activation(rsb[:, t0:t0 + w], p[:, :w], AF.Ln, bias=epsT[:, 0:1], scale=1.0 / 128.0), reads=[p, epsT], writes=[rsb])
                mk.op("act", lambda e: e.activation(rsb[:, :L], rsb[:, :L], AF.Exp, scale=-0.5), reads=[rsb], writes=[rsb])
                mk.op("dve", lambda e: e.tensor_tensor(sqb[:, :L], oacc[:], rsb[:, :L], op=ALU.mult), reads=oaccB + [rsb], writes=[sqb])
                mk.op("dve", lambda e: e.scalar_tensor_tensor(rsb[:, :1024].bitcast(BF16), sqb[:, :L], hw[:, 0:1], zt[:, :L], op0=ALU.mult, op1=ALU.mult), reads=[sqb, hw, zt], writes=[rsb])
                mk.dma("sp", S.odn.t[h], rsb[:, :1024].bitcast(BF16), reads=[rsb], writes=[S.odn])
        mk.barrier()

    g.phase_dn = phase_dn

    def phase_na(heads=range(16)):
        with ExitStack() as ps:
            pb = [pst(ps, "na%d" % i, [128, 512]) for i in range(8)]
            cnt = {"s": 0, "g": 0, "bb": 0}

            def sbank():
                cnt["s"] += 1
                return pb[cnt["s"] % 4]
            ropec = sbt(ps, "ropec", [128, L]); ropes = sbt(ps, "ropes", [128, L])
            mk.dma("sp", ropec[:], I.ropec, writes=[ropec]); mk.dma("sp", ropes[:], I.ropes, writes=[ropes])
            qraws = [sbt(ps, "qraw%d" % i, [128, L]) for i in range(2)]; kraws = [sbt(ps, "kraw%d" % i, [128, T]) for i in range(2)]
            sq = sbt(ps, "nsq", [128, T]); rs = sbt(ps, "nrs", [128, T])
            t1 = sbt(ps, "t1", [128, T]); qbf = sbt(ps, "qbf", [128, L], BF16); kbf = sbt(ps, "kbf", [128, T], BF16)
            vts = [sbt(ps, "vt%d" % i, [128, NCH, 128], BF16) for i in range(2)]
            bias = [sbt(ps, "bias%d" % i, [128, 512]) for i in range(8)]
            et = [sbt(ps, "et%d" % i, [128, 512]) for i in range(2)]
            pT = [sbt(ps, "pT%d" % i, [128, 512], BF16) for i in range(3)]
            t2 = [sbt(ps, "t2_%d" % i, [128, 512]) for i in range(2)]
            ost = sbt(ps, "nost", [128, L], BF16); rden = sbt(ps, "rden", [128, 512])
            rot = K["rot"]
            qbf2 = sbt(ps, "qbf2", [128, L], BF16); kbf2 = sbt(ps, "kbf2", [128, T], BF16)
            qbfs = [qbf, qbf2]; kbfs = [kbf, kbf2]

            def prep_stages(hi_, h):
                slot = hi_ % 2
                qraw = qraws[slot]; kraw = kraws[slot]; vt = vts[slot]

                def loads():
                    mk.dma("sp", qraw[:], S.nq.t[h], reads=[S.nq], writes=[qraw])
                    mk.dma("sp", kraw[:], S.nk.t[h], reads=[S.nk], writes=[kraw])
                    mk.dma("sp", vt[:], S.nv.t.rearrange("(n p) c -> p n c", p=128)[:, :, h * 128:(h + 1) * 128], reads=[S.nv], writes=[vt])

                def normA(raw, Wd):
                    mk.op("act", lambda e, raw=raw, Wd=Wd: e.activation(sq[:, :Wd], raw[:, :Wd], AF.Square), reads=[raw], writes=[sq])
                    for (t0, w) in TOK5:
                        if t0 >= Wd:
                            continue
                        p = sbank()
                        mk.op("pe", lambda e, p=p, t0=t0, w=w: e.matmul(p[:, :w], ones[:], sq[:, t0:t0 + w], start=True, stop=True), reads=[ones, sq], writes=[p])
                        mk.op("act", lambda e, p=p, t0=t0, w=w: e.activation(rs[:, t0:t0 + w], p[:, :w], AF.Ln, bias=epsT[:, 0:1], scale=1.0 / 128.0), reads=[p, epsT], writes=[rs])
                    mk.op("act", lambda e, Wd=Wd: e.activation(rs[:, :Wd], rs[:, :Wd], AF.Exp, scale=-0.5), reads=[rs], writes=[rs])

                def normB(raw, Wd, wc, scl, obf_):
                    mk.op("dve", lambda e, raw=raw, Wd=Wd: e.tensor_tensor(t1[:, :Wd], raw[:, :Wd], rs[:, :Wd], op=ALU.mult), reads=[raw, rs], writes=[t1])
                    mk.op("dve", lambda e, Wd=Wd, wc=wc, scl=scl: e.tensor_scalar(t1[:, :Wd], t1[:, :Wd], hw[:, wc:wc + 1], scl, op0=ALU.mult, op1=ALU.mult), reads=[t1, hw], writes=[t1])
                    for ti in range(4):
                        t0 = ti * 512
                        p = sbank(); tt = t2[ti % 2]
                        mk.op("pe", lambda e, p=p, t0=t0: e.matmul(p[:, :512], rot[:], t1[:, t0:t0 + 512], start=True, stop=True), reads=[rot, t1], writes=[p])
                        mk.op("dve", lambda e, p=p, t0=t0, tt=tt: e.tensor_tensor(tt[:], p[:, :512], ropes[:, t0:t0 + 512], op=ALU.mult), reads=[p, ropes], writes=[tt])
                        mk.op("pool", lambda e, t0=t0: e.tensor_tensor(sq[:, t0:t0 + 512], t1[:, t0:t0 + 512], ropec[:, t0:t0 + 512], op=ALU.mult), reads=[t1, ropec], writes=[sq])
                        mk.op("pool", lambda e, t0=t0, tt=tt, obf_=obf_: e.tensor_tensor(obf_[:, t0:t0 + 512], sq[:, t0:t0 + 512], tt[:], op=ALU.add), reads=[sq, tt], writes=[obf_])
                    if Wd > L:
                        mk.op("act", lambda e, obf_=obf_: e.copy(obf_[:, L:T], t1[:, L:T]), reads=[t1], writes=[obf_])

                def s0():
                    loads(); normA(qraw, L)

                def s1():
                    normB(qraw, L, 1, 128.0 ** -0.5, qbfs[slot])

                def s2():
                    normA(kraw, T)

                def s3():
                    normB(kraw, T, 2, 1.0, kbfs[slot])
                return [s0, s1, s2, s3]

            for st_ in prep_stages(0, heads[0]):
                st_()
            for hi_, h in enumerate(heads):
                slot = hi_ % 2
                qbf = qbfs[slot]; kbf = kbfs[slot]; vt = vts[slot]
                nxt = prep_stages(hi_ + 1, heads[hi_ + 1]) if hi_ + 1 < len(heads) else [None] * 4
                bi = 0
                for m, (lo, nt) in enumerate(NA_BLOCKS):
                    qs = qbf[:, m * 512:(m + 1) * 512]
                    obank = pb[4 + m % 2]; dbank = pb[6 + m % 2]
                    tiles = [("l", lo + 2 * j) for j in range(nt)] + [("c", 0), ("c", 1)]
                    for ti, (kind, kr0) in enumerate(tiles):
                        sb_ = sbank()
                        if kind == "l":
                            keys = kbf[:, kr0 * 64:kr0 * 64 + 128]; V = vt[:, kr0 // 2, :]
                        else:
                            keys = kbf[:, L + kr0 * 128:L + (kr0 + 1) * 128]; V = vt[:, 16 + kr0, :]
                        mk.op("pe", lambda e, sb_=sb_, keys=keys, qs=qs: e.matmul(sb_[:, :512], keys, qs, start=True, stop=True), reads=[kbf, qbf], writes=[sb_])
                        cnt["g"] += 1
                        pt_ = pT[cnt["g"] % 3]
                        if kind == "l":
                            bt = bias[cnt["bb"] % 8]; cnt["bb"] += 1; e_ = et[cnt["g"] % 2]
                            mk.dma("sp", bt[:], I.nab[h, bi], writes=[bt])
                            bi += 1
                            mk.op("dve", lambda e, e_=e_, sb_=sb_, bt=bt: e.tensor_tensor(e_[:], sb_[:, :512], bt[:], op=ALU.add), reads=[sb_, bt], writes=[e_])
                            mk.op("act", lambda e, pt_=pt_, e_=e_: e.activation(pt_[:], e_[:], AF.Exp), reads=[e_], writes=[pt_])
                        else:
                            mk.op("act", lambda e, pt_=pt_, sb_=sb_: e.activation(pt_[:], sb_[:, :512], AF.Exp), reads=[sb_], writes=[pt_])
                        first = (ti == 0); last = (ti == len(tiles) - 1)
                        mk.op("pe", lambda e, obank=obank, V=V, pt_=pt_, first=first, last=last: e.matmul(obank[:, :512], V, pt_[:], start=first, stop=last), reads=[vt, pt_], writes=[obank])
                        mk.op("pe", lambda e, dbank=dbank, pt_=pt_, first=first, last=last: e.matmul(dbank[:, :512], onesb[:], pt_[:], start=first, stop=last), reads=[onesb, pt_], writes=[dbank])
                    mk.op("act", lambda e, dbank=dbank: e.activation(rden[:], dbank[:, :512], AF.Ln), reads=[dbank], writes=[rden])
                    mk.op("act", lambda e: e.activation(rden[:], rden[:], AF.Exp, scale=-1.0), reads=[rden], writes=[rden])
                    mk.op("dve", lambda e, obank=obank, m=m: e.tensor_tensor(ost[:, m * 512:(m + 1) * 512], obank[:, :512], rden[:], op=ALU.mult), reads=[obank, rden], writes=[ost])
                    if nxt[m] is not None:
                        nxt[m]()
                mk.dma("pool", S.ona.t[h], ost[:], reads=[ost], writes=[S.ona])
        mk.barrier()

    g.phase_na = phase_na
    a2 = X["a2"]; norm_mod = X["norm_mod"]

    def make_wloader(ps, nbuf=4):
        stg = [sbt(ps, "stg%d" % i, [128, 4096]) for i in range(2)]
        wbf = [sbt(ps, "wld%d" % i, [128, 4096], BF16) for i in range(nbuf)]
        st = {"i": 0}

        def load(view, a, b):
            i = st["i"]; st["i"] += 1
            s_ = stg[i % 2]; w_ = wbf[i % nbuf]
            mk.dma("sp", s_[:].rearrange("p (a b) -> p a b", b=b), view, writes=[s_])
            mk.op("act", lambda e: e.copy(w_[:, 0:2048], s_[:, 0:2048]), reads=[s_], writes=[w_])
            mk.op("dve", lambda e: e.tensor_copy(w_[:, 2048:4096], s_[:, 2048:4096]), reads=[s_], writes=[w_])
            return w_
        return load

    def pipelined(n, loadfn, computefn):
        cur = loadfn(0)
        for i in range(n):
            nxt = loadfn(i + 1) if i + 1 < n else None
            computefn(i, cur)
            cur = nxt

    def phase_merge():
        with ExitStack() as ps:
            pb = [pst(ps, "mg%d" % i, [128, 512]) for i in range(8)]
            cnt = {"b": 0}

            def bank():
                cnt["b"] += 1
                return pb[cnt["b"] % 8]
            load = make_wloader(ps, nbuf=6)
            odn = sbt(ps, "odn", [128, 16, 1024], BF16); ona = sbt(ps, "ona", [128, 16, 1024], BF16)
            sa = [sbt(ps, "sa%d" % i, [128, 1024]) for i in range(2)]; sb_ = [sbt(ps, "sbb%d" % i, [128, 1024]) for i in range(2)]
            ta = [sbt(ps, "ta%d" % i, [128, 512]) for i in range(2)]; tb = [sbt(ps, "tb%d" % i, [128, 512]) for i in range(2)]
            yst = [sbt(ps, "yst%d" % i, [128, 1024], BF16) for i in range(2)]
            wav = I.w_a.rearrange("(k p) n -> p k n", p=128); wbv = I.w_b.rearrange("(k p) n -> p k n", p=128)
            for half in range(2):
                th0 = half * 1024
                mk.dma("sp", odn[:], S.odn.t.rearrange("h p t -> p h t")[:, :, th0:th0 + 1024], reads=[S.odn], writes=[odn])
                mk.dma("sp", ona[:], S.ona.t.rearrange("h p t -> p h t")[:, :, th0:th0 + 1024], reads=[S.ona], writes=[ona])

                def ld(cb):
                    return (load(wav[:, :, cb * 256:(cb + 1) * 256], 16, 256), load(wbv[:, :, cb * 256:(cb + 1) * 256], 16, 256))

                def comp(cb, ws):
                    wa_, wb_ = ws
                    for ct in range(2):
                        dt = cb * 2 + ct
                        sA = sa[dt % 2]; sB = sb_[dt % 2]; ys = yst[dt % 2]
                        mk.dma("sp", sA[:], S.ga.t[dt][:, th0:th0 + 1024], reads=[S.ga], writes=[sA])
                        mk.dma("sp", sB[:], S.gb.t[dt][:, th0:th0 + 1024], reads=[S.gb], writes=[sB])
                        for tq in range(2):
                            t0 = tq * 512
                            pA = bank(); pB = bank()
                            for k in range(16):
                                mk.op("pe", lambda e, pA=pA, k=k, ct=ct, t0=t0: e.matmul(pA[:, :512], wa_[:, k * 256 + ct * 128:k * 256 + (ct + 1) * 128], odn[:, k, t0:t0 + 512], start=(k == 0), stop=(k == 15)), reads=[wa_, odn], writes=[pA])
                            for k in range(16):
                                mk.op("pe", lambda e, pB=pB, k=k, ct=ct, t0=t0: e.matmul(pB[:, :512], wb_[:, k * 256 + ct * 128:k * 256 + (ct + 1) * 128], ona[:, k, t0:t0 + 512], start=(k == 0), stop=(k == 15)), reads=[wb_, ona], writes=[pB])
                            a_ = ta[tq]; b_ = tb[tq]
                            mk.op("dve", lambda e, a_=a_, pA=pA, sA=sA, t0=t0: e.tensor_tensor(a_[:], pA[:, :512], sA[:, t0:t0 + 512], op=ALU.mult), reads=[pA, sA], writes=[a_])
                            mk.op("dve", lambda e, b_=b_, pB=pB, sB=sB, t0=t0: e.tensor_tensor(b_[:], pB[:, :512], sB[:, t0:t0 + 512], op=ALU.mult), reads=[pB, sB], writes=[b_])
                            mk.op("pool", lambda e, a_=a_, b_=b_, ys=ys, t0=t0: e.tensor_tensor(ys[:, t0:t0 + 512], a_[:], b_[:], op=ALU.add), reads=[a_, b_], writes=[ys])
                        mk.dma("sp", S.y.t[dt][:, th0:th0 + 1024], ys[:], reads=[ys], writes=[S.y])
                pipelined(8, ld, comp)
            mk.barrier()
            yT = odn
            xt = [sbt(ps, "mxt%d" % i, [128, 1024]) for i in range(2)]
            xst = [sbt(ps, "mxs%d" % i, [128, 1024]) for i in range(2)]
            wov = I.w_out.rearrange("(k p) n -> p k n", p=128)
            for half in range(2):
                th0 = half * 1024
                mk.dma("sp", yT[:], S.y.t.rearrange("h p t -> p h t")[:, :, th0:th0 + 1024], reads=[S.y], writes=[yT])

                def ld2(cb):
                    return load(wov[:, :, cb * 256:(cb + 1) * 256], 16, 256)

                def comp2(cb, wo_):
                    for ct in range(2):
                        dt = cb * 2 + ct
                        x_ = xt[dt % 2]; xs = xst[dt % 2]
                        mk.dma("sp", x_[:], I.xT[dt][:, th0:th0 + 1024], writes=[x_])
                        for tq in range(2):
                            t0 = tq * 512
                            p = bank()
                            for k in range(16):
                                mk.op("pe", lambda e, p=p, k=k, ct=ct, t0=t0: e.matmul(p[:, :512], wo_[:, k * 256 + ct * 128:k * 256 + (ct + 1) * 128], yT[:, k, t0:t0 + 512], start=(k == 0), stop=(k == 15)), reads=[wo_, yT], writes=[p])
                            mk.op("dve", lambda e, p=p, xs=xs, x_=x_, dt=dt, t0=t0: e.scalar_tensor_tensor(xs[:, t0:t0 + 512], p[:, :512], modsb[:, 32 + dt, 0:1], x_[:, t0:t0 + 512], op0=ALU.mult, op1=ALU.add), reads=[p, modsb, x_], writes=[xs])
                        mk.dma("sp", S.x1.t[dt][:, th0:th0 + 1024], xs[:], reads=[xs], writes=[S.x1])
                pipelined(8, ld2, comp2)
        mk.barrier()

    g.phase_merge = phase_merge

    def phase_moe():
        with ExitStack() as ps:
            pb = [pst(ps, "me%d" % i, [128, 512]) for i in range(6)]
            pbt = [pst(ps, "met%d" % i, [128, 1024], BF16) for i in range(2)]
            cnt = {"b": 0, "t": 0}

            def bank():
                cnt["b"] += 1
                return pb[cnt["b"] % 6]
            h2tok = sbt(ps, "h2tok", [128, 16, D], BF16)
            logits = sbt(ps, "logits", [128, 16, 16])
            wr = sbt(ps, "wr", [128, 16, 16])
            iota = sbt(ps, "iota", [128, 258])
            mk.dma("sp", wr[:], I.w_r.rearrange("(k p) e -> p k e", p=128), writes=[wr])
            mk.dma("sp", iota[:], I.iota, writes=[iota])
            affT = sbt(ps, "affT", [16, L]); work = sbt(ps, "work", [16, L]); rkmT = sbt(ps, "rkmT", [16, L]); gwT = sbt(ps, "gwT", [16, L])
            rkm = sbt(ps, "rkm", [128, 16, 16])
            with ExitStack() as p1:
                xts = [sbt(p1, "mx%d" % i, [128, 16, 512]) for i in range(1)]
                sq = sbt(p1, "msq", [128, 16, 512]); rs = sbt(p1, "mrs", [128, 512])
                tmp2 = [sbt(p1, "mtmp%d" % i, [128, 512]) for i in range(2)]
                h2f = sq; h2b = sbt(p1, "h2b", [128, 16, 512], BF16)
                xv = S.x1.t.rearrange("k p t -> p k t")
                for tt in range(4):
                    t0 = tt * 512
                    xt = xts[0]
                    mk.dma("sp", xt[:], xv[:, :, t0:t0 + 512], reads=[S.x1], writes=[xt])
                    norm_mod(bank(), xt, sq, rs, tmp2, 512, a2, (lambda k: modsb[:, 48 + k, 0:1]), lambda k: (h2f[:, k, :], h2f))
                    for tc in range(4):
                        p = bank()
                        for k in range(16):
                            mk.op("pe", lambda e, p=p, k=k, tc=tc: e.matmul(p[:, :16], h2f[:, k, tc * 128:(tc + 1) * 128], wr[:, k, :], start=(k == 0), stop=(k == 15)), reads=[h2f, wr], writes=[p])
                        mk.op("dve", lambda e, p=p, tt=tt, tc=tc: e.tensor_copy(logits[:, tt * 4 + tc, :], p[:, :16]), reads=[p], writes=[logits])
                    mk.op("act", lambda e: e.copy(h2b[:], h2f[:]), reads=[h2f], writes=[h2b])
                    for tc in range(4):
                        for kg in range(2):
                            cnt["t"] += 1
                            pt = pbt[cnt["t"] % 2]
                            for kk in range(8):
                                k = kg * 8 + kk
                                mk.op("pe", lambda e, pt=pt, kk=kk, k=k, tc=tc: e.transpose(pt[:, kk * 128:(kk + 1) * 128], h2b[:, k, tc * 128:(tc + 1) * 128], identb[:]), reads=[h2b, identb], writes=[pt])
                            eng = "dve" if (tc + kg) % 2 == 0 else "act"
                            dst = h2tok[:, tt * 4 + tc, kg * 1024:(kg + 1) * 1024]
                            if eng == "dve":
                                mk.op("dve", lambda e, pt=pt, dst=dst: e.tensor_copy(dst, pt[:]), reads=[pt], writes=[h2tok])
                            else:
                                mk.op("act", lambda e, pt=pt, dst=dst: e.copy(dst, pt[:]), reads=[pt], writes=[h2tok])
                mx = sbt(p1, "mx_", [128, 16]); sm = sbt(p1, "sm_", [128, 16])
                for tc in range(16):
                    mk.op("dve", lambda e, tc=tc: e.reduce_max(mx[:, tc:tc + 1], logits[:, tc, :], axis=AX.X), reads=[logits], writes=[mx])
                mk.op("dve", lambda e: e.tensor_scalar(mx[:], mx[:], -1.0, None, op0=ALU.mult), reads=[mx], writes=[mx])
                for tc in range(16):
                    mk.op("act", lambda e, tc=tc: e.activation(logits[:, tc, :], logits[:, tc, :], AF.Exp, bias=mx[:, tc:tc + 1], scale=1.0, accum_out=sm[:, tc:tc + 1]), reads=[logits, mx], writes=[logits, sm])
                mk.op("dve", lambda e: e.reciprocal(sm[:], sm[:]), reads=[sm], writes=[sm])
                for tc in range(16):
                    mk.op("dve", lambda e, tc=tc: e.tensor_scalar(logits[:, tc, :], logits[:, tc, :], sm[:, tc:tc + 1], None, op0=ALU.mult), reads=[logits, sm], writes=[logits])
                for g4 in range(4):
                    p = bank()
                    for q_ in range(4):
                        tc = g4 * 4 + q_
                        mk.op("pe", lambda e, p=p, q_=q_, tc=tc: e.transpose(p[0:16, q_ * 128:(q_ + 1) * 128], logits[:, tc, :], ident[:]), reads=[logits, ident], writes=[p])
                    mk.op("dve", lambda e, p=p, g4=g4: e.tensor_copy(affT[:, g4 * 512:(g4 + 1) * 512], p[0:16, :512]), reads=[p], writes=[affT])
                mk.op("dve", lambda e: e.tensor_copy(work[:], affT[:]), reads=[affT], writes=[work])
                m8 = sbt(p1, "m8", [16, 8])
                for r_ in range(32):
                    mk.op("dve", lambda e: e.max(m8[:], work[:]), reads=[work], writes=[m8])
                    if r_ < 31:
                        mk.op("dve", lambda e: e.match_replace(work[:], m8[:], work[:], -1.0), reads=[work, m8], writes=[work])
                mk.op("dve", lambda e: e.tensor_scalar(work[:], affT[:], m8[:, 7:8], None, op0=ALU.is_ge), reads=[affT, m8], writes=[work])
                mk.op("dve", lambda e: e.tensor_tensor(gwT[:], affT[:], work[:], op=ALU.mult), reads=[affT, work], writes=[gwT])
                onesr = sbt(p1, "onesr", [16, L])
                mk.op("pool", lambda e: e.memset(onesr[:], 1.0), writes=[onesr])
                mk.op("dve", lambda e: e.tensor_tensor_scan(rkmT[:], onesr[:], work[:], 0.0, op0=ALU.mult, op1=ALU.add), reads=[onesr, work], writes=[rkmT])
                mk.op("dve", lambda e: e.tensor_tensor(rkmT[:], rkmT[:], work[:], op=ALU.mult), reads=[rkmT, work], writes=[rkmT])
                mk.op("dve", lambda e: e.tensor_scalar(rkmT[:], rkmT[:], -1.0, None, op0=ALU.add), reads=[rkmT], writes=[rkmT])
                for g4 in range(4):
                    p = bank()
                    for q_ in range(4):
                        tc = g4 * 4 + q_
                        mk.op("pe", lambda e, p=p, q_=q_, tc=tc: e.transpose(p[:, q_ * 16:(q_ + 1) * 16], rkmT[:, tc * 128:(tc + 1) * 128], ident[0:16, 0:16]), reads=[rkmT, ident], writes=[p])
                    mk.op("dve", lambda e, p=p, g4=g4: e.tensor_copy(rkm[:, g4 * 4:(g4 + 1) * 4, :], p[:, :64].rearrange("p (a b) -> p a b", b=16)), reads=[p], writes=[rkm])
            mk.barrier()
            with ExitStack() as p2:
                load = make_wloader(p2, nbuf=4)
                sel = [sbt(p2, "sel%d" % i, [128, 16, 256], BF16) for i in range(2)]
                xg = sbt(p2, "xg", [128, 16, 256], BF16)
                hid = sbt(p2, "hid", [128, 8, 256], BF16)
                st_ = [sbt(p2, "sil%d" % i, [128, 256]) for i in range(2)]
                yest = [sbt(p2, "yest%d" % i, [128, D], BF16) for i in range(2)]
                for ex in range(16):
                    sl_ = sel[ex % 2]
                    for tc in range(16):
                        eng = "dve"
                        mk.op(eng, lambda e, sl_=sl_, tc=tc, ex=ex: e.tensor_scalar(sl_[:, tc, :], iota[:, 0:256], rkm[:, tc, ex:ex + 1], None, op0=ALU.is_equal), reads=[iota, rkm], writes=[sl_])
                    for Dc in range(16):
                        p = bank()
                        for tc in range(16):
                            mk.op("pe", lambda e, p=p, tc=tc, Dc=Dc, sl_=sl_: e.matmul(p[:, :256], h2tok[:, tc, Dc * 128:(Dc + 1) * 128], sl_[:, tc, :], start=(tc == 0), stop=(tc == 15)), reads=[h2tok, sl_], writes=[p])
                        if Dc % 2 == 0:
                            mk.op("dve", lambda e, p=p, Dc=Dc: e.tensor_copy(xg[:, Dc, :], p[:, :256]), reads=[p], writes=[xg])
                        else:
                            mk.op("act", lambda e, p=p, Dc=Dc: e.copy(xg[:, Dc, :], p[:, :256]), reads=[p], writes=[xg])
                    w1v = I.ew1[ex].rearrange("(k p) f -> p k f", p=128); w3v = I.ew3[ex].rearrange("(k p) f -> p k f", p=128)

                    def ld(fb, w1v=w1v, w3v=w3v):
                        return (load(w1v[:, :, fb * 256:(fb + 1) * 256], 16, 256), load(w3v[:, :, fb * 256:(fb + 1) * 256], 16, 256))

                    def comp(fb, ws):
                        w1_, w3_ = ws
                        for ft in range(2):
                            p1_ = bank(); p3_ = bank()
                            for k in range(16):
                                mk.op("pe", lambda e, p1_=p1_, k=k, ft=ft: e.matmul(p1_[:, :256], w1_[:, k * 256 + ft * 128:k * 256 + (ft + 1) * 128], xg[:, k, :], start=(k == 0), stop=(k == 15)), reads=[w1_, xg], writes=[p1_])
                            for k in range(16):
                                mk.op("pe", lambda e, p3_=p3_, k=k, ft=ft: e.matmul(p3_[:, :256], w3_[:, k * 256 + ft * 128:k * 256 + (ft + 1) * 128], xg[:, k, :], start=(k == 0), stop=(k == 15)), reads=[w3_, xg], writes=[p3_])
                            s_ = st_[ft]
                            mk.op("act", lambda e, s_=s_, p1_=p1_: e.activation(s_[:], p1_[:, :256], AF.Silu), reads=[p1_], writes=[s_])
                            mk.op("dve", lambda e, s_=s_, p3_=p3_, fb=fb, ft=ft: e.tensor_tensor(hid[:, fb * 2 + ft, :], s_[:], p3_[:, :256], op=ALU.mult), reads=[s_, p3_], writes=[hid])
                    pipelined(4, ld, comp)
                    w2v = I.ew2[ex].rearrange("(k p) n -> p k n", p=128)

                    def ld2(nb, w2v=w2v):
                        return load(w2v[:, :, nb * 512:(nb + 1) * 512], 8, 512)

                    def comp2(nb, w2_):
                        for s2 in range(2):
                            p = bank(); ys = yest[s2]
                            for f in range(8):
                                mk.op("pe", lambda e, p=p, f=f, s2=s2: e.matmul(p[:, :512], hid[:, f, s2 * 128:(s2 + 1) * 128], w2_[:, f * 512:(f + 1) * 512], start=(f == 0), stop=(f == 7)), reads=[hid, w2_], writes=[p])
                            if s2 == 0:
                                mk.op("dve", lambda e, p=p, ys=ys, nb=nb: e.tensor_copy(ys[:, nb * 512:(nb + 1) * 512], p[:, :512]), reads=[p], writes=[ys])
                            else:
                                mk.op("act", lambda e, p=p, ys=ys, nb=nb: e.copy(ys[:, nb * 512:(nb + 1) * 512], p[:, :512]), reads=[p], writes=[ys])
                    pipelined(4, ld2, comp2)
                    for s2 in range(2):
                        mk.dma("sp", S.ye.t[ex * 2 + s2], yest[s2][:], reads=[yest[s2]], writes=[S.ye])
            mk.barrier()
            with ExitStack() as p3:
                selT = sbt(p3, "selT", [16, 16, 128])
                for ex in range(16):
                    mk.op("dve", lambda e, ex=ex: e.tensor_copy(selT[:, ex, :], ident[0:16, ex:ex + 1].to_broadcast([16, 128])), reads=[ident], writes=[selT])
                SGT = sbt(p3, "SGT", [128, 32, 512], BF16)
                gwB = [sbt(p3, "gwB%d" % i, [128, 512]) for i in range(2)]
                yeD = [sbt(p3, "yeD%d" % i, [128, 32, 128], BF16) for i in range(2)]
                x1t = [sbt(p3, "x1t%d" % i, [128, 512]) for i in range(2)]
                ot = [sbt(p3, "ot%d" % i, [128, 512]) for i in range(2)]
                yev = S.ye.t.rearrange("q p d -> p q d")
                for tt in range(4):
                    t0 = tt * 512
                    for ex in range(16):
                        pR = bank(); pG = bank(); gb_ = gwB[ex % 2]
                        mk.op("pe", lambda e, pR=pR, ex=ex, t0=t0: e.matmul(pR[:, :512], selT[:, ex, :], rkmT[:, t0:t0 + 512], start=True, stop=True), reads=[selT, rkmT], writes=[pR])
                        mk.op("pe", lambda e, pG=pG, ex=ex, t0=t0: e.matmul(pG[:, :512], selT[:, ex, :], gwT[:, t0:t0 + 512], start=True, stop=True), reads=[selT, gwT], writes=[pG])
                        mk.op("act", lambda e, gb_=gb_, pG=pG: e.copy(gb_[:], pG[:, :512]), reads=[pG], writes=[gb_])
                        for s2 in range(2):
                            mk.op("dve", lambda e, pR=pR, gb_=gb_, ex=ex, s2=s2: e.scalar_tensor_tensor(SGT[:, ex * 2 + s2, :], pR[:, :512], iota[:, 256 + s2:257 + s2], gb_[:], op0=ALU.is_equal, op1=ALU.mult), reads=[pR, iota, gb_], writes=[SGT])
                    for Dc in range(16):
                        yd = yeD[Dc % 2]; x_ = x1t[Dc % 2]; o_ = ot[Dc % 2]
                        mk.dma("sp", yd[:], yev[:, :, Dc * 128:(Dc + 1) * 128], reads=[S.ye], writes=[yd])
                        mk.dma("sp", x_[:], S.x1.t[Dc][:, t0:t0 + 512], reads=[S.x1], writes=[x_])
                        p = bank()
                        for q_ in range(32):
                            mk.op("pe", lambda e, p=p, q_=q_, yd=yd: e.matmul(p[:, :512], yd[:, q_, :], SGT[:, q_, :], start=(q_ == 0), stop=(q_ == 31)), reads=[yd, SGT], writes=[p])
                        mk.op("dve", lambda e, p=p, o_=o_, x_=x_, Dc=Dc: e.scalar_tensor_tensor(o_[:], p[:, :512], modsb[:, 80 + Dc, 0:1], x_[:], op0=ALU.mult, op1=ALU.add), reads=[p, modsb, x_], writes=[o_])
                        mk.dma("sp", outT.t[Dc][:, t0:t0 + 512], o_[:], reads=[o_], writes=[outT])
        mk.barrier()

    g.phase_moe = phase_moe


def prep_shared(inp):
    f = np.float32
    sh = {}
    sh["ada_w"] = np.ascontiguousarray(inp["ada_w"][0], f)
    sh["ada_b"] = np.ascontiguousarray(inp["ada_b"][0].reshape(96, 128).T, f)
    sh["n1w"] = np.ascontiguousarray(inp["norm1_w"][0].reshape(16, 128).T, f)
    sh["n2w"] = np.ascontiguousarray(inp["norm2_w"][0].reshape(16, 128).T, f)
    sh["w_in"] = np.ascontiguousarray(inp["w_in"][0], f)
    sh["conv_w"] = np.ascontiguousarray(inp["conv_w"][0].T.reshape(48, 128, 5).transpose(1, 0, 2), f)
    sc = np.concatenate([inp["dn_a_log"][0].reshape(-1), inp["dn_dt_bias"][0].reshape(-1)]).astype(f)
    sh["dn_sc"] = np.ascontiguousarray(np.broadcast_to(sc[None, :], (128, 64)), f)
    sh["hw"] = np.ascontiguousarray(np.stack([inp["dn_norm_w"][0], inp["na_q_norm_w"][0], inp["na_k_norm_w"][0]], axis=1), f)
    c = _consts()
    sh["consts"] = np.ascontiguousarray(np.stack([c[n] for n in CONST_NAMES]), f)
    sh["ropec"], sh["ropes"] = _rope_tables()
    sh["nab"] = _na_bias_table(np.asarray(inp["na_rpb"][0], f))
    io = np.zeros((128, 258), f)
    io[:, :256] = np.arange(256, dtype=f)[None, :]
    io[:, 256] = np.arange(128, dtype=f)
    io[:, 257] = np.arange(128, dtype=f) + 128
    sh["iota"] = io
    ii = np.arange(128)
    mks = [(ii[:, None] // 16 == ii[None, :] // 16)]
    for b_ in (16, 32, 64):
        same = (ii[:, None] // (2 * b_) == ii[None, :] // (2 * b_))
        hi = (ii % (2 * b_)) >= b_
        mks.append(same & (hi[:, None] != hi[None, :]))
    sh["dnmask"] = np.ascontiguousarray(np.stack(mks).astype(f))
    sh["w_a"] = np.ascontiguousarray(inp["w_branch_a"][0], f)
    sh["w_b"] = np.ascontiguousarray(inp["w_branch_b"][0], f)
    sh["w_out"] = np.ascontiguousarray(inp["w_out"][0], f)
    sh["w_r"] = np.ascontiguousarray(inp["w_router"][0], f)
    sh["ew1"] = np.ascontiguousarray(inp["expert_w1"][0], f)
    sh["ew3"] = np.ascontiguousarray(inp["expert_w3"][0], f)
    sh["ew2"] = np.ascontiguousarray(inp["expert_w2"][0], f)
    return sh


def prep_core(inp, b):
    f = np.float32
    m = {}
    xc = np.concatenate([inp["x"][b], inp["ctx"][b]], axis=0)
    m["xT"] = np.ascontiguousarray(xc.T.reshape(16, 128, T), f)
    cs = np.stack([inp["c"][b].reshape(16, 128), inp["c_ctx"].reshape(16, 128)], axis=-1)
    m["cs"] = np.ascontiguousarray(cs.transpose(1, 0, 2).reshape(128, 32), f)
    return m


_CACHE = {}


def kernel(**inputs):
    inp = {k: np.asarray(v) for k, v in inputs.items()}
    if "nc" not in _CACHE:
        _CACHE["nc"] = build_nc()[0]
    nc = _CACHE["nc"]
    sh = prep_shared(inp)
    in_maps = []
    for b in range(8):
        m = dict(sh)
        m.update(prep_core(inp, b))
        in_maps.append(m)
    res = run_bass_kernel_spmd(nc, in_maps, core_ids=list(range(8)))
    out = np.stack([np.asarray(r["outT"], np.float32).reshape(D, L).T for r in res.results], axis=0)
    return np.ascontiguousarray(out, np.float32)
```

```python
import os
import numpy as np
from contextlib import ExitStack
import ml_dtypes
import concourse.bass as bass
import concourse.mybir as mybir
from concourse.bass_utils import run_bass_kernel_spmd

F32 = mybir.dt.float32
BF16 = mybir.dt.bfloat16
F32R = mybir.dt.float32r
ALU = mybir.AluOpType
AF = mybir.ActivationFunctionType
AX = mybir.AxisListType

D = 2048
L = 2048
CTX = 256
T = L + CTX
INW = 18496
NCH = T // 128
EPS = 1e-6
SEM_ROLL = 30000


class Buf:
    __slots__ = ("name", "lw", "rd", "excl")

    def __init__(self, name=""):
        self.name = name
        self.lw = None
        self.rd = []
        self.excl = False


class TL:
    def __init__(self, t, name=""):
        self.t = t
        self.b = Buf(name)

    def __getitem__(self, k):
        return self.t[k]


def _b(x):
    return x.b if isinstance(x, TL) else x


class MK:
    ENG = ("pe", "act", "dve", "pool", "sp")

    def __init__(self, nc, es):
        self.nc = nc
        self.es = es
        self.eo = {"pe": nc.tensor, "act": nc.scalar, "dve": nc.vector, "pool": nc.gpsimd, "sp": nc.sync}
        self.cnt = {e: 0 for e in self.ENG}
        self.semi = 0
        self.cur = {}
        for e in self.ENG:
            self.cur[e] = self._newsem()
        self.seen = {e: {} for e in self.ENG}
        self.dsem = {}
        self.dpos = {}
        for q in ("sp", "act", "pool"):
            n = 16 if q == "sp" else 4
            self.dsem[q] = [[self._newsem(), 0, None] for _ in range(n)]
            self.dpos[q] = 0
        self.ninst = 0

    def _newsem(self):
        self.semi += 1
        return self.es.enter_context(self.nc.semaphore("s%d" % self.semi))

    def _wait(self, e, ev):
        if ev is None:
            return
        sem, val, src = ev
        if src == e and e == "pe":
            return
        key = id(sem)
        if self.seen[e].get(key, 0) >= val:
            return
        self.seen[e][key] = val
        self.eo[e].wait_ge(sem, val)

    def _deps(self, e, reads, writes):
        for b in reads:
            self._wait(e, b.lw)
        for b in writes:
            self._wait(e, b.lw)
            for r in b.rd:
                self._wait(e, r)

    def _commit(self, ev, reads, writes):
        for b in writes:
            b.lw = ev
            b.rd = []
        for b in reads:
            if b not in writes:
                b.rd.append(ev)
                if len(b.rd) > 16:
                    d = {}
                    for r in b.rd:
                        k = id(r[0])
                        if k not in d or d[k][1] < r[1]:
                            d[k] = r
                    b.rd = list(d.values())

    def op(self, e, fn, reads=(), writes=()):
        reads = [_b(x) for x in reads]
        writes = [_b(x) for x in writes]
        writes = writes + [b for b in reads if b.excl and b not in writes]
        reads = [b for b in reads if not b.excl]
        self._deps(e, reads, writes)
        if self.cnt[e] >= SEM_ROLL:
            self.cur[e] = self._newsem()
            self.cnt[e] = 0
        self.cnt[e] += 1
        ev = (self.cur[e], self.cnt[e], e)
        fn(self.eo[e]).then_inc(ev[0], 1)
        self._commit(ev, reads, writes)
        self.ninst += 1
        return ev

    def dma(self, q, out, in_, reads=(), writes=(), **kw):
        reads = [_b(x) for x in reads]
        writes = [_b(x) for x in writes]
        self._deps(q, reads, writes)
        slot = self.dsem[q][self.dpos[q]]
        self.dpos[q] = (self.dpos[q] + 1) % len(self.dsem[q])
        if slot[2] is not None:
            self._wait(q, slot[2])
        slot[1] += 16
        ev = (slot[0], slot[1], "dma")
        slot[2] = ev
        self.eo[q].dma_start(out=out, in_=in_, **kw).then_inc(slot[0], 16)
        self._commit(ev, reads, writes)
        self.ninst += 1
        return ev

    def barrier(self):
        evs = []
        for e in self.ENG:
            if self.cnt[e] > 0:
                evs.append((self.cur[e], self.cnt[e], "x"))
        for q in self.dsem:
            for slot in self.dsem[q]:
                if slot[2] is not None:
                    evs.append(slot[2])
        for e in self.ENG:
            for ev in evs:
                self._wait(e, ev)


class Ctx:
    pass


_UID = [0]


def _alloc(nc, stack, kind, name, shape, dt):
    f = nc.sbuf_tensor if kind == "sb" else nc.psum_tensor
    _UID[0] += 1
    name = "%s_%s_%d" % (kind, name, _UID[0])
    tl = TL(stack.enter_context(f(name, list(shape), dt)), name)
    if kind == "ps":
        tl.b.excl = True
    return tl


def _consts():
    c = {}
    i = np.arange(128)
    c["ident"] = np.eye(128, dtype=np.float32)
    c["ones"] = np.ones((128, 128), np.float32)
    c["uf"] = (i[:, None] <= i[None, :]).astype(np.float32)
    c["ub"] = (i[:, None] >= i[None, :]).astype(np.float32)
    NEG = -1.0e6
    c["mf"] = np.where(i[None, :] >= i[:, None], 0.0, NEG).astype(np.float32)
    c["mb"] = np.where(i[None, :] <= i[:, None], 0.0, NEG).astype(np.float32)
    c["offd"] = (1.0 - np.eye(128)).astype(np.float32)
    R = np.zeros((128, 128), np.float32)
    for p in range(128):
        q = p + 32 if (p % 64) < 32 else p - 32
        R[q, p] = 1.0
    c["rot"] = R
    return c


CONST_NAMES = ["ident", "ones", "uf", "ub", "mf", "mb", "offd", "rot"]


def _rope_tables():
    pos = np.arange(L)
    row = (pos // 64).astype(np.float32)
    col = (pos % 64).astype(np.float32)
    half = 64
    inv = (np.float32(10000.0) ** (-np.arange(0, half, 2, dtype=np.float32) / np.float32(half))).astype(np.float32)
    ang_r = row[:, None] * inv[None, :]
    ang_c = col[:, None] * inv[None, :]
    cr, sr, cc, sc = np.cos(ang_r), np.sin(ang_r), np.cos(ang_c), np.sin(ang_c)
    COS = np.concatenate([cr, cr, cc, cc], axis=1).T.astype(np.float32)
    SIN = np.concatenate([-sr, sr, -sc, sc], axis=1).T.astype(np.float32)
    return np.ascontiguousarray(COS), np.ascontiguousarray(SIN)


NA_BLOCKS = [(0, 6), (4, 8), (12, 8), (20, 6)]


def _na_bias_table(rpb):
    H = rpb.shape[0]
    out = np.full((H, 28, 128, 512), -30000.0, np.float32)
    ti = 0
    qq = np.arange(512)
    qr_l, qc = qq // 64, qq % 64
    kk = np.arange(128)
    kr_l, kc = kk // 64, kk % 64
    qstart = np.clip(qc - 8, 0, 48)
    for m, (lo, nt) in enumerate(NA_BLOCKS):
        qr = 8 * m + qr_l
        rs = np.clip(qr - 4, 0, 24)
        for j in range(nt):
            kr = lo + 2 * j + kr_l
            okr = (kr[:, None] >= rs[None, :]) & (kr[:, None] < rs[None, :] + 8) & (kr[:, None] < 32)
            okc = (kc[:, None] >= qstart[None, :]) & (kc[:, None] < qstart[None, :] + 16)
            ok = okr & okc
            dr = np.clip(kr[:, None] - qr[None, :] + 7, 0, 14)
            dc = np.clip(kc[:, None] - qc[None, :] + 15, 0, 30)
            g = rpb[:, dr, dc]
            out[:, ti] = np.where(ok[None], g, np.float32(-30000.0))
            ti += 1
    return out


def build_nc(upto=99, dump=(), feed=(), run=None, heads=range(16)):
    nc = bass.Bass("TRN2", target_bir_lowering=False)
    g = Ctx()
    g.nc = nc

    def din(name, shape, dt=F32):
        return nc.dram_tensor(name, list(shape), dt, kind="ExternalInput").ap()

    def dscr(name, shape, dt=F32):
        kind = "ExternalOutput" if name in dump else ("ExternalInput" if name in feed else "Internal")
        return TL(nc.dram_tensor(name, list(shape), dt, kind=kind).ap(), name)

    I = Ctx()
    I.xT = din("xT", [16, 128, T])
    I.cs = din("cs", [128, 32])
    I.ada_w = din("ada_w", [D, 6 * D])
    I.ada_b = din("ada_b", [128, 96])
    I.n1w = din("n1w", [128, 16])
    I.n2w = din("n2w", [128, 16])
    I.w_in = din("w_in", [D, INW])
    I.conv_w = din("conv_w", [128, 48, 5])
    I.dn_sc = din("dn_sc", [128, 64])
    I.hw = din("hw", [128, 3])
    I.consts = din("consts", [len(CONST_NAMES), 128, 128])
    I.ropec = din("ropec", [128, L])
    I.ropes = din("ropes", [128, L])
    I.nab = din("nab", [16, 28, 128, 512])
    I.iota = din("iota", [128, 258])
    I.dnmask = din("dnmask", [4, 128, 128])
    I.w_a = din("w_a", [D, D])
    I.w_b = din("w_b", [D, D])
    I.w_out = din("w_out", [D, D])
    I.w_r = din("w_r", [D, 16])
    I.ew1 = din("ew1", [16, D, 1024])
    I.ew3 = din("ew3", [16, D, 1024])
    I.ew2 = din("ew2", [16, 1024, D])
    outT = TL(nc.dram_tensor("outT", [16, 128, L], F32, kind="ExternalOutput").ap(), "outT")

    S = Ctx()
    S.dn = dscr("s_dn", [48, 128, T])
    S.z = dscr("s_z", [16, 128, L])
    S.ba = dscr("s_ba", [T, 64])
    S.nq = dscr("s_nq", [16, 128, L])
    S.nk = dscr("s_nk", [16, 128, T])
    S.nv = dscr("s_nv", [T, D], BF16)
    S.ga = dscr("s_ga", [16, 128, L])
    S.gb = dscr("s_gb", [16, 128, L])
    S.odn = dscr("s_odn", [16, 128, L], BF16)
    S.ona = dscr("s_ona", [16, 128, L], BF16)
    S.y = dscr("s_y", [16, 128, L], BF16)
    S.x1 = dscr("s_x1", [16, 128, L])
    S.ye = dscr("s_ye", [32, 128, D], BF16)
    S.hT = dscr("s_hT", [16, 128, T], BF16)
    S.grow = dscr("s_grow", [32, T])
    S.brow = dscr("s_brow", [32, T])

    with ExitStack() as es:
        mk = MK(nc, es)
        g.mk = mk

        def sbt(stack, name, shape, dt=F32):
            return _alloc(nc, stack, "sb", name, shape, dt)

        def pst(stack, name, shape, dt=F32):
            return _alloc(nc, stack, "ps", name, shape, dt)

        K = {}
        for i, n in enumerate(CONST_NAMES):
            K[n] = sbt(es, "k_" + n, [128, 128])
            mk.dma("sp", K[n][:], I.consts[i], writes=[K[n]])
        onesb = sbt(es, "onesb", [128, 128], BF16)
        identb = sbt(es, "identb", [128, 128], BF16)
        mk.op("dve", lambda e: e.tensor_copy(onesb[:], K["ones"][:]), reads=[K["ones"]], writes=[onesb])
        mk.op("dve", lambda e: e.tensor_copy(identb[:], K["ident"][:]), reads=[K["ident"]], writes=[identb])
        epsT = sbt(es, "epsT", [128, 1])
        mk.op("dve", lambda e: e.memset(epsT[:], EPS), writes=[epsT])
        modsb = sbt(es, "modsb", [128, 96, 2])
        a1 = sbt(es, "a1", [128, 16]); a1c = sbt(es, "a1c", [128, 16]); a2 = sbt(es, "a2", [128, 16])
        n1w = sbt(es, "n1w", [128, 16]); n2w = sbt(es, "n2w", [128, 16])
        hw = sbt(es, "hw", [128, 3])
        mk.dma("sp", n1w[:], I.n1w, writes=[n1w])
        mk.dma("sp", n2w[:], I.n2w, writes=[n2w])
        mk.dma("sp", hw[:], I.hw, writes=[hw])

        def phase_mod():
            with ExitStack() as ps:
                cs = sbt(ps, "cs", [128, 32]); sc_ = sbt(ps, "silu_c", [128, 32])
                adab = sbt(ps, "adab", [128, 96])
                wb = [sbt(ps, "adaw%d" % i, [128, 16, 512]) for i in range(2)]
                pp = [pst(ps, "p0_%d" % i, [128, 512]) for i in range(2)]
                mk.dma("sp", cs[:], I.cs, writes=[cs])
                mk.dma("sp", adab[:], I.ada_b, writes=[adab])
                mk.op("act", lambda e: e.activation(sc_[:], cs[:], AF.Silu), reads=[cs], writes=[sc_])
                wv = I.ada_w.rearrange("(k p) n -> p k n", p=128)
                for nb in range(24):
                    w = wb[nb % 2]
                    mk.dma("sp", w[:], wv[:, :, nb * 512:(nb + 1) * 512], writes=[w])
                    for ct in range(4):
                        j = nb * 4 + ct
                        p = pp[j % 2]
                        for k in range(16):
                            mk.op("pe", lambda e, p=p, w=w, k=k, ct=ct: e.matmul(
                                p[:, 0:2], w[:, k, ct * 128:(ct + 1) * 128], sc_[:, 2 * k:2 * k + 2],
                                start=(k == 0), stop=(k == 15)), reads=[w, sc_], writes=[p])
                        mk.op("dve", lambda e, p=p, j=j: e.tensor_scalar(
                            modsb[:, j, :], p[:, 0:2], adab[:, j:j + 1], None, op0=ALU.add),
                            reads=[p, adab], writes=[modsb])
                mk.op("dve", lambda e: e.scalar_tensor_tensor(a1[:], modsb[:, 16:32, 0], 1.0, n1w[:], op0=ALU.add, op1=ALU.mult),
                      reads=[modsb, n1w], writes=[a1])
                mk.op("dve", lambda e: e.scalar_tensor_tensor(a1c[:], modsb[:, 16:32, 1], 1.0, n1w[:], op0=ALU.add, op1=ALU.mult),
                      reads=[modsb, n1w], writes=[a1c])
                mk.op("dve", lambda e: e.scalar_tensor_tensor(a2[:], modsb[:, 64:80, 0], 1.0, n2w[:], op0=ALU.add, op1=ALU.mult),
                      reads=[modsb, n2w], writes=[a2])
            mk.barrier()

        def norm_mod(ps_bank, xt, sq, rs, tmp2, w, a_t, bcol, out_fn, extra_reads=()):
            mk.op("act", lambda e: e.activation(sq[:, :, :w], xt[:, :, :w], AF.Square), reads=[xt], writes=[sq])
            for k in range(16):
                mk.op("pe", lambda e, k=k: e.matmul(ps_bank[:, :w], K["ones"][:], sq[:, k, :w], start=(k == 0), stop=(k == 15)),
                      reads=[K["ones"], sq], writes=[ps_bank])
            mk.op("act", lambda e: e.activation(rs[:, :w], ps_bank[:, :w], AF.Ln, bias=epsT[:, 0:1], scale=1.0 / D),
                  reads=[ps_bank, epsT], writes=[rs])
            mk.op("act", lambda e: e.activation(rs[:, :w], rs[:, :w], AF.Exp, scale=-0.5), reads=[rs], writes=[rs])
            for k in range(16):
                tm = tmp2[k % 2]
                mk.op("dve", lambda e, k=k, tm=tm: e.tensor_tensor(tm[:, :w], xt[:, k, :w], rs[:, :w], op=ALU.mult),
                      reads=[xt, rs], writes=[tm])
                o, ow = out_fn(k)
                mk.op("act", lambda e, k=k, tm=tm, o=o: e.activation(o, tm[:, :w], AF.Identity, bias=bcol(k), scale=a_t[:, k:k + 1]),
                      reads=[tm, a_t, modsb] + list(extra_reads), writes=[ow])

        TOK_TILES = [(0, 512), (512, 512), (1024, 512), (1536, 512), (2048, 256)]

        def phase_inproj():
            with ExitStack() as ps:
                hT = sbt(ps, "hT", [128, 16, T], BF16)
                pb = [pst(ps, "p1_%d" % i, [128, 512]) for i in range(8)]
                with ExitStack() as p1:
                    xts = [sbt(p1, "xt%d" % i, [128, 16, 512]) for i in range(2)]
                    sq = sbt(p1, "sq", [128, 16, 512])
                    rs = sbt(p1, "rs", [128, 512])
                    tmp2 = [sbt(p1, "tmp%d" % i, [128, 512]) for i in range(2)]
                    xv = I.xT.rearrange("k p t -> p k t")
                    for ti, (t0, w) in enumerate(TOK_TILES):
                        xt = xts[ti % 2]
                        mk.dma("sp", xt[:, :, :w], xv[:, :, t0:t0 + w], writes=[xt])
                        ctxp = (t0 >= L)
                        norm_mod(pb[ti % 2], xt, sq, rs, tmp2, w, a1c if ctxp else a1,
                                 (lambda k, c=(1 if ctxp else 0): modsb[:, k, c:c + 1]),
                                 lambda k, t0=t0, w=w: (hT[:, k, t0:t0 + w], hT))
                    if "s_hT" in dump:
                        mk.dma("sp", S.hT.t.rearrange("k p t -> p k t"), hT[:], reads=[hT], writes=[S.hT])
                mk.barrier()
                if upto < 2:
                    return
                stg = [sbt(ps, "wstg%d" % i, [128, 16, 256]) for i in range(2)]
                wbf = [sbt(ps, "wbf%d" % i, [128, 16, 256], BF16) for i in range(2)]
                ost = [sbt(ps, "ost%d" % i, [128, T]) for i in range(2)]
                vst = [sbt(ps, "vst%d" % i, [128, NCH, 256], BF16) for i in range(2)]
                bst = sbt(ps, "bst", [128, NCH, 64])
                wv = I.w_in.rearrange("(k p) n -> p k n", p=128)
                blocks = []
                for c0 in range(0, 6144, 256):
                    blocks.append((c0, 256, "fm", (S.dn, c0 // 128), T, "copy"))
                for c0 in range(6144, 8192, 256):
                    blocks.append((c0, 256, "fm", (S.z, (c0 - 6144) // 128), L, "silu"))
                blocks.append((8192, 64, "ba", None, T, None))
                for c0 in range(8256, 10304, 256):
                    blocks.append((c0, 256, "fm", (S.nq, (c0 - 8256) // 128), L, "copy"))
                for c0 in range(10304, 12352, 256):
                    blocks.append((c0, 256, "fm", (S.nk, (c0 - 10304) // 128), T, "copy"))
                for c0 in range(12352, 14400, 256):
                    blocks.append((c0, 256, "tm", c0 - 12352, T, None))
                for c0 in range(14400, 16448, 256):
                    blocks.append((c0, 256, "fm", (S.ga, (c0 - 14400) // 128), L, "sig"))
                for c0 in range(16448, 18496, 256):
                    blocks.append((c0, 256, "fm", (S.gb, (c0 - 16448) // 128), L, "sig"))
                st = {"pi": 0, "oi": 0}

                def load(bi):
                    c0, nc_, *_ = blocks[bi]
                    s_ = stg[bi % 2]; wb_ = wbf[bi % 2]
                    mk.dma("sp", s_[:, :, :nc_], wv[:, :, c0:c0 + nc_], writes=[s_])
                    mk.op("act", lambda e: e.copy(wb_[:, 0:8, :nc_], s_[:, 0:8, :nc_]), reads=[s_], writes=[wb_])
                    mk.op("dve", lambda e: e.tensor_copy(wb_[:, 8:16, :nc_], s_[:, 8:16, :nc_]), reads=[s_], writes=[wb_])

                load(0)
                for bi, (c0, nc_, kind, dst, thi, epi) in enumerate(blocks):
                    if bi + 1 < len(blocks):
                        load(bi + 1)
                    wb_ = wbf[bi % 2]
                    if kind == "fm":
                        for ctile in range(nc_ // 128):
                            o = ost[st["oi"] % 2]; st["oi"] += 1
                            for (t0, w) in TOK_TILES:
                                if t0 >= thi:
                                    continue
                                p = pb[st["pi"] % 8]; st["pi"] += 1
                                for k in range(16):
                                    mk.op("pe", lambda e, p=p, k=k, ctile=ctile, t0=t0, w=w: e.matmul(
                                        p[:, :w], wb_[:, k, ctile * 128:(ctile + 1) * 128], hT[:, k, t0:t0 + w],
                                        start=(k == 0), stop=(k == 15)), reads=[wb_, hT], writes=[p])
                                if epi == "copy":
                                    mk.op("dve", lambda e, p=p, o=o, t0=t0, w=w: e.tensor_copy(o[:, t0:t0 + w], p[:, :w]), reads=[p], writes=[o])
                                else:
                                    fn = AF.Silu if epi == "silu" else AF.Sigmoid
                                    mk.op("act", lambda e, p=p, o=o, t0=t0, w=w, fn=fn: e.activation(o[:, t0:t0 + w], p[:, :w], fn), reads=[p], writes=[o])
                            dt_, ti_ = dst
                            mk.dma("sp", dt_.t[ti_ + ctile][:, 0:thi], o[:, 0:thi], reads=[o], writes=[dt_])
                    elif kind == "tm":
                        vs = vst[st["oi"] % 2]; st["oi"] += 1
                        for ti in range(NCH):
                            p = pb[st["pi"] % 8]; st["pi"] += 1
                            for k in range(16):
                                mk.op("pe", lambda e, p=p, k=k, ti=ti: e.matmul(
                                    p[:, :256], hT[:, k, ti * 128:(ti + 1) * 128], wb_[:, k, :256],
                                    start=(k == 0), stop=(k == 15)), reads=[wb_, hT], writes=[p])
                            eng = "dve" if ti % 2 == 0 else "act"
                            if eng == "dve":
                                mk.op("dve", lambda e, p=p, ti=ti: e.tensor_copy(vs[:, ti, :], p[:, :256]), reads=[p], writes=[vs])
                            else:
                                mk.op("act", lambda e, p=p, ti=ti: e.copy(vs[:, ti, :], p[:, :256]), reads=[p], writes=[vs])
                        mk.dma("sp", S.nv.t.rearrange("(n p) c -> p n c", p=128)[:, :, dst:dst + 256], vs[:], reads=[vs], writes=[S.nv])
                    else:
                        for ti in range(NCH):
                            p = pb[st["pi"] % 8]; st["pi"] += 1
                            for k in range(16):
                                mk.op("pe", lambda e, p=p, k=k, ti=ti: e.matmul(
                                    p[:, :64], hT[:, k, ti * 128:(ti + 1) * 128], wb_[:, k, :64],
                                    start=(k == 0), stop=(k == 15)), reads=[wb_, hT], writes=[p])
                            mk.op("dve", lambda e, p=p, ti=ti: e.tensor_copy(bst[:, ti, :], p[:, :64]), reads=[p], writes=[bst])
                        mk.dma("sp", S.ba.t.rearrange("(n p) c -> p n c", p=128), bst[:], reads=[bst], writes=[S.ba])
            mk.barrier()

        g.phase_mod = phase_mod
        g.phase_inproj = phase_inproj
        phases_extra(g, I, S, K, mk, sbt, pst, es, outT, dict(
            modsb=modsb, a2=a2, hw=hw, epsT=epsT, onesb=onesb, identb=identb, norm_mod=norm_mod, dump=dump, upto=upto))

        if run is None:
            run = ("mod", "inproj", "dn", "na", "merge", "moe")
        if "mod" in run:
            phase_mod()
        if "inproj" in run:
            phase_inproj()
        if "dn" in run:
            g.phase_dn(heads)
        if "na" in run:
            g.phase_na(heads)
        if "merge" in run:
            g.phase_merge()
        if "moe" in run:
            g.phase_moe()
        if "modsb" in dump:
            md = nc.dram_tensor("modsb_o", [128, 192], F32, kind="ExternalOutput").ap()
            mk.dma("sp", md, modsb[:].rearrange("p a b -> p (a b)"), reads=[modsb])
        mk.barrier()
        g.ninst = mk.ninst
    return nc, g


def phases_extra(g, I, S, K, mk, sbt, pst, es, outT, X):
    nc = g.nc
    modsb = X["modsb"]; hw = X["hw"]; epsT = X["epsT"]; onesb = X["onesb"]; identb = X["identb"]
    dump = X["dump"]
    TOK5 = [(0, 512), (512, 512), (1024, 512), (1536, 512), (2048, 256)]
    ident = K["ident"]; ones = K["ones"]

    def phase_dn(heads=range(16)):
        with ExitStack() as ps:
            pw = [pst(ps, "dnw%d" % i, [128, 512]) for i in range(2)]
            pqb = [pst(ps, "dnq%d" % i, [128, 512]) for i in range(6)]
            class SlotV(TL):
                def __init__(self, bank, j):
                    self.t = bank.t[:, j * 128:(j + 1) * 128]
                    self.b = bank.b

            banks = [[SlotV(pqb[i], j) for j in range(4)] for i in range(6)]
            sl = {"i": 0, "w": 0}

            def nbank():
                sl["i"] += 1
                return banks[sl["i"] % 6]

            def nwide():
                sl["w"] += 1
                return pw[sl["w"] % 2]

            ba = sbt(ps, "ba", [128, NCH, 64]); dsc = sbt(ps, "dsc", [128, 64]); negexp = sbt(ps, "negexp", [128, 32])
            betaC = sbt(ps, "betaC", [128, NCH, 32]); gC = sbt(ps, "gC", [128, NCH, 32])
            cw = sbt(ps, "cw", [128, 48, 5])
            mk.dma("sp", ba[:], S.ba.t.rearrange("(n p) c -> p n c", p=128), reads=[S.ba], writes=[ba])
            mk.dma("sp", dsc[:], I.dn_sc, writes=[dsc])
            mk.dma("sp", cw[:], I.conv_w, writes=[cw])
            mk.op("act", lambda e: e.activation(negexp[:], dsc[:, 0:32], AF.Exp), reads=[dsc], writes=[negexp])
            mk.op("dve", lambda e: e.tensor_scalar(negexp[:], negexp[:], -1.0, None, op0=ALU.mult), reads=[negexp], writes=[negexp])
            mk.op("act", lambda e: e.activation(betaC[:], ba[:, :, 0:32], AF.Sigmoid), reads=[ba], writes=[betaC])
            for n in range(NCH):
                mk.op("dve", lambda e, n=n: e.tensor_tensor(gC[:, n, :], ba[:, n, 32:64], dsc[:, 32:64], op=ALU.add), reads=[ba, dsc], writes=[gC])
            mk.op("act", lambda e: e.activation(gC[:], gC[:], AF.Exp), reads=[gC], writes=[gC])
            mk.op("act", lambda e: e.activation(gC[:], gC[:], AF.Ln, bias=1.0), reads=[gC], writes=[gC])
            for n in range(NCH):
                mk.op("dve", lambda e, n=n: e.tensor_tensor(gC[:, n, :], gC[:, n, :], negexp[:], op=ALU.mult), reads=[gC, negexp], writes=[gC])

            with ExitStack() as p0:
                growT = [sbt(p0, "growT%d" % i, [16, NCH, 128]) for i in range(2)]
                browT = [sbt(p0, "browT%d" % i, [16, NCH, 128]) for i in range(2)]
                Um0 = [K["uf"], K["ub"]]
                for n in range(NCH):
                    bk = nbank()
                    for d in range(2):
                        mk.op("pe", lambda e, bk=bk, d=d, n=n: e.matmul(bk[d][0:16, :], gC[:, n, d * 16:(d + 1) * 16], Um0[d][:], start=True, stop=True), reads=[gC, Um0[d]], writes=[bk[d]])
                        mk.op("pe", lambda e, bk=bk, d=d, n=n: e.transpose(bk[2 + d][0:16, :], betaC[:, n, d * 16:(d + 1) * 16], ident[:]), reads=[betaC, ident], writes=[bk[2 + d]])
                    for d in range(2):
                        mk.op("dve", lambda e, bk=bk, d=d, n=n: e.tensor_copy(growT[d][:, n, :], bk[d][0:16, :]), reads=[bk[d]], writes=[growT[d]])
                        mk.op("act", lambda e, bk=bk, d=d, n=n: e.copy(browT[d][:, n, :], bk[2 + d][0:16, :]), reads=[bk[2 + d]], writes=[browT[d]])
                for d in range(2):
                    mk.dma("sp", S.grow.t[d * 16:(d + 1) * 16, :], growT[d][:].rearrange("p a b -> p (a b)"), reads=[growT[d]], writes=[S.grow])
                    mk.dma("sp", S.brow.t[d * 16:(d + 1) * 16, :], browT[d][:].rearrange("p a b -> p (a b)"), reads=[browT[d]], writes=[S.brow])
            mk.barrier()
            gbs = [sbt(ps, "gbs%d" % i, [128, 128]) for i in range(8)]
            bbs = [sbt(ps, "bbs%d" % i, [128, 128]) for i in range(8)]
            gbc = {"i": 0}
            raw = sbt(ps, "raw", [128, T]); sqb = raw; rsb = sbt(ps, "rsb", [128, T])
            cq = sbt(ps, "cq", [128, T]); ck = sbt(ps, "ck", [128, T]); cv = sbt(ps, "cv", [128, T])
            ktok = sbt(ps, "ktok", [128, NCH, 128]); vtok = sbt(ps, "vtok", [128, NCH, 128])
            oacc = sbt(ps, "oacc", [128, L]); zt = cv
            oaccB = [Buf("oacc%d" % n) for n in range(16)]
            NS = 8
            uS = sbt(ps, "uS", [128, NS, 128]); wS = sbt(ps, "wS", [128, NS, 128])
            aS = sbt(ps, "aS", [128, NS, 128]); qS = sbt(ps, "qS", [128, NS, 128])
            uV = [TL(uS.t[:, i, :]) for i in range(NS)]; wV = [TL(wS.t[:, i, :]) for i in range(NS)]
            aV = [TL(aS.t[:, i, :]) for i in range(NS)]; qV = [TL(qS.t[:, i, :]) for i in range(NS)]
            W = 4
            wb = {}
            for nm in ["A0", "A1", "B0", "B1", "P0", "P1", "ApI", "dm1", "dm2", "dec", "dec2", "eGb", "BM", "Af"]:
                wb[nm] = [sbt(ps, "w%s%d" % (nm, i), [128, 128]) for i in range(W)]
            wb["vb"] = [sbt(ps, "wvb%d" % i, [128, 128]) for i in range(W)]
            wb["kbg"] = [sbt(ps, "wkbg%d" % i, [128, 128]) for i in range(W)]
            bd16 = sbt(ps, "bd16", [128, 128]); msk = [sbt(ps, "msk%d" % i, [128, 128]) for i in range(3)]
            mk.dma("sp", bd16[:], I.dnmask[0], writes=[bd16])
            for i_ in range(3):
                mk.dma("sp", msk[i_][:], I.dnmask[1 + i_], writes=[msk[i_]])
            kdec = [sbt(ps, "kdec%d" % i, [128, 128]) for i in range(4)]
            vnew = [sbt(ps, "vnew%d" % i, [128, 128]) for i in range(4)]
            Sst = [[sbt(ps, "S%d_%d" % (d, i), [128, 128]) for i in range(2)] for d in range(2)]
            small = {}
            for nm in ["gc", "bc", "Gcol", "eGcol", "kbs", "kds", "eGl", "tmp"]:
                small[nm] = [sbt(ps, "sm%s%d" % (nm, d), [128, NCH]) for d in range(2)]
            Umat = [K["uf"], K["ub"]]; Mm = [K["mf"], K["mb"]]; Mo = [K["mb"], K["mf"]]
            offd = K["offd"]

            R = lambda ap: ap.bitcast(F32R)
            DN_STOP = int(os.environ.get("DN_STOP", "99"))
            PREP_STOP = int(os.environ.get("PREP_STOP", "99"))
            for h in heads:
                if DN_STOP <= 0:
                    break
                for idx, (acc, eng) in enumerate([(cq, "dve"), (ck, "dve"), (cv, "pool")]):
                    mk.dma("sp", raw[:], S.dn.t[idx * 16 + h], reads=[S.dn], writes=[raw])
                    t_ = idx * 16 + h
                    if eng == "dve":
                        mk.op(eng, lambda e, acc=acc, t_=t_: e.tensor_scalar(R(acc[:]), raw[:], cw[:, t_, 2:3], None, op0=ALU.mult),
                              reads=[raw, cw], writes=[acc])
                    else:
                        mk.op(eng, lambda e, acc=acc, t_=t_: e.tensor_tensor(R(acc[:]), raw[:], cw[:, t_, 2:3].to_broadcast([128, T]), op=ALU.mult),
                              reads=[raw, cw], writes=[acc])
                    for (s0, s1) in [(0, L), (L, T)]:
                        for jj in (0, 1, 3, 4):
                            sh = jj - 2
                            d0 = s0 + max(0, -sh); d1 = s1 - max(0, sh)
                            if eng == "dve":
                                mk.op(eng, lambda e, acc=acc, t_=t_, jj=jj, d0=d0, d1=d1, sh=sh: e.scalar_tensor_tensor(
                                    R(acc[:, d0:d1]), raw[:, d0 + sh:d1 + sh], cw[:, t_, jj:jj + 1], acc[:, d0:d1], op0=ALU.mult, op1=ALU.add),
                                    reads=[raw, cw, acc], writes=[acc])
                            else:
                                mk.op(eng, lambda e, t_=t_, jj=jj, d0=d0, d1=d1, sh=sh: e.tensor_tensor(
                                    rsb[:, d0:d1], raw[:, d0 + sh:d1 + sh], cw[:, t_, jj:jj + 1].to_broadcast([128, d1 - d0]), op=ALU.mult),
                                    reads=[raw, cw], writes=[rsb])
                                mk.op(eng, lambda e, acc=acc, d0=d0, d1=d1: e.tensor_tensor(
                                    R(acc[:, d0:d1]), acc[:, d0:d1], rsb[:, d0:d1], op=ALU.add),
                                    reads=[rsb, acc], writes=[acc])
                    mk.op("act", lambda e, acc=acc: e.activation(R(acc[:]), acc[:], AF.Silu), reads=[acc], writes=[acc])
                if DN_STOP <= 1:
                    break
                for acc, scl in [(cq, 128.0 ** -0.5), (ck, 1.0)]:
                    mk.op("act", lambda e, acc=acc: e.activation(sqb[:], acc[:], AF.Square), reads=[acc], writes=[sqb])
                    for (t0, w) in TOK5:
                        p = nwide()
                        mk.op("pe", lambda e, p=p, t0=t0, w=w: e.matmul(p[:, :w], ones[:], sqb[:, t0:t0 + w], start=True, stop=True),
                              reads=[ones, sqb], writes=[p])
                        mk.op("act", lambda e, p=p, t0=t0, w=w: e.activation(rsb[:, t0:t0 + w], p[:, :w], AF.Ln, bias=epsT[:, 0:1], scale=1.0),
                              reads=[p, epsT], writes=[rsb])
                    mk.op("act", lambda e: e.activation(rsb[:], rsb[:], AF.Exp, scale=-0.5), reads=[rsb], writes=[rsb])
                    mk.op("dve", lambda e, acc=acc, scl=scl: e.scalar_tensor_tensor(R(acc[:]), acc[:], scl, rsb[:], op0=ALU.mult, op1=ALU.mult),
                          reads=[acc, rsb], writes=[acc])
                if DN_STOP <= 2:
                    break
                for src, dst in [(ck, ktok), (cv, vtok)]:
                    for n0 in range(0, NCH, 4):
                        nn = min(4, NCH - n0)
                        p = nwide()
                        for q_ in range(nn):
                            n = n0 + q_
                            mk.op("pe", lambda e, p=p, q_=q_, n=n, src=src: e.transpose(p[:, q_ * 128:(q_ + 1) * 128], src[:, n * 128:(n + 1) * 128], ident[:]),
                                  reads=[src, ident], writes=[p])
                        mk.op("act", lambda e, p=p, n0=n0, nn=nn, dst=dst: e.copy(dst[:, n0:n0 + nn, :], p[:, :nn * 128].rearrange("p (a b) -> p a b", b=128)),
                              reads=[p], writes=[dst])
                if DN_STOP <= 3:
                    break
                mk.dma("sp", zt[:, :L], S.z.t[h], reads=[S.z], writes=[zt])
                for d in range(2):
                    ci = d * 16 + h
                    gc = small["gc"][d]; bc = small["bc"][d]
                    mk.op("dve", lambda e, gc=gc, ci=ci: e.tensor_copy(gc[:], gC[:, :, ci]), reads=[gC], writes=[gc])
                    mk.op("dve", lambda e, bc=bc, ci=ci: e.tensor_copy(bc[:], betaC[:, :, ci]), reads=[betaC], writes=[bc])
                    bk = nbank(); p1 = bk[0]; p2 = bk[1]
                    mk.op("pe", lambda e, p1=p1, d=d, gc=gc: e.matmul(p1[:, :NCH], Umat[d][:], gc[:], start=True, stop=True), reads=[Umat[d], gc], writes=[p1])
                    mk.op("pe", lambda e, p2=p2, gc=gc: e.matmul(p2[:, :NCH], ones[:], gc[:], start=True, stop=True), reads=[ones, gc], writes=[p2])
                    Gcol = small["Gcol"][d]; eGcol = small["eGcol"][d]; kbs = small["kbs"][d]; kds = small["kds"][d]; eGl = small["eGl"][d]; tmp = small["tmp"][d]
                    mk.op("dve", lambda e, p1=p1, Gcol=Gcol: e.tensor_copy(Gcol[:], p1[:, :NCH]), reads=[p1], writes=[Gcol])
                    mk.op("act", lambda e, p1=p1, eGcol=eGcol: e.activation(eGcol[:], p1[:, :NCH], AF.Exp), reads=[p1], writes=[eGcol])
                    mk.op("dve", lambda e, kbs=kbs, bc=bc, eGcol=eGcol: e.tensor_tensor(kbs[:], bc[:], eGcol[:], op=ALU.mult), reads=[bc, eGcol], writes=[kbs])
                    mk.op("dve", lambda e, tmp=tmp, p2=p2, Gcol=Gcol: e.tensor_tensor(tmp[:], p2[:, :NCH], Gcol[:], op=ALU.subtract), reads=[p2, Gcol], writes=[tmp])
                    mk.op("act", lambda e, kds=kds, tmp=tmp: e.activation(kds[:], tmp[:], AF.Exp), reads=[tmp], writes=[kds])
                    mk.op("act", lambda e, eGl=eGl, p2=p2: e.activation(eGl[:], p2[:, :NCH], AF.Exp), reads=[p2], writes=[eGl])
                    mk.op("dve", lambda e, d=d: e.tensor_scalar(R(Sst[d][0][:]), ident[:], 0.0, None, op0=ALU.mult), reads=[ident], writes=[Sst[d][0]])

                fo = [16, 17] + list(range(16)); bo = [17, 16] + list(range(15, -1, -1))
                seq = []
                for i_ in range(NCH):
                    seq.append((0, fo[i_])); seq.append((1, bo[i_]))
                spos = {0: 0, 1: 0}
                oinit = set()

                def prep(wave):
                    cds = [(wi, d, n, (wave_base + wi) % NS) for wi, (d, n) in enumerate(wave)]
                    P = {}
                    for wi, d, n, si in cds:
                        gc = small["gc"][d]; bc = small["bc"][d]; Gcol = small["Gcol"][d]
                        kc = ck[:, n * 128:(n + 1) * 128]; qc = cq[:, n * 128:(n + 1) * 128]
                        _b0, _b1, KKp, QKp = nbank()
                        MMS = "kq"
                        GBs = gbs[gbc["i"] % 8]; BBs = bbs[gbc["i"] % 8]; gbc["i"] += 1
                        ci_ = d * 16 + h
                        mk.dma("sp", GBs[:], S.grow.t[ci_:ci_ + 1, n * 128:(n + 1) * 128].partition_broadcast(128), reads=[S.grow], writes=[GBs])
                        mk.dma("sp", BBs[:], S.brow.t[ci_:ci_ + 1, n * 128:(n + 1) * 128].partition_broadcast(128), reads=[S.brow], writes=[BBs])
                        GBv = GBs[:]; BBv = BBs[:]
                        if "k" in MMS:
                          mk.op("pe", lambda e, KKp=KKp, kc=kc: e.matmul(KKp[:], R(kc), R(kc), start=True, stop=True), reads=[ck], writes=[KKp])
                        if "q" in MMS:
                          mk.op("pe", lambda e, QKp=QKp, kc=kc, qc=qc: e.matmul(QKp[:], R(kc), R(qc), start=True, stop=True), reads=[ck, cq], writes=[QKp])
                        if PREP_STOP <= 1:
                            continue
                        dm1 = wb["dm1"][wi]; dm2 = wb["dm2"][wi]; dec = wb["dec"][wi]; dec2 = wb["dec2"][wi]; eGb = wb["eGb"][wi]; BM = wb["BM"][wi]
                        mk.op("dve", lambda e, dm1=dm1, GBv=GBv, Gcol=Gcol, n=n, d=d: e.scalar_tensor_tensor(dm1[:], GBv, Gcol[:, n:n + 1], Mm[d][:], op0=ALU.subtract, op1=ALU.add), reads=[GBs, Gcol, Mm[d]], writes=[dm1])
                        mk.op("dve", lambda e, dm2=dm2, GBv=GBv, Gcol=Gcol, n=n, d=d: e.scalar_tensor_tensor(dm2[:], GBv, Gcol[:, n:n + 1], Mo[d][:], op0=ALU.subtract, op1=ALU.subtract), reads=[GBs, Gcol, Mo[d]], writes=[dm2])
                        mk.op("act", lambda e, dec=dec, dm1=dm1: e.activation(R(dec[:]), dm1[:], AF.Exp), reads=[dm1], writes=[dec])
                        mk.op("act", lambda e, dec2=dec2, dm2=dm2: e.activation(R(dec2[:]), dm2[:], AF.Exp, scale=-1.0), reads=[dm2], writes=[dec2])
                        mk.op("act", lambda e, eGb=eGb, GBv=GBv: e.activation(R(eGb[:]), GBv, AF.Exp), reads=[GBs], writes=[eGb])
                        mk.op("pool", lambda e, BM=BM, BBv=BBv: e.tensor_tensor(BM[:], BBv, offd[:], op=ALU.mult), reads=[BBs, offd], writes=[BM])
                        mk.op("dve", lambda e, si=si, QKp=QKp, dec=dec: e.tensor_tensor(R(aV[si][:]), QKp[:], dec[:], op=ALU.mult), reads=[QKp, dec], writes=[aV[si]])
                        mk.op("pool", lambda e, BM=BM, dec=dec: e.tensor_tensor(BM[:], BM[:], dec[:], op=ALU.mult), reads=[BM, dec], writes=[BM])
                        mk.op("pool", lambda e, dec2=dec2: e.tensor_tensor(R(dec2[:]), dec2[:], offd[:], op=ALU.mult), reads=[dec2, offd], writes=[dec2])
                        A = wb["A0"][wi]; B = wb["B0"][wi]; Pm = wb["P0"][wi]; Af = wb["Af"][wi]
                        mk.op("dve", lambda e, KKp=KKp, BM=BM: e.tensor_tensor(BM[:], KKp[:], BM[:], op=ALU.mult), reads=[KKp, BM], writes=[BM])
                        mk.op("dve", lambda e, Af=Af, KKp=KKp, bc=bc, n=n, dec2=dec2: e.scalar_tensor_tensor(Af[:], KKp[:], bc[:, n:n + 1], dec2[:], op0=ALU.mult, op1=ALU.mult), reads=[KKp, bc, dec2], writes=[Af])
                        mk.op("pool", lambda e, si=si, qc=qc, eGb=eGb: e.tensor_tensor(R(qV[si][:]), qc, eGb[:], op=ALU.mult), reads=[cq, eGb], writes=[qV[si]])
                        mk.op("pool", lambda e, B=B, BM=BM: e.tensor_tensor(R(B[:]), BM[:], bd16[:], op=ALU.mult), reads=[BM, bd16], writes=[B])
                        mk.op("pool", lambda e, A=A, Af=Af: e.tensor_tensor(R(A[:]), Af[:], bd16[:], op=ALU.mult), reads=[Af, bd16], writes=[A])
                        mk.op("pool", lambda e, Pm=Pm, B=B: e.tensor_tensor(R(Pm[:]), ident[:], B[:], op=ALU.subtract), reads=[ident, B], writes=[Pm])
                        P[wi] = [A, B, Pm]
                    if PREP_STOP <= 2:
                        return
                    NLEV = 3
                    for lev in range(1, NLEV + 1):
                        nxt = "1" if lev % 2 == 1 else "0"
                        pend = {}
                        for wi, d, n, si in cds:
                            A, B, Pm = P[wi]
                            bk = nbank()
                            Ap = bk[0]
                            mk.op("pe", lambda e, Ap=Ap, A=A, B=B: e.matmul(Ap[:], R(B[:]), R(A[:]), start=True, stop=True), reads=[A, B], writes=[Ap])
                            Bp = None
                            if lev < NLEV:
                                Bp = bk[1]
                                mk.op("pe", lambda e, Bp=Bp, A=A, B=B: e.matmul(Bp[:], R(A[:]), R(B[:]), start=True, stop=True), reads=[A, B], writes=[Bp])
                            pend[wi] = (Ap, Bp, bk)
                        for wi, d, n, si in cds:
                            Ap, Bp, bk = pend[wi]
                            ApI = wb["ApI"][wi]
                            mk.op("dve", lambda e, ApI=ApI, Ap=Ap: e.tensor_tensor(R(ApI[:]), Ap[:], ident[:], op=ALU.add), reads=[Ap, ident], writes=[ApI])
                            if lev < NLEV:
                                An = wb["A" + nxt][wi]; Bn = wb["B" + nxt][wi]
                                mk.op("act", lambda e, An=An, Ap=Ap: e.copy(R(An[:]), Ap[:]), reads=[Ap], writes=[An])
                                mk.op("act", lambda e, Bn=Bn, Bp=Bp: e.copy(R(Bn[:]), Bp[:]), reads=[Bp], writes=[Bn])
                                P[wi][0] = An; P[wi][1] = Bn
                        pend2 = {}
                        for wi, d, n, si in cds:
                            Pm = P[wi][2]; ApI = wb["ApI"][wi]
                            Pp = pend[wi][2][2]
                            mk.op("pe", lambda e, Pp=Pp, ApI=ApI, Pm=Pm: e.matmul(Pp[:], R(ApI[:]), R(Pm[:]), start=True, stop=True), reads=[ApI, Pm], writes=[Pp])
                            pend2[wi] = Pp
                        for wi, d, n, si in cds:
                            Pn = wb["P" + nxt][wi]
                            Pp = pend2[wi]
                            if wi % 2 == 0:
                                mk.op("dve", lambda e, Pn=Pn, Pp=Pp: e.tensor_copy(R(Pn[:]), Pp[:]), reads=[Pp], writes=[Pn])
                            else:
                                mk.op("act", lambda e, Pn=Pn, Pp=Pp: e.copy(R(Pn[:]), Pp[:]), reads=[Pp], writes=[Pn])
                            P[wi][2] = Pn
                    for wi, d, n, si in cds:
                        Dr = P[wi][2]; Dl = wb["dec2"][wi]
                        bk = nbank()
                        mk.op("pe", lambda e, bk=bk, Dr=Dr: e.transpose(bk[0][:], Dr[:], ident[:]), reads=[Dr, ident], writes=[bk[0]])
                        mk.op("act", lambda e, bk=bk, Dl=Dl: e.copy(R(Dl[:]), bk[0][:]), reads=[bk[0]], writes=[Dl])
                    for bl in range(3):
                        Ms = msk[bl]
                        pend = {}
                        for wi, d, n, si in cds:
                            Dr = P[wi][2]; AM = wb["eGb"][wi]; Af = wb["Af"][wi]
                            mk.op("pool", lambda e, AM=AM, Af=Af, Ms=Ms: e.tensor_tensor(R(AM[:]), Af[:], Ms[:], op=ALU.mult), reads=[Af, Ms], writes=[AM])
                            bk = nbank()
                            mk.op("pe", lambda e, bk=bk, AM=AM, Dr=Dr: e.matmul(bk[0][:], R(AM[:]), R(Dr[:]), start=True, stop=True), reads=[AM, Dr], writes=[bk[0]])
                            pend[wi] = bk
                        for wi, d, n, si in cds:
                            bk = pend[wi]; Ysb = wb["dec"][wi]
                            mk.op("act", lambda e, bk=bk, Ysb=Ysb: e.copy(R(Ysb[:]), bk[0][:]), reads=[bk[0]], writes=[Ysb])
                        for wi, d, n, si in cds:
                            bk = pend[wi]; Ysb = wb["dec"][wi]; Dl = wb["dec2"][wi]
                            mk.op("pe", lambda e, bk=bk, Dl=Dl, Ysb=Ysb: e.matmul(bk[1][:], R(Dl[:]), R(Ysb[:]), start=True, stop=True), reads=[Dl, Ysb], writes=[bk[1]])
                        for wi, d, n, si in cds:
                            bk = pend[wi]; Dr = P[wi][2]
                            Dn = wb["P1"][wi] if Dr is wb["P0"][wi] else wb["P0"][wi]
                            mk.op("dve", lambda e, bk=bk, Dr=Dr, Dn=Dn: e.tensor_tensor(R(Dn[:]), Dr[:], bk[1][:], op=ALU.subtract), reads=[Dr, bk[1]], writes=[Dn])
                            P[wi][2] = Dn
                        if bl < 2:
                            for wi, d, n, si in cds:
                                bk = pend[wi]; Dn = P[wi][2]; Dl = wb["dec2"][wi]
                                mk.op("pe", lambda e, bk=bk, Dn=Dn: e.transpose(bk[2][:], Dn[:], ident[:]), reads=[Dn, ident], writes=[bk[2]])
                                mk.op("act", lambda e, bk=bk, Dl=Dl: e.copy(R(Dl[:]), bk[2][:]), reads=[bk[2]], writes=[Dl])
                    if PREP_STOP <= 3:
                        return
                    for wi, d, n, si in cds:
                        TT = P[wi][2]
                        bc = small["bc"][d]; kbs = small["kbs"][d]
                        vb = wb["vb"][wi]; kbg = wb["kbg"][wi]
                        mk.op("dve", lambda e, vb=vb, n=n, bc=bc: e.tensor_scalar(R(vb[:]), vtok[:, n, :], bc[:, n:n + 1], None, op0=ALU.mult), reads=[vtok, bc], writes=[vb])
                        mk.op("dve", lambda e, kbg=kbg, n=n, kbs=kbs: e.tensor_scalar(R(kbg[:]), ktok[:, n, :], kbs[:, n:n + 1], None, op0=ALU.mult), reads=[ktok, kbs], writes=[kbg])
                        bk = nbank(); up = bk[0]; wp = bk[1]
                        mk.op("pe", lambda e, up=up, TT=TT, vb=vb: e.matmul(up[:], R(TT[:]), R(vb[:]), start=True, stop=True), reads=[TT, vb], writes=[up])
                        mk.op("pe", lambda e, wp=wp, TT=TT, kbg=kbg: e.matmul(wp[:], R(kbg[:]), R(TT[:]), start=True, stop=True), reads=[TT, kbg], writes=[wp])
                        mk.op("act", lambda e, si=si, up=up: e.copy(uV[si][:], up[:]), reads=[up], writes=[uV[si]])
                        mk.op("dve", lambda e, si=si, wp=wp: e.tensor_scalar(R(wV[si][:]), wp[:], -1.0, None, op0=ALU.mult), reads=[wp], writes=[wV[si]])

                def scan(wave):
                    for wi, (d, n) in enumerate(wave):
                        si = (wave_base + wi) % NS
                        kds = small["kds"][d]; eGl = small["eGl"][d]
                        Sc = Sst[d][spos[d] % 2]; Sn = Sst[d][(spos[d] + 1) % 2]; spos[d] += 1
                        kd = kdec[si % 4]; vn = vnew[si % 4]
                        mk.op("act", lambda e, kd=kd, n=n, kds=kds: e.activation(R(kd[:]), ktok[:, n, :], AF.Copy, scale=kds[:, n:n + 1]), reads=[ktok, kds], writes=[kd])
                        bk = nbank(); vp = bk[0]; sp_ = bk[1]; bk2 = nbank()
                        mk.op("pe", lambda e, vp=vp, si=si, Sc=Sc: e.matmul(vp[:], R(wV[si][:]), R(Sc[:]), start=True, stop=True), reads=[wV[si], Sc], writes=[vp])
                        mk.op("dve", lambda e, vn=vn, vp=vp, si=si: e.tensor_tensor(R(vn[:]), vp[:], uV[si][:], op=ALU.add), reads=[vp, uV[si]], writes=[vn])
                        if n < 16:
                            op_ = bk2[0]
                            mk.op("pe", lambda e, op_=op_, Sc=Sc, si=si: e.matmul(op_[:], R(Sc[:]), R(qV[si][:]), start=True, stop=False), reads=[Sc, qV[si]], writes=[op_])
                            mk.op("pe", lambda e, op_=op_, vn=vn, si=si: e.matmul(op_[:], R(vn[:]), R(aV[si][:]), start=False, stop=True), reads=[vn, aV[si]], writes=[op_])
                            if n not in oinit:
                                oinit.add(n)
                                mk.op("act", lambda e, op_=op_, n=n: e.copy(oacc[:, n * 128:(n + 1) * 128], op_[:]), reads=[op_], writes=[oaccB[n]])
                            else:
                                mk.op("dve", lambda e, op_=op_, n=n: e.tensor_tensor(oacc[:, n * 128:(n + 1) * 128], op_[:], oacc[:, n * 128:(n + 1) * 128], op=ALU.add), reads=[op_, oaccB[n]], writes=[oaccB[n]])
                        mk.op("pe", lambda e, sp_=sp_, kd=kd, vn=vn: e.matmul(sp_[:], R(kd[:]), R(vn[:]), start=True, stop=True), reads=[kd, vn], writes=[sp_])
                        mk.op("dve", lambda e, Sn=Sn, Sc=Sc, eGl=eGl, n=n, sp_=sp_: e.scalar_tensor_tensor(R(Sn[:]), Sc[:], eGl[:, n:n + 1], sp_[:], op0=ALU.mult, op1=ALU.add), reads=[Sc, eGl, sp_], writes=[Sn])

                waves = [seq[i_:i_ + W] for i_ in range(0, len(seq), W)]
                if DN_STOP <= 4:
                    break
                for wv_i, wave in enumerate(waves):
                    wave_base = wv_i * W
                    prep(wave)
                    if DN_STOP <= 5:
                        break
                    scan(wave)
                    if DN_STOP <= 6:
                        break
                if DN_STOP <= 6:
                    break
                mk.op("act", lambda e: e.activation(sqb[:, :L], oacc[:], AF.Square), reads=oaccB, writes=[sqb])
                for (t0, w) in TOK5[:4]:
                    p = nwide()
                    mk.op("pe", lambda e, p=p, t0=t0, w=w: e.matmul(p[:, :w], ones[:], sqb[:, t0:t0 + w], start=True, stop=True), reads=[ones, sqb], writes=[p])
                    mk.op("act", lambda e, p=p, t0=t0, w=w: e.activation(rsb[:, t0:t0 + w], p[:, :w], AF.Ln, bias=epsT[:, 0:1], scale=1.0 / 128.0), reads=[p, epsT], writes=[rsb])
                mk.op("act", lambda e: e.activation(rsb[:, :L], rsb[:, :L], AF.Exp, scale=-0.5), reads=[rsb], writes=[rsb])
                mk.op("dve", lambda e: e.tensor_tensor(sqb[:, :L], oacc[:], rsb[:, :L], op=ALU.mult), reads=oaccB + [rsb], writes=[sqb])
                mk.op("dve", lambda e: e.scalar_tensor_tensor(rsb[:, :1024].bitcast(BF16), sqb[:, :L], hw[:, 0:1], zt[:, :L], op0=ALU.mult, op1=ALU.mult), reads=[sqb, hw, zt], writes=[rsb])
                mk.dma("sp", S.odn.t[h], rsb[:, :1024].bitcast(BF16), reads=[rsb], writes=[S.odn])
        mk.barrier()

    g.phase_dn = phase_dn

    def phase_na(heads=range(16)):
        with ExitStack() as ps:
            pb = [pst(ps, "na%d" % i, [128, 512]) for i in range(8)]
            cnt = {"s": 0, "g": 0, "bb": 0}

            def sbank():
                cnt["s"] += 1
                return pb[cnt["s"] % 4]
            ropec = sbt(ps, "ropec", [128, L]); ropes = sbt(ps, "ropes", [128, L])
            mk.dma("sp", ropec[:], I.ropec, writes=[ropec]); mk.dma("sp", ropes[:], I.ropes, writes=[ropes])
            qraws = [sbt(ps, "qraw%d" % i, [128, L]) for i in range(2)]; kraws = [sbt(ps, "kraw%d" % i, [128, T]) for i in range(2)]
            sq = sbt(ps, "nsq", [128, T]); rs = sbt(ps, "nrs", [128, T])
            t1 = sbt(ps, "t1", [128, T]); qbf = sbt(ps, "qbf", [128, L], BF16); kbf = sbt(ps, "kbf", [128, T], BF16)
            vts = [sbt(ps, "vt%d" % i, [128, NCH, 128], BF16) for i in range(2)]
            bias = [sbt(ps, "bias%d" % i, [128, 512]) for i in range(8)]
            et = [sbt(ps, "et%d" % i, [128, 512]) for i in range(2)]
            pT = [sbt(ps, "pT%d" % i, [128, 512], BF16) for i in range(3)]
            t2 = [sbt(ps, "t2_%d" % i, [128, 512]) for i in range(2)]
            ost = sbt(ps, "nost", [128, L], BF16); rden = sbt(ps, "rden", [128, 512])
            rot = K["rot"]
            qbf2 = sbt(ps, "qbf2", [128, L], BF16); kbf2 = sbt(ps, "kbf2", [128, T], BF16)
            qbfs = [qbf, qbf2]; kbfs = [kbf, kbf2]

            def prep_stages(hi_, h):
                slot = hi_ % 2
                qraw = qraws[slot]; kraw = kraws[slot]; vt = vts[slot]

                def loads():
                    mk.dma("sp", qraw[:], S.nq.t[h], reads=[S.nq], writes=[qraw])
                    mk.dma("sp", kraw[:], S.nk.t[h], reads=[S.nk], writes=[kraw])
                    mk.dma("sp", vt[:], S.nv.t.rearrange("(n p) c -> p n c", p=128)[:, :, h * 128:(h + 1) * 128], reads=[S.nv], writes=[vt])

                def normA(raw, Wd):
                    mk.op("act", lambda e, raw=raw, Wd=Wd: e.activation(sq[:, :Wd], raw[:, :Wd], AF.Square), reads=[raw], writes=[sq])
                    for (t0, w) in TOK5:
                        if t0 >= Wd:
                            continue
                        p = sbank()
                        mk.op("pe", lambda e, p=p, t0=t0, w=w: e.matmul(p[:, :w], ones[:], sq[:, t0:t0 + w], start=True, stop=True), reads=[ones, sq], writes=[p])
                        mk.op("act", lambda e, p=p, t0=t0, w=w: e.activation(rs[:, t0:t0 + w], p[:, :w], AF.Ln, bias=epsT[:, 0:1], scale=1.0 / 128.0), reads=[p, epsT], writes=[rs])
                    mk.op("act", lambda e, Wd=Wd: e.activation(rs[:, :Wd], rs[:, :Wd], AF.Exp, scale=-0.5), reads=[rs], writes=[rs])

                def normB(raw, Wd, wc, scl, obf_):
                    mk.op("dve", lambda e, raw=raw, Wd=Wd: e.tensor_tensor(t1[:, :Wd], raw[:, :Wd], rs[:, :Wd], op=ALU.mult), reads=[raw, rs], writes=[t1])
                    mk.op("dve", lambda e, Wd=Wd, wc=wc, scl=scl: e.tensor_scalar(t1[:, :Wd], t1[:, :Wd], hw[:, wc:wc + 1], scl, op0=ALU.mult, op1=ALU.mult), reads=[t1, hw], writes=[t1])
                    for ti in range(4):
                        t0 = ti * 512
                        p = sbank(); tt = t2[ti % 2]
                        mk.op("pe", lambda e, p=p, t0=t0: e.matmul(p[:, :512], rot[:], t1[:, t0:t0 + 512], start=True, stop=True), reads=[rot, t1], writes=[p])
                        mk.op("dve", lambda e, p=p, t0=t0, tt=tt: e.tensor_tensor(tt[:], p[:, :512], ropes[:, t0:t0 + 512], op=ALU.mult), reads=[p, ropes], writes=[tt])
                        mk.op("pool", lambda e, t0=t0: e.tensor_tensor(sq[:, t0:t0 + 512], t1[:, t0:t0 + 512], ropec[:, t0:t0 + 512], op=ALU.mult), reads=[t1, ropec], writes=[sq])
                        mk.op("pool", lambda e, t0=t0, tt=tt, obf_=obf_: e.tensor_tensor(obf_[:, t0:t0 + 512], sq[:, t0:t0 + 512], tt[:], op=ALU.add), reads=[sq, tt], writes=[obf_])
                    if Wd > L:
                        mk.op("act", lambda e, obf_=obf_: e.copy(obf_[:, L:T], t1[:, L:T]), reads=[t1], writes=[obf_])

                def s0():
                    loads(); normA(qraw, L)

                def s1():
                    normB(qraw, L, 1, 128.0 ** -0.5, qbfs[slot])

                def s2():
                    normA(kraw, T)

                def s3():
                    normB(kraw, T, 2, 1.0, kbfs[slot])
                return [s0, s1, s2, s3]

            for st_ in prep_stages(0, heads[0]):
                st_()
            for hi_, h in enumerate(heads):
                slot = hi_ % 2
                qbf = qbfs[slot]; kbf = kbfs[slot]; vt = vts[slot]
                nxt = prep_stages(hi_ + 1, heads[hi_ + 1]) if hi_ + 1 < len(heads) else [None] * 4
                bi = 0
                for m, (lo, nt) in enumerate(NA_BLOCKS):
                    qs = qbf[:, m * 512:(m + 1) * 512]
                    obank = pb[4 + m % 2]; dbank = pb[6 + m % 2]
                    tiles = [("l", lo + 2 * j) for j in range(nt)] + [("c", 0), ("c", 1)]
                    for ti, (kind, kr0) in enumerate(tiles):
                        sb_ = sbank()
                        if kind == "l":
                            keys = kbf[:, kr0 * 64:kr0 * 64 + 128]; V = vt[:, kr0 // 2, :]
                        else:
                            keys = kbf[:, L + kr0 * 128:L + (kr0 + 1) * 128]; V = vt[:, 16 + kr0, :]
                        mk.op("pe", lambda e, sb_=sb_, keys=keys, qs=qs: e.matmul(sb_[:, :512], keys, qs, start=True, stop=True), reads=[kbf, qbf], writes=[sb_])
                        cnt["g"] += 1
                        pt_ = pT[cnt["g"] % 3]
                        if kind == "l":
                            bt = bias[cnt["bb"] % 8]; cnt["bb"] += 1; e_ = et[cnt["g"] % 2]
                            mk.dma("sp", bt[:], I.nab[h, bi], writes=[bt])
                            bi += 1
                            mk.op("dve", lambda e, e_=e_, sb_=sb_, bt=bt: e.tensor_tensor(e_[:], sb_[:, :512], bt[:], op=ALU.add), reads=[sb_, bt], writes=[e_])
                            mk.op("act", lambda e, pt_=pt_, e_=e_: e.activation(pt_[:], e_[:], AF.Exp), reads=[e_], writes=[pt_])
                        else:
                            mk.op("act", lambda e, pt_=pt_, sb_=sb_: e.activation(pt_[:], sb_[:, :512], AF.Exp), reads=[sb_], writes=[pt_])
                        first = (ti == 0); last = (ti == len(tiles) - 1)
                        mk.op("pe", lambda e, obank=obank, V=V, pt_=pt_, first=first, last=last: e.matmul(obank[:, :512], V, pt_[:], start=first, stop=last), reads=[vt, pt_], writes=[obank])
                        mk.op("pe", lambda e, dbank=dbank, pt_=pt_, first=first, last=last: e.matmul(dbank[:, :512], onesb[:], pt_[:], start=first, stop=last), reads=[onesb, pt_], writes=[dbank])
                    mk.op("act", lambda e, dbank=dbank: e.activation(rden[:], dbank[:, :512], AF.Ln), reads=[dbank], writes=[rden])
                    mk.op("act", lambda e: e.activation(rden[:], rden[:], AF.Exp, scale=-1.0), reads=[rden], writes=[rden])
                    mk.op("dve", lambda e, obank=obank, m=m: e.tensor_tensor(ost[:, m * 512:(m + 1) * 512], obank[:, :512], rden[:], op=ALU.mult), reads=[obank, rden], writes=[ost])
                    if nxt[m] is not None:
                        nxt[m]()
                mk.dma("pool", S.ona.t[h], ost[:], reads=[ost], writes=[S.ona])
        mk.barrier()

    g.phase_na = phase_na
    a2 = X["a2"]; norm_mod = X["norm_mod"]

    def make_wloader(ps, nbuf=4):
        stg = [sbt(ps, "stg%d" % i, [128, 4096]) for i in range(2)]
        wbf = [sbt(ps, "wld%d" % i, [128, 4096], BF16) for i in range(nbuf)]
        st = {"i": 0}

        def load(view, a, b):
            i = st["i"]; st["i"] += 1
            s_ = stg[i % 2]; w_ = wbf[i % nbuf]
            mk.dma("sp", s_[:].rearrange("p (a b) -> p a b", b=b), view, writes=[s_])
            mk.op("act", lambda e: e.copy(w_[:, 0:2048], s_[:, 0:2048]), reads=[s_], writes=[w_])
            mk.op("dve", lambda e: e.tensor_copy(w_[:, 2048:4096], s_[:, 2048:4096]), reads=[s_], writes=[w_])
            return w_
        return load

    def pipelined(n, loadfn, computefn):
        cur = loadfn(0)
        for i in range(n):
            nxt = loadfn(i + 1) if i + 1 < n else None
            computefn(i, cur)
            cur = nxt

    def phase_merge():
        with ExitStack() as ps:
            pb = [pst(ps, "mg%d" % i, [128, 512]) for i in range(8)]
            cnt = {"b": 0}

            def bank():
                cnt["b"] += 1
                return pb[cnt["b"] % 8]
            load = make_wloader(ps, nbuf=6)
            odn = sbt(ps, "odn", [128, 16, 1024], BF16); ona = sbt(ps, "ona", [128, 16, 1024], BF16)
            sa = [sbt(ps, "sa%d" % i, [128, 1024]) for i in range(2)]; sb_ = [sbt(ps, "sbb%d" % i, [128, 1024]) for i in range(2)]
            ta = [sbt(ps, "ta%d" % i, [128, 512]) for i in range(2)]; tb = [sbt(ps, "tb%d" % i, [128, 512]) for i in range(2)]
            yst = [sbt(ps, "yst%d" % i, [128, 1024], BF16) for i in range(2)]
            wav = I.w_a.rearrange("(k p) n -> p k n", p=128); wbv = I.w_b.rearrange("(k p) n -> p k n", p=128)
            for half in range(2):
                th0 = half * 1024
                mk.dma("sp", odn[:], S.odn.t.rearrange("h p t -> p h t")[:, :, th0:th0 + 1024], reads=[S.odn], writes=[odn])
                mk.dma("sp", ona[:], S.ona.t.rearrange("h p t -> p h t")[:, :, th0:th0 + 1024], reads=[S.ona], writes=[ona])

                def ld(cb):
                    return (load(wav[:, :, cb * 256:(cb + 1) * 256], 16, 256), load(wbv[:, :, cb * 256:(cb + 1) * 256], 16, 256))

                def comp(cb, ws):
                    wa_, wb_ = ws
                    for ct in range(2):
                        dt = cb * 2 + ct
                        sA = sa[dt % 2]; sB = sb_[dt % 2]; ys = yst[dt % 2]
                        mk.dma("sp", sA[:], S.ga.t[dt][:, th0:th0 + 1024], reads=[S.ga], writes=[sA])
                        mk.dma("sp", sB[:], S.gb.t[dt][:, th0:th0 + 1024], reads=[S.gb], writes=[sB])
                        for tq in range(2):
                            t0 = tq * 512
                            pA = bank(); pB = bank()
                            for k in range(16):
                                mk.op("pe", lambda e, pA=pA, k=k, ct=ct, t0=t0: e.matmul(pA[:, :512], wa_[:, k * 256 + ct * 128:k * 256 + (ct + 1) * 128], odn[:, k, t0:t0 + 512], start=(k == 0), stop=(k == 15)), reads=[wa_, odn], writes=[pA])
                            for k in range(16):
                                mk.op("pe", lambda e, pB=pB, k=k, ct=ct, t0=t0: e.matmul(pB[:, :512], wb_[:, k * 256 + ct * 128:k * 256 + (ct + 1) * 128], ona[:, k, t0:t0 + 512], start=(k == 0), stop=(k == 15)), reads=[wb_, ona], writes=[pB])
                            a_ = ta[tq]; b_ = tb[tq]
                            mk.op("dve", lambda e, a_=a_, pA=pA, sA=sA, t0=t0: e.tensor_tensor(a_[:], pA[:, :512], sA[:, t0:t0 + 512], op=ALU.mult), reads=[pA, sA], writes=[a_])
                            mk.op("dve", lambda e, b_=b_, pB=pB, sB=sB, t0=t0: e.tensor_tensor(b_[:], pB[:, :512], sB[:, t0:t0 + 512], op=ALU.mult), reads=[pB, sB], writes=[b_])
                            mk.op("pool", lambda e, a_=a_, b_=b_, ys=ys, t0=t0: e.tensor_tensor(ys[:, t0:t0 + 512], a_[:], b_[:], op=ALU.add), reads=[a_, b_], writes=[ys])
                        mk.dma("sp", S.y.t[dt][:, th0:th0 + 1024], ys[:], reads=[ys], writes=[S.y])
                pipelined(8, ld, comp)
            mk.barrier()
            yT = odn
            xt = [sbt(ps, "mxt%d" % i, [128, 1024]) for i in range(2)]
            xst = [sbt(ps, "mxs%d" % i, [128, 1024]) for i in range(2)]
            wov = I.w_out.rearrange("(k p) n -> p k n", p=128)
            for half in range(2):
                th0 = half * 1024
                mk.dma("sp", yT[:], S.y.t.rearrange("h p t -> p h t")[:, :, th0:th0 + 1024], reads=[S.y], writes=[yT])

                def ld2(cb):
                    return load(wov[:, :, cb * 256:(cb + 1) * 256], 16, 256)

                def comp2(cb, wo_):
                    for ct in range(2):
                        dt = cb * 2 + ct
                        x_ = xt[dt % 2]; xs = xst[dt % 2]
                        mk.dma("sp", x_[:], I.xT[dt][:, th0:th0 + 1024], writes=[x_])
                        for tq in range(2):
                            t0 = tq * 512
                            p = bank()
                            for k in range(16):
                                mk.op("pe", lambda e, p=p, k=k, ct=ct, t0=t0: e.matmul(p[:, :512], wo_[:, k * 256 + ct * 128:k * 256 + (ct + 1) * 128], yT[:, k, t0:t0 + 512], start=(k == 0), stop=(k == 15)), reads=[wo_, yT], writes=[p])
                            mk.op("dve", lambda e, p=p, xs=xs, x_=x_, dt=dt, t0=t0: e.scalar_tensor_tensor(xs[:, t0:t0 + 512], p[:, :512], modsb[:, 32 + dt, 0:1], x_[:, t0:t0 + 512], op0=ALU.mult, op1=ALU.add), reads=[p, modsb, x_], writes=[xs])
                        mk.dma("sp", S.x1.t[dt][:, th0:th0 + 1024], xs[:], reads=[xs], writes=[S.x1])
                pipelined(8, ld2, comp2)
        mk.barrier()

    g.phase_merge = phase_merge

    def phase_moe():
        with ExitStack() as ps:
            pb = [pst(ps, "me%d" % i, [128, 512]) for i in range(6)]
            pbt = [pst(ps, "met%d" % i, [128, 1024], BF16) for i in range(2)]
            cnt = {"b": 0, "t": 0}

            def bank():
                cnt["b"] += 1
                return pb[cnt["b"] % 6]
            h2tok = sbt(ps, "h2tok", [128, 16, D], BF16)
            logits = sbt(ps, "logits", [128, 16, 16])
            wr = sbt(ps, "wr", [128, 16, 16])
            iota = sbt(ps, "iota", [128, 258])
            mk.dma("sp", wr[:], I.w_r.rearrange("(k p) e -> p k e", p=128), writes=[wr])
            mk.dma("sp", iota[:], I.iota, writes=[iota])
            affT = sbt(ps, "affT", [16, L]); work = sbt(ps, "work", [16, L]); rkmT = sbt(ps, "rkmT", [16, L]); gwT = sbt(ps, "gwT", [16, L])
            rkm = sbt(ps, "rkm", [128, 16, 16])
            with ExitStack() as p1:
                xts = [sbt(p1, "mx%d" % i, [128, 16, 512]) for i in range(1)]
                sq = sbt(p1, "msq", [128, 16, 512]); rs = sbt(p1, "mrs", [128, 512])
                tmp2 = [sbt(p1, "mtmp%d" % i, [128, 512]) for i in range(2)]
                h2f = sq; h2b = sbt(p1, "h2b", [128, 16, 512], BF16)
                xv = S.x1.t.rearrange("k p t -> p k t")
                for tt in range(4):
                    t0 = tt * 512
                    xt = xts[0]
                    mk.dma("sp", xt[:], xv[:, :, t0:t0 + 512], reads=[S.x1], writes=[xt])
                    norm_mod(bank(), xt, sq, rs, tmp2, 512, a2, (lambda k: modsb[:, 48 + k, 0:1]), lambda k: (h2f[:, k, :], h2f))
                    for tc in range(4):
                        p = bank()
                        for k in range(16):
                            mk.op("pe", lambda e, p=p, k=k, tc=tc: e.matmul(p[:, :16], h2f[:, k, tc * 128:(tc + 1) * 128], wr[:, k, :], start=(k == 0), stop=(k == 15)), reads=[h2f, wr], writes=[p])
                        mk.op("dve", lambda e, p=p, tt=tt, tc=tc: e.tensor_copy(logits[:, tt * 4 + tc, :], p[:, :16]), reads=[p], writes=[logits])
                    mk.op("act", lambda e: e.copy(h2b[:], h2f[:]), reads=[h2f], writes=[h2b])
                    for tc in range(4):
                        for kg in range(2):
                            cnt["t"] += 1
                            pt = pbt[cnt["t"] % 2]
                            for kk in range(8):
                                k = kg * 8 + kk
                                mk.op("pe", lambda e, pt=pt, kk=kk, k=k, tc=tc: e.transpose(pt[:, kk * 128:(kk + 1) * 128], h2b[:, k, tc * 128:(tc + 1) * 128], identb[:]), reads=[h2b, identb], writes=[pt])
                            eng = "dve" if (tc + kg) % 2 == 0 else "act"
                            dst = h2tok[:, tt * 4 + tc, kg * 1024:(kg + 1) * 1024]
                            if eng == "dve":
                                mk.op("dve", lambda e, pt=pt, dst=dst: e.tensor_copy(dst, pt[:]), reads=[pt], writes=[h2tok])
                            else:
                                mk.op("act", lambda e, pt=pt, dst=dst: e.copy(dst, pt[:]), reads=[pt], writes=[h2tok])
                mx = sbt(p1, "mx_", [128, 16]); sm = sbt(p1, "sm_", [128, 16])
                for tc in range(16):
                    mk.op("dve", lambda e, tc=tc: e.reduce_max(mx[:, tc:tc + 1], logits[:, tc, :], axis=AX.X), reads=[logits], writes=[mx])
                mk.op("dve", lambda e: e.tensor_scalar(mx[:], mx[:], -1.0, None, op0=ALU.mult), reads=[mx], writes=[mx])
                for tc in range(16):
                    mk.op("act", lambda e, tc=tc: e.activation(logits[:, tc, :], logits[:, tc, :], AF.Exp, bias=mx[:, tc:tc + 1], scale=1.0, accum_out=sm[:, tc:tc + 1]), reads=[logits, mx], writes=[logits, sm])
                mk.op("dve", lambda e: e.reciprocal(sm[:], sm[:]), reads=[sm], writes=[sm])
                for tc in range(16):
                    mk.op("dve", lambda e, tc=tc: e.tensor_scalar(logits[:, tc, :], logits[:, tc, :], sm[:, tc:tc + 1], None, op0=ALU.mult), reads=[logits, sm], writes=[logits])
                for g4 in range(4):
                    p = bank()
                    for q_ in range(4):
                        tc = g4 * 4 + q_
                        mk.op("pe", lambda e, p=p, q_=q_, tc=tc: e.transpose(p[0:16, q_ * 128:(q_ + 1) * 128], logits[:, tc, :], ident[:]), reads=[logits, ident], writes=[p])
                    mk.op("dve", lambda e, p=p, g4=g4: e.tensor_copy(affT[:, g4 * 512:(g4 + 1) * 512], p[0:16, :512]), reads=[p], writes=[affT])
                mk.op("dve", lambda e: e.tensor_copy(work[:], affT[:]), reads=[affT], writes=[work])
                m8 = sbt(p1, "m8", [16, 8])
                for r_ in range(32):
                    mk.op("dve", lambda e: e.max(m8[:], work[:]), reads=[work], writes=[m8])
                    if r_ < 31:
                        mk.op("dve", lambda e: e.match_replace(work[:], m8[:], work[:], -1.0), reads=[work, m8], writes=[work])
                mk.op("dve", lambda e: e.tensor_scalar(work[:], affT[:], m8[:, 7:8], None, op0=ALU.is_ge), reads=[affT, m8], writes=[work])
                mk.op("dve", lambda e: e.tensor_tensor(gwT[:], affT[:], work[:], op=ALU.mult), reads=[affT, work], writes=[gwT])
                onesr = sbt(p1, "onesr", [16, L])
                mk.op("pool", lambda e: e.memset(onesr[:], 1.0), writes=[onesr])
                mk.op("dve", lambda e: e.tensor_tensor_scan(rkmT[:], onesr[:], work[:], 0.0, op0=ALU.mult, op1=ALU.add), reads=[onesr, work], writes=[rkmT])
                mk.op("dve", lambda e: e.tensor_tensor(rkmT[:], rkmT[:], work[:], op=ALU.mult), reads=[rkmT, work], writes=[rkmT])
                mk.op("dve", lambda e: e.tensor_scalar(rkmT[:], rkmT[:], -1.0, None, op0=ALU.add), reads=[rkmT], writes=[rkmT])
                for g4 in range(4):
                    p = bank()
                    for q_ in range(4):
                        tc = g4 * 4 + q_
                        mk.op("pe", lambda e, p=p, q_=q_, tc=tc: e.transpose(p[:, q_ * 16:(q_ + 1) * 16], rkmT[:, tc * 128:(tc + 1) * 128], ident[0:16, 0:16]), reads=[rkmT, ident], writes=[p])
                    mk.op("dve", lambda e, p=p, g4=g4: e.tensor_copy(rkm[:, g4 * 4:(g4 + 1) * 4, :], p[:, :64].rearrange("p (a b) -> p a b", b=16)), reads=[p], writes=[rkm])
            mk.barrier()
            with ExitStack() as p2:
                load = make_wloader(p2, nbuf=4)
                sel = [sbt(p2, "sel%d" % i, [128, 16, 256], BF16) for i in range(2)]
                xg = sbt(p2, "xg", [128, 16, 256], BF16)
                hid = sbt(p2, "hid", [128, 8, 256], BF16)
                st_ = [sbt(p2, "sil%d" % i, [128, 256]) for i in range(2)]
                yest = [sbt(p2, "yest%d" % i, [128, D], BF16) for i in range(2)]
                for ex in range(16):
                    sl_ = sel[ex % 2]
                    for tc in range(16):
                        eng = "dve"
                        mk.op(eng, lambda e, sl_=sl_, tc=tc, ex=ex: e.tensor_scalar(sl_[:, tc, :], iota[:, 0:256], rkm[:, tc, ex:ex + 1], None, op0=ALU.is_equal), reads=[iota, rkm], writes=[sl_])
                    for Dc in range(16):
                        p = bank()
                        for tc in range(16):
                            mk.op("pe", lambda e, p=p, tc=tc, Dc=Dc, sl_=sl_: e.matmul(p[:, :256], h2tok[:, tc, Dc * 128:(Dc + 1) * 128], sl_[:, tc, :], start=(tc == 0), stop=(tc == 15)), reads=[h2tok, sl_], writes=[p])
                        if Dc % 2 == 0:
                            mk.op("dve", lambda e, p=p, Dc=Dc: e.tensor_copy(xg[:, Dc, :], p[:, :256]), reads=[p], writes=[xg])
                        else:
                            mk.op("act", lambda e, p=p, Dc=Dc: e.copy(xg[:, Dc, :], p[:, :256]), reads=[p], writes=[xg])
                    w1v = I.ew1[ex].rearrange("(k p) f -> p k f", p=128); w3v = I.ew3[ex].rearrange("(k p) f -> p k f", p=128)

                    def ld(fb, w1v=w1v, w3v=w3v):
                        return (load(w1v[:, :, fb * 256:(fb + 1) * 256], 16, 256), load(w3v[:, :, fb * 256:(fb + 1) * 256], 16, 256))

                    def comp(fb, ws):
                        w1_, w3_ = ws
                        for ft in range(2):
                            p1_ = bank(); p3_ = bank()
                            for k in range(16):
                                mk.op("pe", lambda e, p1_=p1_, k=k, ft=ft: e.matmul(p1_[:, :256], w1_[:, k * 256 + ft * 128:k * 256 + (ft + 1) * 128], xg[:, k, :], start=(k == 0), stop=(k == 15)), reads=[w1_, xg], writes=[p1_])
                            for k in range(16):
                                mk.op("pe", lambda e, p3_=p3_, k=k, ft=ft: e.matmul(p3_[:, :256], w3_[:, k * 256 + ft * 128:k * 256 + (ft + 1) * 128], xg[:, k, :], start=(k == 0), stop=(k == 15)), reads=[w3_, xg], writes=[p3_])
                            s_ = st_[ft]
                            mk.op("act", lambda e, s_=s_, p1_=p1_: e.activation(s_[:], p1_[:, :256], AF.Silu), reads=[p1_], writes=[s_])
                            mk.op("dve", lambda e, s_=s_, p3_=p3_, fb=fb, ft=ft: e.tensor_tensor(hid[:, fb * 2 + ft, :], s_[:], p3_[:, :256], op=ALU.mult), reads=[s_, p3_], writes=[hid])
                    pipelined(4, ld, comp)
                    w2v = I.ew2[ex].rearrange("(k p) n -> p k n", p=128)

                    def ld2(nb, w2v=w2v):
                        return load(w2v[:, :, nb * 512:(nb + 1) * 512], 8, 512)

                    def comp2(nb, w2_):
                        for s2 in range(2):
                            p = bank(); ys = yest[s2]
                            for f in range(8):
                                mk.op("pe", lambda e, p=p, f=f, s2=s2: e.matmul(p[:, :512], hid[:, f, s2 * 128:(s2 + 1) * 128], w2_[:, f * 512:(f + 1) * 512], start=(f == 0), stop=(f == 7)), reads=[hid, w2_], writes=[p])
                            if s2 == 0:
                                mk.op("dve", lambda e, p=p, ys=ys, nb=nb: e.tensor_copy(ys[:, nb * 512:(nb + 1) * 512], p[:, :512]), reads=[p], writes=[ys])
                            else:
                                mk.op("act", lambda e, p=p, ys=ys, nb=nb: e.copy(ys[:, nb * 512:(nb + 1) * 512], p[:, :512]), reads=[p], writes=[ys])
                    pipelined(4, ld2, comp2)
                    for s2 in range(2):
                        mk.dma("sp", S.ye.t[ex * 2 + s2], yest[s2][:], reads=[yest[s2]], writes=[S.ye])
            mk.barrier()
            with ExitStack() as p3:
                selT = sbt(p3, "selT", [16, 16, 128])
                for ex in range(16):
                    mk.op("dve", lambda e, ex=ex: e.tensor_copy(selT[:, ex, :], ident[0:16, ex:ex + 1].to_broadcast([16, 128])), reads=[ident], writes=[selT])
                SGT = sbt(p3, "SGT", [128, 32, 512], BF16)
                gwB = [sbt(p3, "gwB%d" % i, [128, 512]) for i in range(2)]
                yeD = [sbt(p3, "yeD%d" % i, [128, 32, 128], BF16) for i in range(2)]
                x1t = [sbt(p3, "x1t%d" % i, [128, 512]) for i in range(2)]
                ot = [sbt(p3, "ot%d" % i, [128, 512]) for i in range(2)]
                yev = S.ye.t.rearrange("q p d -> p q d")
                for tt in range(4):
                    t0 = tt * 512
                    for ex in range(16):
                        pR = bank(); pG = bank(); gb_ = gwB[ex % 2]
                        mk.op("pe", lambda e, pR=pR, ex=ex, t0=t0: e.matmul(pR[:, :512], selT[:, ex, :], rkmT[:, t0:t0 + 512], start=True, stop=True), reads=[selT, rkmT], writes=[pR])
                        mk.op("pe", lambda e, pG=pG, ex=ex, t0=t0: e.matmul(pG[:, :512], selT[:, ex, :], gwT[:, t0:t0 + 512], start=True, stop=True), reads=[selT, gwT], writes=[pG])
                        mk.op("act", lambda e, gb_=gb_, pG=pG: e.copy(gb_[:], pG[:, :512]), reads=[pG], writes=[gb_])
                        for s2 in range(2):
                            mk.op("dve", lambda e, pR=pR, gb_=gb_, ex=ex, s2=s2: e.scalar_tensor_tensor(SGT[:, ex * 2 + s2, :], pR[:, :512], iota[:, 256 + s2:257 + s2], gb_[:], op0=ALU.is_equal, op1=ALU.mult), reads=[pR, iota, gb_], writes=[SGT])
                    for Dc in range(16):
                        yd = yeD[Dc % 2]; x_ = x1t[Dc % 2]; o_ = ot[Dc % 2]
                        mk.dma("sp", yd[:], yev[:, :, Dc * 128:(Dc + 1) * 128], reads=[S.ye], writes=[yd])
                        mk.dma("sp", x_[:], S.x1.t[Dc][:, t0:t0 + 512], reads=[S.x1], writes=[x_])
                        p = bank()
                        for q_ in range(32):
                            mk.op("pe", lambda e, p=p, q_=q_, yd=yd: e.matmul(p[:, :512], yd[:, q_, :], SGT[:, q_, :], start=(q_ == 0), stop=(q_ == 31)), reads=[yd, SGT], writes=[p])
                        mk.op("dve", lambda e, p=p, o_=o_, x_=x_, Dc=Dc: e.scalar_tensor_tensor(o_[:], p[:, :512], modsb[:, 80 + Dc, 0:1], x_[:], op0=ALU.mult, op1=ALU.add), reads=[p, modsb, x_], writes=[o_])
                        mk.dma("sp", outT.t[Dc][:, t0:t0 + 512], o_[:], reads=[o_], writes=[outT])
        mk.barrier()

    g.phase_moe = phase_moe


def prep_shared(inp):
    f = np.float32
    sh = {}
    sh["ada_w"] = np.ascontiguousarray(inp["ada_w"][0], f)
    sh["ada_b"] = np.ascontiguousarray(inp["ada_b"][0].reshape(96, 128).T, f)
    sh["n1w"] = np.ascontiguousarray(inp["norm1_w"][0].reshape(16, 128).T, f)
    sh["n2w"] = np.ascontiguousarray(inp["norm2_w"][0].reshape(16, 128).T, f)
    sh["w_in"] = np.ascontiguousarray(inp["w_in"][0], f)
    sh["conv_w"] = np.ascontiguousarray(inp["conv_w"][0].T.reshape(48, 128, 5).transpose(1, 0, 2), f)
    sc = np.concatenate([inp["dn_a_log"][0].reshape(-1), inp["dn_dt_bias"][0].reshape(-1)]).astype(f)
    sh["dn_sc"] = np.ascontiguousarray(np.broadcast_to(sc[None, :], (128, 64)), f)
    sh["hw"] = np.ascontiguousarray(np.stack([inp["dn_norm_w"][0], inp["na_q_norm_w"][0], inp["na_k_norm_w"][0]], axis=1), f)
    c = _consts()
    sh["consts"] = np.ascontiguousarray(np.stack([c[n] for n in CONST_NAMES]), f)
    sh["ropec"], sh["ropes"] = _rope_tables()
    sh["nab"] = _na_bias_table(np.asarray(inp["na_rpb"][0], f))
    io = np.zeros((128, 258), f)
    io[:, :256] = np.arange(256, dtype=f)[None, :]
    io[:, 256] = np.arange(128, dtype=f)
    io[:, 257] = np.arange(128, dtype=f) + 128
    sh["iota"] = io
    ii = np.arange(128)
    mks = [(ii[:, None] // 16 == ii[None, :] // 16)]
    for b_ in (16, 32, 64):
        same = (ii[:, None] // (2 * b_) == ii[None, :] // (2 * b_))
        hi = (ii % (2 * b_)) >= b_
        mks.append(same & (hi[:, None] != hi[None, :]))
    sh["dnmask"] = np.ascontiguousarray(np.stack(mks).astype(f))
    sh["w_a"] = np.ascontiguousarray(inp["w_branch_a"][0], f)
    sh["w_b"] = np.ascontiguousarray(inp["w_branch_b"][0], f)
    sh["w_out"] = np.ascontiguousarray(inp["w_out"][0], f)
    sh["w_r"] = np.ascontiguousarray(inp["w_router"][0], f)
    sh["ew1"] = np.ascontiguousarray(inp["expert_w1"][0], f)
    sh["ew3"] = np.ascontiguousarray(inp["expert_w3"][0], f)
    sh["ew2"] = np.ascontiguousarray(inp["expert_w2"][0], f)
    return sh


def prep_core(inp, b):
    f = np.float32
    m = {}
    xc = np.concatenate([inp["x"][b], inp["ctx"][b]], axis=0)
    m["xT"] = np.ascontiguousarray(xc.T.reshape(16, 128, T), f)
    cs = np.stack([inp["c"][b].reshape(16, 128), inp["c_ctx"].reshape(16, 128)], axis=-1)
    m["cs"] = np.ascontiguousarray(cs.transpose(1, 0, 2).reshape(128, 32), f)
    return m


_CACHE = {}


def kernel(**inputs):
    inp = {k: np.asarray(v) for k, v in inputs.items()}
    if "nc" not in _CACHE:
        _CACHE["nc"] = build_nc()[0]
    nc = _CACHE["nc"]
    sh = prep_shared(inp)
    in_maps = []
    for b in range(8):
        m = dict(sh)
        m.update(prep_core(inp, b))
        in_maps.append(m)
    res = run_bass_kernel_spmd(nc, in_maps, core_ids=list(range(8)))
    out = np.stack([np.asarray(r["outT"], np.float32).reshape(D, L).T for r in res.results], axis=0)
    return np.ascontiguousarray(out, np.float32)
```

```python
import os
import numpy as np
from contextlib import ExitStack
import ml_dtypes
import concourse.bass as bass
import concourse.mybir as mybir
from concourse.bass_utils import run_bass_kernel_spmd

F32 = mybir.dt.float32
BF16 = mybir.dt.bfloat16
F32R = mybir.dt.float32r
ALU = mybir.AluOpType
AF = mybir.ActivationFunctionType
AX = mybir.AxisListType

D = 2048
L = 2048
CTX = 256
T = L + CTX
INW = 18496
NCH = T // 128
EPS = 1e-6
SEM_ROLL = 30000


class Buf:
    __slots__ = ("name", "lw", "rd", "excl")

    def __init__(self, name=""):
        self.name = name
        self.lw = None
        self.rd = []
        self.excl = False


class TL:
    def __init__(self, t, name=""):
        self.t = t
        self.b = Buf(name)

    def __getitem__(self, k):
        return self.t[k]


def _b(x):
    return x.b if isinstance(x, TL) else x


class MK:
    ENG = ("pe", "act", "dve", "pool", "sp")

    def __init__(self, nc, es):
        self.nc = nc
        self.es = es
        self.eo = {"pe": nc.tensor, "act": nc.scalar, "dve": nc.vector, "pool": nc.gpsimd, "sp": nc.sync}
        self.cnt = {e: 0 for e in self.ENG}
        self.semi = 0
        self.cur = {}
        for e in self.ENG:
            self.cur[e] = self._newsem()
        self.seen = {e: {} for e in self.ENG}
        self.dsem = {}
        self.dpos = {}
        for q in ("sp", "act", "pool"):
            n = 16 if q == "sp" else 4
            self.dsem[q] = [[self._newsem(), 0, None] for _ in range(n)]
            self.dpos[q] = 0
        self.ninst = 0

    def _newsem(self):
        self.semi += 1
        return self.es.enter_context(self.nc.semaphore("s%d" % self.semi))

    def _wait(self, e, ev):
        if ev is None:
            return
        sem, val, src = ev
        if src == e and e == "pe":
            return
        key = id(sem)
        if self.seen[e].get(key, 0) >= val:
            return
        self.seen[e][key] = val
        self.eo[e].wait_ge(sem, val)

    def _deps(self, e, reads, writes):
        for b in reads:
            self._wait(e, b.lw)
        for b in writes:
            self._wait(e, b.lw)
            for r in b.rd:
                self._wait(e, r)

    def _commit(self, ev, reads, writes):
        for b in writes:
            b.lw = ev
            b.rd = []
        for b in reads:
            if b not in writes:
                b.rd.append(ev)
                if len(b.rd) > 16:
                    d = {}
                    for r in b.rd:
                        k = id(r[0])
                        if k not in d or d[k][1] < r[1]:
                            d[k] = r
                    b.rd = list(d.values())

    def op(self, e, fn, reads=(), writes=()):
        reads = [_b(x) for x in reads]
        writes = [_b(x) for x in writes]
        writes = writes + [b for b in reads if b.excl and b not in writes]
        reads = [b for b in reads if not b.excl]
        self._deps(e, reads, writes)
        if self.cnt[e] >= SEM_ROLL:
            self.cur[e] = self._newsem()
            self.cnt[e] = 0
        self.cnt[e] += 1
        ev = (self.cur[e], self.cnt[e], e)
        fn(self.eo[e]).then_inc(ev[0], 1)
        self._commit(ev, reads, writes)
        self.ninst += 1
        return ev

    def dma(self, q, out, in_, reads=(), writes=(), **kw):
        reads = [_b(x) for x in reads]
        writes = [_b(x) for x in writes]
        self._deps(q, reads, writes)
        slot = self.dsem[q][self.dpos[q]]
        self.dpos[q] = (self.dpos[q] + 1) % len(self.dsem[q])
        if slot[2] is not None:
            self._wait(q, slot[2])
        slot[1] += 16
        ev = (slot[0], slot[1], "dma")
        slot[2] = ev
        self.eo[q].dma_start(out=out, in_=in_, **kw).then_inc(slot[0], 16)
        self._commit(ev, reads, writes)
        self.ninst += 1
        return ev

    def barrier(self):
        evs = []
        for e in self.ENG:
            if self.cnt[e] > 0:
                evs.append((self.cur[e], self.cnt[e], "x"))
        for q in self.dsem:
            for slot in self.dsem[q]:
                if slot[2] is not None:
                    evs.append(slot[2])
        for e in self.ENG:
            for ev in evs:
                self._wait(e, ev)


class Ctx:
    pass


_UID = [0]


def _alloc(nc, stack, kind, name, shape, dt):
    f = nc.sbuf_tensor if kind == "sb" else nc.psum_tensor
    _UID[0] += 1
    name = "%s_%s_%d" % (kind, name, _UID[0])
    tl = TL(stack.enter_context(f(name, list(shape), dt)), name)
    if kind == "ps":
        tl.b.excl = True
    return tl


def _consts():
    c = {}
    i = np.arange(128)
    c["ident"] = np.eye(128, dtype=np.float32)
    c["ones"] = np.ones((128, 128), np.float32)
    c["uf"] = (i[:, None] <= i[None, :]).astype(np.float32)
    c["ub"] = (i[:, None] >= i[None, :]).astype(np.float32)
    NEG = -1.0e6
    c["mf"] = np.where(i[None, :] >= i[:, None], 0.0, NEG).astype(np.float32)
    c["mb"] = np.where(i[None, :] <= i[:, None], 0.0, NEG).astype(np.float32)
    c["offd"] = (1.0 - np.eye(128)).astype(np.float32)
    R = np.zeros((128, 128), np.float32)
    for p in range(128):
        q = p + 32 if (p % 64) < 32 else p - 32
        R[q, p] = 1.0
    c["rot"] = R
    return c


CONST_NAMES = ["ident", "ones", "uf", "ub", "mf", "mb", "offd", "rot"]


def _rope_tables():
    pos = np.arange(L)
    row = (pos // 64).astype(np.float32)
    col = (pos % 64).astype(np.float32)
    half = 64
    inv = (np.float32(10000.0) ** (-np.arange(0, half, 2, dtype=np.float32) / np.float32(half))).astype(np.float32)
    ang_r = row[:, None] * inv[None, :]
    ang_c = col[:, None] * inv[None, :]
    cr, sr, cc, sc = np.cos(ang_r), np.sin(ang_r), np.cos(ang_c), np.sin(ang_c)
    COS = np.concatenate([cr, cr, cc, cc], axis=1).T.astype(np.float32)
    SIN = np.concatenate([-sr, sr, -sc, sc], axis=1).T.astype(np.float32)
    return np.ascontiguousarray(COS), np.ascontiguousarray(SIN)


NA_BLOCKS = [(0, 6), (4, 8), (12, 8), (20, 6)]


def _na_bias_table(rpb):
    H = rpb.shape[0]
    out = np.full((H, 28, 128, 512), -30000.0, np.float32)
    ti = 0
    qq = np.arange(512)
    qr_l, qc = qq // 64, qq % 64
    kk = np.arange(128)
    kr_l, kc = kk // 64, kk % 64
    qstart = np.clip(qc - 8, 0, 48)
    for m, (lo, nt) in enumerate(NA_BLOCKS):
        qr = 8 * m + qr_l
        rs = np.clip(qr - 4, 0, 24)
        for j in range(nt):
            kr = lo + 2 * j + kr_l
            okr = (kr[:, None] >= rs[None, :]) & (kr[:, None] < rs[None, :] + 8) & (kr[:, None] < 32)
            okc = (kc[:, None] >= qstart[None, :]) & (kc[:, None] < qstart[None, :] + 16)
            ok = okr & okc
            dr = np.clip(kr[:, None] - qr[None, :] + 7, 0, 14)
            dc = np.clip(kc[:, None] - qc[None, :] + 15, 0, 30)
            g = rpb[:, dr, dc]
            out[:, ti] = np.where(ok[None], g, np.float32(-30000.0))
            ti += 1
    return out


def build_nc(upto=99, dump=(), feed=(), run=None, heads=range(16)):
    nc = bass.Bass("TRN2", target_bir_lowering=False)
    g = Ctx()
    g.nc = nc

    def din(name, shape, dt=F32):
        return nc.dram_tensor(name, list(shape), dt, kind="ExternalInput").ap()

    def dscr(name, shape, dt=F32):
        kind = "ExternalOutput" if name in dump else ("ExternalInput" if name in feed else "Internal")
        return TL(nc.dram_tensor(name, list(shape), dt, kind=kind).ap(), name)

    I = Ctx()
    I.xT = din("xT", [16, 128, T])
    I.cs = din("cs", [128, 32])
    I.ada_w = din("ada_w", [D, 6 * D])
    I.ada_b = din("ada_b", [128, 96])
    I.n1w = din("n1w", [128, 16])
    I.n2w = din("n2w", [128, 16])
    I.w_in = din("w_in", [D, INW])
    I.conv_w = din("conv_w", [128, 48, 5])
    I.dn_sc = din("dn_sc", [128, 64])
    I.hw = din("hw", [128, 3])
    I.consts = din("consts", [len(CONST_NAMES), 128, 128])
    I.ropec = din("ropec", [128, L])
    I.ropes = din("ropes", [128, L])
    I.nab = din("nab", [16, 28, 128, 512])
    I.iota = din("iota", [128, 258])
    I.dnmask = din("dnmask", [4, 128, 128])
    I.w_a = din("w_a", [D, D])
    I.w_b = din("w_b", [D, D])
    I.w_out = din("w_out", [D, D])
    I.w_r = din("w_r", [D, 16])
    I.ew1 = din("ew1", [16, D, 1024])
    I.ew3 = din("ew3", [16, D, 1024])
    I.ew2 = din("ew2", [16, 1024, D])
    outT = TL(nc.dram_tensor("outT", [16, 128, L], F32, kind="ExternalOutput").ap(), "outT")

    S = Ctx()
    S.dn = dscr("s_dn", [48, 128, T])
    S.z = dscr("s_z", [16, 128, L])
    S.ba = dscr("s_ba", [T, 64])
    S.nq = dscr("s_nq", [16, 128, L])
    S.nk = dscr("s_nk", [16, 128, T])
    S.nv = dscr("s_nv", [T, D], BF16)
    S.ga = dscr("s_ga", [16, 128, L])
    S.gb = dscr("s_gb", [16, 128, L])
    S.odn = dscr("s_odn", [16, 128, L], BF16)
    S.ona = dscr("s_ona", [16, 128, L], BF16)
    S.y = dscr("s_y", [16, 128, L], BF16)
    S.x1 = dscr("s_x1", [16, 128, L])
    S.ye = dscr("s_ye", [32, 128, D], BF16)
    S.hT = dscr("s_hT", [16, 128, T], BF16)
    S.grow = dscr("s_grow", [32, T])
    S.brow = dscr("s_brow", [32, T])

    with ExitStack() as es:
        mk = MK(nc, es)
        g.mk = mk

        def sbt(stack, name, shape, dt=F32):
            return _alloc(nc, stack, "sb", name, shape, dt)

        def pst(stack, name, shape, dt=F32):
            return _alloc(nc, stack, "ps", name, shape, dt)

        K = {}
        for i, n in enumerate(CONST_NAMES):
            K[n] = sbt(es, "k_" + n, [128, 128])
            mk.dma("sp", K[n][:], I.consts[i], writes=[K[n]])
        onesb = sbt(es, "onesb", [128, 128], BF16)
        identb = sbt(es, "identb", [128, 128], BF16)
        mk.op("dve", lambda e: e.tensor_copy(onesb[:], K["ones"][:]), reads=[K["ones"]], writes=[onesb])
        mk.op("dve", lambda e: e.tensor_copy(identb[:], K["ident"][:]), reads=[K["ident"]], writes=[identb])
        epsT = sbt(es, "epsT", [128, 1])
        mk.op("dve", lambda e: e.memset(epsT[:], EPS), writes=[epsT])
        modsb = sbt(es, "modsb", [128, 96, 2])
        a1 = sbt(es, "a1", [128, 16]); a1c = sbt(es, "a1c", [128, 16]); a2 = sbt(es, "a2", [128, 16])
        n1w = sbt(es, "n1w", [128, 16]); n2w = sbt(es, "n2w", [128, 16])
        hw = sbt(es, "hw", [128, 3])
        mk.dma("sp", n1w[:], I.n1w, writes=[n1w])
        mk.dma("sp", n2w[:], I.n2w, writes=[n2w])
        mk.dma("sp", hw[:], I.hw, writes=[hw])

        def phase_mod():
            with ExitStack() as ps:
                cs = sbt(ps, "cs", [128, 32]); sc_ = sbt(ps, "silu_c", [128, 32])
                adab = sbt(ps, "adab", [128, 96])
                wb = [sbt(ps, "adaw%d" % i, [128, 16, 512]) for i in range(2)]
                pp = [pst(ps, "p0_%d" % i, [128, 512]) for i in range(2)]
                mk.dma("sp", cs[:], I.cs, writes=[cs])
                mk.dma("sp", adab[:], I.ada_b, writes=[adab])
                mk.op("act", lambda e: e.activation(sc_[:], cs[:], AF.Silu), reads=[cs], writes=[sc_])
                wv = I.ada_w.rearrange("(k p) n -> p k n", p=128)
                for nb in range(24):
                    w = wb[nb % 2]
                    mk.dma("sp", w[:], wv[:, :, nb * 512:(nb + 1) * 512], writes=[w])
                    for ct in range(4):
                        j = nb * 4 + ct
                        p = pp[j % 2]
                        for k in range(16):
                            mk.op("pe", lambda e, p=p, w=w, k=k, ct=ct: e.matmul(
                                p[:, 0:2], w[:, k, ct * 128:(ct + 1) * 128], sc_[:, 2 * k:2 * k + 2],
                                start=(k == 0), stop=(k == 15)), reads=[w, sc_], writes=[p])
                        mk.op("dve", lambda e, p=p, j=j: e.tensor_scalar(
                            modsb[:, j, :], p[:, 0:2], adab[:, j:j + 1], None, op0=ALU.add),
                            reads=[p, adab], writes=[modsb])
                mk.op("dve", lambda e: e.scalar_tensor_tensor(a1[:], modsb[:, 16:32, 0], 1.0, n1w[:], op0=ALU.add, op1=ALU.mult),
                      reads=[modsb, n1w], writes=[a1])
                mk.op("dve", lambda e: e.scalar_tensor_tensor(a1c[:], modsb[:, 16:32, 1], 1.0, n1w[:], op0=ALU.add, op1=ALU.mult),
                      reads=[modsb, n1w], writes=[a1c])
                mk.op("dve", lambda e: e.scalar_tensor_tensor(a2[:], modsb[:, 64:80, 0], 1.0, n2w[:], op0=ALU.add, op1=ALU.mult),
                      reads=[modsb, n2w], writes=[a2])
            mk.barrier()

        def norm_mod(ps_bank, xt, sq, rs, tmp2, w, a_t, bcol, out_fn, extra_reads=()):
            mk.op("act", lambda e: e.activation(sq[:, :, :w], xt[:, :, :w], AF.Square), reads=[xt], writes=[sq])
            for k in range(16):
                mk.op("pe", lambda e, k=k: e.matmul(ps_bank[:, :w], K["ones"][:], sq[:, k, :w], start=(k == 0), stop=(k == 15)),
                      reads=[K["ones"], sq], writes=[ps_bank])
            mk.op("act", lambda e: e.activation(rs[:, :w], ps_bank[:, :w], AF.Ln, bias=epsT[:, 0:1], scale=1.0 / D),
                  reads=[ps_bank, epsT], writes=[rs])
            mk.op("act", lambda e: e.activation(rs[:, :w], rs[:, :w], AF.Exp, scale=-0.5), reads=[rs], writes=[rs])
            for k in range(16):
                tm = tmp2[k % 2]
                mk.op("dve", lambda e, k=k, tm=tm: e.tensor_tensor(tm[:, :w], xt[:, k, :w], rs[:, :w], op=ALU.mult),
                      reads=[xt, rs], writes=[tm])
                o, ow = out_fn(k)
                mk.op("act", lambda e, k=k, tm=tm, o=o: e.activation(o, tm[:, :w], AF.Identity, bias=bcol(k), scale=a_t[:, k:k + 1]),
                      reads=[tm, a_t, modsb] + list(extra_reads), writes=[ow])

        TOK_TILES = [(0, 512), (512, 512), (1024, 512), (1536, 512), (2048, 256)]

        def phase_inproj():
            with ExitStack() as ps:
                hT = sbt(ps, "hT", [128, 16, T], BF16)
                pb = [pst(ps, "p1_%d" % i, [128, 512]) for i in range(8)]
                with ExitStack() as p1:
                    xts = [sbt(p1, "xt%d" % i, [128, 16, 512]) for i in range(2)]
                    sq = sbt(p1, "sq", [128, 16, 512])
                    rs = sbt(p1, "rs", [128, 512])
                    tmp2 = [sbt(p1, "tmp%d" % i, [128, 512]) for i in range(2)]
                    xv = I.xT.rearrange("k p t -> p k t")
                    for ti, (t0, w) in enumerate(TOK_TILES):
                        xt = xts[ti % 2]
                        mk.dma("sp", xt[:, :, :w], xv[:, :, t0:t0 + w], writes=[xt])
                        ctxp = (t0 >= L)
                        norm_mod(pb[ti % 2], xt, sq, rs, tmp2, w, a1c if ctxp else a1,
                                 (lambda k, c=(1 if ctxp else 0): modsb[:, k, c:c + 1]),
                                 lambda k, t0=t0, w=w: (hT[:, k, t0:t0 + w], hT))
                    if "s_hT" in dump:
                        mk.dma("sp", S.hT.t.rearrange("k p t -> p k t"), hT[:], reads=[hT], writes=[S.hT])
                mk.barrier()
                if upto < 2:
                    return
                stg = [sbt(ps, "wstg%d" % i, [128, 16, 256]) for i in range(2)]
                wbf = [sbt(ps, "wbf%d" % i, [128, 16, 256], BF16) for i in range(2)]
                ost = [sbt(ps, "ost%d" % i, [128, T]) for i in range(2)]
                vst = [sbt(ps, "vst%d" % i, [128, NCH, 256], BF16) for i in range(2)]
                bst = sbt(ps, "bst", [128, NCH, 64])
                wv = I.w_in.rearrange("(k p) n -> p k n", p=128)
                blocks = []
                for c0 in range(0, 6144, 256):
                    blocks.append((c0, 256, "fm", (S.dn, c0 // 128), T, "copy"))
                for c0 in range(6144, 8192, 256):
                    blocks.append((c0, 256, "fm", (S.z, (c0 - 6144) // 128), L, "silu"))
                blocks.append((8192, 64, "ba", None, T, None))
                for c0 in range(8256, 10304, 256):
                    blocks.append((c0, 256, "fm", (S.nq, (c0 - 8256) // 128), L, "copy"))
                for c0 in range(10304, 12352, 256):
                    blocks.append((c0, 256, "fm", (S.nk, (c0 - 10304) // 128), T, "copy"))
                for c0 in range(12352, 14400, 256):
                    blocks.append((c0, 256, "tm", c0 - 12352, T, None))
                for c0 in range(14400, 16448, 256):
                    blocks.append((c0, 256, "fm", (S.ga, (c0 - 14400) // 128), L, "sig"))
                for c0 in range(16448, 18496, 256):
                    blocks.append((c0, 256, "fm", (S.gb, (c0 - 16448) // 128), L, "sig"))
                st = {"pi": 0, "oi": 0}

                def load(bi):
                    c0, nc_, *_ = blocks[bi]
                    s_ = stg[bi % 2]; wb_ = wbf[bi % 2]
                    mk.dma("sp", s_[:, :, :nc_], wv[:, :, c0:c0 + nc_], writes=[s_])
                    mk.op("act", lambda e: e.copy(wb_[:, 0:8, :nc_], s_[:, 0:8, :nc_]), reads=[s_], writes=[wb_])
                    mk.op("dve", lambda e: e.tensor_copy(wb_[:, 8:16, :nc_], s_[:, 8:16, :nc_]), reads=[s_], writes=[wb_])

                load(0)
                for bi, (c0, nc_, kind, dst, thi, epi) in enumerate(blocks):
                    if bi + 1 < len(blocks):
                        load(bi + 1)
                    wb_ = wbf[bi % 2]
                    if kind == "fm":
                        for ctile in range(nc_ // 128):
                            o = ost[st["oi"] % 2]; st["oi"] += 1
                            for (t0, w) in TOK_TILES:
                                if t0 >= thi:
                                    continue
                                p = pb[st["pi"] % 8]; st["pi"] += 1
                                for k in range(16):
                                    mk.op("pe", lambda e, p=p, k=k, ctile=ctile, t0=t0, w=w: e.matmul(
                                        p[:, :w], wb_[:, k, ctile * 128:(ctile + 1) * 128], hT[:, k, t0:t0 + w],
                                        start=(k == 0), stop=(k == 15)), reads=[wb_, hT], writes=[p])
                                if epi == "copy":
                                    mk.op("dve", lambda e, p=p, o=o, t0=t0, w=w: e.tensor_copy(o[:, t0:t0 + w], p[:, :w]), reads=[p], writes=[o])
                                else:
                                    fn = AF.Silu if epi == "silu" else AF.Sigmoid
                                    mk.op("act", lambda e, p=p, o=o, t0=t0, w=w, fn=fn: e.activation(o[:, t0:t0 + w], p[:, :w], fn), reads=[p], writes=[o])
                            dt_, ti_ = dst
                            mk.dma("sp", dt_.t[ti_ + ctile][:, 0:thi], o[:, 0:thi], reads=[o], writes=[dt_])
                    elif kind == "tm":
                        vs = vst[st["oi"] % 2]; st["oi"] += 1
                        for ti in range(NCH):
                            p = pb[st["pi"] % 8]; st["pi"] += 1
                            for k in range(16):
                                mk.op("pe", lambda e, p=p, k=k, ti=ti: e.matmul(
                                    p[:, :256], hT[:, k, ti * 128:(ti + 1) * 128], wb_[:, k, :256],
                                    start=(k == 0), stop=(k == 15)), reads=[wb_, hT], writes=[p])
                            eng = "dve" if ti % 2 == 0 else "act"
                            if eng == "dve":
                                mk.op("dve", lambda e, p=p, ti=ti: e.tensor_copy(vs[:, ti, :], p[:, :256]), reads=[p], writes=[vs])
                            else:
                                mk.op("act", lambda e, p=p, ti=ti: e.copy(vs[:, ti, :], p[:, :256]), reads=[p], writes=[vs])
                        mk.dma("sp", S.nv.t.rearrange("(n p) c -> p n c", p=128)[:, :, dst:dst + 256], vs[:], reads=[vs], writes=[S.nv])
                    else:
                        for ti in range(NCH):
                            p = pb[st["pi"] % 8]; st["pi"] += 1
                            for k in range(16):
                                mk.op("pe", lambda e, p=p, k=k, ti=ti: e.matmul(
                                    p[:, :64], hT[:, k, ti * 128:(ti + 1) * 128], wb_[:, k, :64],
                                    start=(k == 0), stop=(k == 15)), reads=[wb_, hT], writes=[p])
                            mk.op("dve", lambda e, p=p, ti=ti: e.tensor_copy(bst[:, ti, :], p[:, :64]), reads=[p], writes=[bst])
                        mk.dma("sp", S.ba.t.rearrange("(n p) c -> p n c", p=128), bst[:], reads=[bst], writes=[S.ba])
            mk.barrier()

        g.phase_mod = phase_mod
        g.phase_inproj = phase_inproj
        phases_extra(g, I, S, K, mk, sbt, pst, es, outT, dict(
            modsb=modsb, a2=a2, hw=hw, epsT=epsT, onesb=onesb, identb=identb, norm_mod=norm_mod, dump=dump, upto=upto))

        if run is None:
            run = ("mod", "inproj", "dn", "na", "merge", "moe")
        if "mod" in run:
            phase_mod()
        if "inproj" in run:
            phase_inproj()
        if "dn" in run:
            g.phase_dn(heads)
        if "na" in run:
            g.phase_na(heads)
        if "merge" in run:
            g.phase_merge()
        if "moe" in run:
            g.phase_moe()
        if "modsb" in dump:
            md = nc.dram_tensor("modsb_o", [128, 192], F32, kind="ExternalOutput").ap()
            mk.dma("sp", md, modsb[:].rearrange("p a b -> p (a b)"), reads=[modsb])
        mk.barrier()
        g.ninst = mk.ninst
    return nc, g


def phases_extra(g, I, S, K, mk, sbt, pst, es, outT, X):
    nc = g.nc
    modsb = X["modsb"]; hw = X["hw"]; epsT = X["epsT"]; onesb = X["onesb"]; identb = X["identb"]
    dump = X["dump"]
    TOK5 = [(0, 512), (512, 512), (1024, 512), (1536, 512), (2048, 256)]
    ident = K["ident"]; ones = K["ones"]

    def phase_dn(heads=range(16)):
        with ExitStack() as ps:
            pw = [pst(ps, "dnw%d" % i, [128, 512]) for i in range(2)]
            pqb = [pst(ps, "dnq%d" % i, [128, 512]) for i in range(6)]
            class SlotV(TL):
                def __init__(self, bank, j):
                    self.t = bank.t[:, j * 128:(j + 1) * 128]
                    self.b = bank.b

            banks = [[SlotV(pqb[i], j) for j in range(4)] for i in range(6)]
            sl = {"i": 0, "w": 0}

            def nbank():
                sl["i"] += 1
                return banks[sl["i"] % 6]

            def nwide():
                sl["w"] += 1
                return pw[sl["w"] % 2]

            ba = sbt(ps, "ba", [128, NCH, 64]); dsc = sbt(ps, "dsc", [128, 64]); negexp = sbt(ps, "negexp", [128, 32])
            betaC = sbt(ps, "betaC", [128, NCH, 32]); gC = sbt(ps, "gC", [128, NCH, 32])
            cw = sbt(ps, "cw", [128, 48, 5])
            mk.dma("sp", ba[:], S.ba.t.rearrange("(n p) c -> p n c", p=128), reads=[S.ba], writes=[ba])
            mk.dma("sp", dsc[:], I.dn_sc, writes=[dsc])
            mk.dma("sp", cw[:], I.conv_w, writes=[cw])
            mk.op("act", lambda e: e.activation(negexp[:], dsc[:, 0:32], AF.Exp), reads=[dsc], writes=[negexp])
            mk.op("dve", lambda e: e.tensor_scalar(negexp[:], negexp[:], -1.0, None, op0=ALU.mult), reads=[negexp], writes=[negexp])
            mk.op("act", lambda e: e.activation(betaC[:], ba[:, :, 0:32], AF.Sigmoid), reads=[ba], writes=[betaC])
            for n in range(NCH):
                mk.op("dve", lambda e, n=n: e.tensor_tensor(gC[:, n, :], ba[:, n, 32:64], dsc[:, 32:64], op=ALU.add), reads=[ba, dsc], writes=[gC])
            mk.op("act", lambda e: e.activation(gC[:], gC[:], AF.Exp), reads=[gC], writes=[gC])
            mk.op("act", lambda e: e.activation(gC[:], gC[:], AF.Ln, bias=1.0), reads=[gC], writes=[gC])
            for n in range(NCH):
                mk.op("dve", lambda e, n=n: e.tensor_tensor(gC[:, n, :], gC[:, n, :], negexp[:], op=ALU.mult), reads=[gC, negexp], writes=[gC])

            with ExitStack() as p0:
                growT = [sbt(p0, "growT%d" % i, [16, NCH, 128]) for i in range(2)]
                browT = [sbt(p0, "browT%d" % i, [16, NCH, 128]) for i in range(2)]
                Um0 = [K["uf"], K["ub"]]
                for n in range(NCH):
                    bk = nbank()
                    for d in range(2):
                        mk.op("pe", lambda e, bk=bk, d=d, n=n: e.matmul(bk[d][0:16, :], gC[:, n, d * 16:(d + 1) * 16], Um0[d][:], start=True, stop=True), reads=[gC, Um0[d]], writes=[bk[d]])
                        mk.op("pe", lambda e, bk=bk, d=d, n=n: e.transpose(bk[2 + d][0:16, :], betaC[:, n, d * 16:(d + 1) * 16], ident[:]), reads=[betaC, ident], writes=[bk[2 + d]])
                    for d in range(2):
                        mk.op("dve", lambda e, bk=bk, d=d, n=n: e.tensor_copy(growT[d][:, n, :], bk[d][0:16, :]), reads=[bk[d]], writes=[growT[d]])
                        mk.op("act", lambda e, bk=bk, d=d, n=n: e.copy(browT[d][:, n, :], bk[2 + d][0:16, :]), reads=[bk[2 + d]], writes=[browT[d]])
                for d in range(2):
                    mk.dma("sp", S.grow.t[d * 16:(d + 1) * 16, :], growT[d][:].rearrange("p a b -> p (a b)"), reads=[growT[d]], writes=[S.grow])
                    mk.dma("sp", S.brow.t[d * 16:(d + 1) * 16, :], browT[d][:].rearrange("p a b -> p (a b)"), reads=[browT[d]], writes=[S.brow])
            mk.barrier()
            gbs = [sbt(ps, "gbs%d" % i, [128, 128]) for i in range(8)]
            bbs = [sbt(ps, "bbs%d" % i, [128, 128]) for i in range(8)]
            gbc = {"i": 0}
            raws = [sbt(ps, "raw%d" % i, [128, T]) for i in range(3)]; raw = raws[1]; sqb = raw; rsb = sbt(ps, "rsb", [128, T])
            cq = sbt(ps, "cq", [128, T]); ck = sbt(ps, "ck", [128, T]); cv = sbt(ps, "cv", [128, T])
            ktok = sbt(ps, "ktok", [128, NCH, 128]); vtok = sbt(ps, "vtok", [128, NCH, 128])
            oacc = sbt(ps, "oacc", [128, L]); zt = cv
            oaccB = [Buf("oacc%d" % n) for n in range(16)]
            NS = 8
            uS = sbt(ps, "uS", [128, NS, 128]); wS = sbt(ps, "wS", [128, NS, 128])
            aS = sbt(ps, "aS", [128, NS, 128]); qS = sbt(ps, "qS", [128, NS, 128])
            uV = [TL(uS.t[:, i, :]) for i in range(NS)]; wV = [TL(wS.t[:, i, :]) for i in range(NS)]
            aV = [TL(aS.t[:, i, :]) for i in range(NS)]; qV = [TL(qS.t[:, i, :]) for i in range(NS)]
            W = 4
            wb = {}
            for nm in ["A0", "A1", "B0", "B1", "P0", "P1", "ApI", "dm1", "dm2", "dec", "dec2", "eGb", "BM", "Af"]:
                wb[nm] = [sbt(ps, "w%s%d" % (nm, i), [128, 128]) for i in range(W)]
            wb["vb"] = [sbt(ps, "wvb%d" % i, [128, 128]) for i in range(W)]
            wb["kbg"] = [sbt(ps, "wkbg%d" % i, [128, 128]) for i in range(W)]
            bd16 = sbt(ps, "bd16", [128, 128]); msk = [sbt(ps, "msk%d" % i, [128, 128]) for i in range(3)]
            mk.dma("sp", bd16[:], I.dnmask[0], writes=[bd16])
            for i_ in range(3):
                mk.dma("sp", msk[i_][:], I.dnmask[1 + i_], writes=[msk[i_]])
            kdec = [sbt(ps, "kdec%d" % i, [128, 128]) for i in range(4)]
            vnew = [sbt(ps, "vnew%d" % i, [128, 128]) for i in range(4)]
            Sst = [[sbt(ps, "S%d_%d" % (d, i), [128, 128]) for i in range(2)] for d in range(2)]
            small = {}
            for nm in ["gc", "bc", "Gcol", "eGcol", "kbs", "kds", "eGl", "tmp"]:
                small[nm] = [sbt(ps, "sm%s%d" % (nm, d), [128, NCH]) for d in range(2)]
            Umat = [K["uf"], K["ub"]]; Mm = [K["mf"], K["mb"]]; Mo = [K["mb"], K["mf"]]
            offd = K["offd"]

            R = lambda ap: ap.bitcast(F32R)
            DN_STOP = int(os.environ.get("DN_STOP", "99"))
            PREP_STOP = int(os.environ.get("PREP_STOP", "99"))
            for h in heads:
                if DN_STOP <= 0:
                    break
                for idx, (acc, eng) in enumerate([(cq, "dve"), (ck, "dve"), (cv, "pool")]):
                    raw = raws[idx]
                    mk.dma("sp", raw[:], S.dn.t[idx * 16 + h], reads=[S.dn], writes=[raw])
                    t_ = idx * 16 + h
                    if eng == "dve":
                        mk.op(eng, lambda e, acc=acc, t_=t_: e.tensor_scalar(R(acc[:]), raw[:], cw[:, t_, 2:3], None, op0=ALU.mult),
                              reads=[raw, cw], writes=[acc])
                    else:
                        mk.op(eng, lambda e, acc=acc, t_=t_: e.tensor_tensor(R(acc[:]), raw[:], cw[:, t_, 2:3].to_broadcast([128, T]), op=ALU.mult),
                              reads=[raw, cw], writes=[acc])
                    for (s0, s1) in [(0, L), (L, T)]:
                        for jj in (0, 1, 3, 4):
                            sh = jj - 2
                            d0 = s0 + max(0, -sh); d1 = s1 - max(0, sh)
                            if eng == "dve":
                                mk.op(eng, lambda e, acc=acc, t_=t_, jj=jj, d0=d0, d1=d1, sh=sh: e.scalar_tensor_tensor(
                                    R(acc[:, d0:d1]), raw[:, d0 + sh:d1 + sh], cw[:, t_, jj:jj + 1], acc[:, d0:d1], op0=ALU.mult, op1=ALU.add),
                                    reads=[raw, cw, acc], writes=[acc])
                            else:
                                mk.op(eng, lambda e, t_=t_, jj=jj, d0=d0, d1=d1, sh=sh: e.tensor_tensor(
                                    rsb[:, d0:d1], raw[:, d0 + sh:d1 + sh], cw[:, t_, jj:jj + 1].to_broadcast([128, d1 - d0]), op=ALU.mult),
                                    reads=[raw, cw], writes=[rsb])
                                mk.op(eng, lambda e, acc=acc, d0=d0, d1=d1: e.tensor_tensor(
                                    R(acc[:, d0:d1]), acc[:, d0:d1], rsb[:, d0:d1], op=ALU.add),
                                    reads=[rsb, acc], writes=[acc])
                    mk.op("act", lambda e, acc=acc: e.activation(R(acc[:]), acc[:], AF.Silu), reads=[acc], writes=[acc])
                if DN_STOP <= 1:
                    break
                for acc, scl in [(cq, 128.0 ** -0.5), (ck, 1.0)]:
                    mk.op("act", lambda e, acc=acc: e.activation(sqb[:], acc[:], AF.Square), reads=[acc], writes=[sqb])
                    for (t0, w) in TOK5:
                        p = nwide()
                        mk.op("pe", lambda e, p=p, t0=t0, w=w: e.matmul(p[:, :w], ones[:], sqb[:, t0:t0 + w], start=True, stop=True),
                              reads=[ones, sqb], writes=[p])
                        mk.op("act", lambda e, p=p, t0=t0, w=w: e.activation(rsb[:, t0:t0 + w], p[:, :w], AF.Ln, bias=epsT[:, 0:1], scale=1.0),
                              reads=[p, epsT], writes=[rsb])
                    mk.op("act", lambda e: e.activation(rsb[:], rsb[:], AF.Exp, scale=-0.5), reads=[rsb], writes=[rsb])
                    mk.op("dve", lambda e, acc=acc, scl=scl: e.scalar_tensor_tensor(R(acc[:]), acc[:], scl, rsb[:], op0=ALU.mult, op1=ALU.mult),
                          reads=[acc, rsb], writes=[acc])
                if DN_STOP <= 2:
                    break
                for src, dst in [(ck, ktok), (cv, vtok)]:
                    for n0 in range(0, NCH, 4):
                        nn = min(4, NCH - n0)
                        p = nwide()
                        for q_ in range(nn):
                            n = n0 + q_
                            mk.op("pe", lambda e, p=p, q_=q_, n=n, src=src: e.transpose(p[:, q_ * 128:(q_ + 1) * 128], src[:, n * 128:(n + 1) * 128], ident[:]),
                                  reads=[src, ident], writes=[p])
                        mk.op("act", lambda e, p=p, n0=n0, nn=nn, dst=dst: e.copy(dst[:, n0:n0 + nn, :], p[:, :nn * 128].rearrange("p (a b) -> p a b", b=128)),
                              reads=[p], writes=[dst])
                if DN_STOP <= 3:
                    break
                mk.dma("sp", zt[:, :L], S.z.t[h], reads=[S.z], writes=[zt])
                for d in range(2):
                    ci = d * 16 + h
                    gc = small["gc"][d]; bc = small["bc"][d]
                    mk.op("dve", lambda e, gc=gc, ci=ci: e.tensor_copy(gc[:], gC[:, :, ci]), reads=[gC], writes=[gc])
                    mk.op("dve", lambda e, bc=bc, ci=ci: e.tensor_copy(bc[:], betaC[:, :, ci]), reads=[betaC], writes=[bc])
                    bk = nbank(); p1 = bk[0]; p2 = bk[1]
                    mk.op("pe", lambda e, p1=p1, d=d, gc=gc: e.matmul(p1[:, :NCH], Umat[d][:], gc[:], start=True, stop=True), reads=[Umat[d], gc], writes=[p1])
                    mk.op("pe", lambda e, p2=p2, gc=gc: e.matmul(p2[:, :NCH], ones[:], gc[:], start=True, stop=True), reads=[ones, gc], writes=[p2])
                    Gcol = small["Gcol"][d]; eGcol = small["eGcol"][d]; kbs = small["kbs"][d]; kds = small["kds"][d]; eGl = small["eGl"][d]; tmp = small["tmp"][d]
                    mk.op("dve", lambda e, p1=p1, Gcol=Gcol: e.tensor_copy(Gcol[:], p1[:, :NCH]), reads=[p1], writes=[Gcol])
                    mk.op("act", lambda e, p1=p1, eGcol=eGcol: e.activation(eGcol[:], p1[:, :NCH], AF.Exp), reads=[p1], writes=[eGcol])
                    mk.op("dve", lambda e, kbs=kbs, bc=bc, eGcol=eGcol: e.tensor_tensor(kbs[:], bc[:], eGcol[:], op=ALU.mult), reads=[bc, eGcol], writes=[kbs])
                    mk.op("dve", lambda e, tmp=tmp, p2=p2, Gcol=Gcol: e.tensor_tensor(tmp[:], p2[:, :NCH], Gcol[:], op=ALU.subtract), reads=[p2, Gcol], writes=[tmp])
                    mk.op("act", lambda e, kds=kds, tmp=tmp: e.activation(kds[:], tmp[:], AF.Exp), reads=[tmp], writes=[kds])
                    mk.op("act", lambda e, eGl=eGl, p2=p2: e.activation(eGl[:], p2[:, :NCH], AF.Exp), reads=[p2], writes=[eGl])
                    mk.op("dve", lambda e, d=d: e.tensor_scalar(R(Sst[d][0][:]), ident[:], 0.0, None, op0=ALU.mult), reads=[ident], writes=[Sst[d][0]])

                fo = [16, 17] + list(range(16)); bo = [17, 16] + list(range(15, -1, -1))
                seq = []
                for i_ in range(NCH):
                    seq.append((0, fo[i_])); seq.append((1, bo[i_]))
                spos = {0: 0, 1: 0}
                oinit = set()

                def prep(wave):
                    cds = [(wi, d, n, (wave_base + wi) % NS) for wi, (d, n) in enumerate(wave)]
                    P = {}
                    for wi, d, n, si in cds:
                        gc = small["gc"][d]; bc = small["bc"][d]; Gcol = small["Gcol"][d]
                        kc = ck[:, n * 128:(n + 1) * 128]; qc = cq[:, n * 128:(n + 1) * 128]
                        _b0, _b1, KKp, QKp = nbank()
                        MMS = "kq"
                        GBs = gbs[gbc["i"] % 8]; BBs = bbs[gbc["i"] % 8]; gbc["i"] += 1
                        ci_ = d * 16 + h
                        mk.dma("sp", GBs[:], S.grow.t[ci_:ci_ + 1, n * 128:(n + 1) * 128].partition_broadcast(128), reads=[S.grow], writes=[GBs])
                        mk.dma("sp", BBs[:], S.brow.t[ci_:ci_ + 1, n * 128:(n + 1) * 128].partition_broadcast(128), reads=[S.brow], writes=[BBs])
                        GBv = GBs[:]; BBv = BBs[:]
                        if "k" in MMS:
                          mk.op("pe", lambda e, KKp=KKp, kc=kc: e.matmul(KKp[:], R(kc), R(kc), start=True, stop=True), reads=[ck], writes=[KKp])
                        if "q" in MMS:
                          mk.op("pe", lambda e, QKp=QKp, kc=kc, qc=qc: e.matmul(QKp[:], R(kc), R(qc), start=True, stop=True), reads=[ck, cq], writes=[QKp])
                        if PREP_STOP <= 1:
                            continue
                        dm1 = wb["dm1"][wi]; dm2 = wb["dm2"][wi]; dec = wb["dec"][wi]; dec2 = wb["dec2"][wi]; eGb = wb["eGb"][wi]; BM = wb["BM"][wi]
                        mk.op("dve", lambda e, dm1=dm1, GBv=GBv, Gcol=Gcol, n=n, d=d: e.scalar_tensor_tensor(dm1[:], GBv, Gcol[:, n:n + 1], Mm[d][:], op0=ALU.subtract, op1=ALU.add), reads=[GBs, Gcol, Mm[d]], writes=[dm1])
                        mk.op("dve", lambda e, dm2=dm2, GBv=GBv, Gcol=Gcol, n=n, d=d: e.scalar_tensor_tensor(dm2[:], GBv, Gcol[:, n:n + 1], Mo[d][:], op0=ALU.subtract, op1=ALU.subtract), reads=[GBs, Gcol, Mo[d]], writes=[dm2])
                        mk.op("act", lambda e, dec=dec, dm1=dm1: e.activation(R(dec[:]), dm1[:], AF.Exp), reads=[dm1], writes=[dec])
                        mk.op("act", lambda e, dec2=dec2, dm2=dm2: e.activation(R(dec2[:]), dm2[:], AF.Exp, scale=-1.0), reads=[dm2], writes=[dec2])
                        mk.op("act", lambda e, eGb=eGb, GBv=GBv: e.activation(R(eGb[:]), GBv, AF.Exp), reads=[GBs], writes=[eGb])
                        mk.op("pool", lambda e, BM=BM, BBv=BBv: e.tensor_tensor(BM[:], BBv, offd[:], op=ALU.mult), reads=[BBs, offd], writes=[BM])
                        mk.op("dve", lambda e, si=si, QKp=QKp, dec=dec: e.tensor_tensor(R(aV[si][:]), QKp[:], dec[:], op=ALU.mult), reads=[QKp, dec], writes=[aV[si]])
                        mk.op("pool", lambda e, BM=BM, dec=dec: e.tensor_tensor(BM[:], BM[:], dec[:], op=ALU.mult), reads=[BM, dec], writes=[BM])
                        mk.op("pool", lambda e, dec2=dec2: e.tensor_tensor(R(dec2[:]), dec2[:], offd[:], op=ALU.mult), reads=[dec2, offd], writes=[dec2])
                        A = wb["A0"][wi]; B = wb["B0"][wi]; Pm = wb["P0"][wi]; Af = wb["Af"][wi]
                        mk.op("dve", lambda e, KKp=KKp, BM=BM: e.tensor_tensor(BM[:], KKp[:], BM[:], op=ALU.mult), reads=[KKp, BM], writes=[BM])
                        mk.op("dve", lambda e, Af=Af, KKp=KKp, bc=bc, n=n, dec2=dec2: e.scalar_tensor_tensor(Af[:], KKp[:], bc[:, n:n + 1], dec2[:], op0=ALU.mult, op1=ALU.mult), reads=[KKp, bc, dec2], writes=[Af])
                        mk.op("pool", lambda e, si=si, qc=qc, eGb=eGb: e.tensor_tensor(R(qV[si][:]), qc, eGb[:], op=ALU.mult), reads=[cq, eGb], writes=[qV[si]])
                        mk.op("pool", lambda e, B=B, BM=BM: e.tensor_tensor(R(B[:]), BM[:], bd16[:], op=ALU.mult), reads=[BM, bd16], writes=[B])
                        mk.op("pool", lambda e, A=A, Af=Af: e.tensor_tensor(R(A[:]), Af[:], bd16[:], op=ALU.mult), reads=[Af, bd16], writes=[A])
                        mk.op("pool", lambda e, Pm=Pm, B=B: e.tensor_tensor(R(Pm[:]), ident[:], B[:], op=ALU.subtract), reads=[ident, B], writes=[Pm])
                        P[wi] = [A, B, Pm]
                    if PREP_STOP <= 2:
                        return
                    NLEV = 3
                    for lev in range(1, NLEV + 1):
                        nxt = "1" if lev % 2 == 1 else "0"
                        pend = {}
                        for wi, d, n, si in cds:
                            A, B, Pm = P[wi]
                            bk = nbank()
                            Ap = bk[0]
                            mk.op("pe", lambda e, Ap=Ap, A=A, B=B: e.matmul(Ap[:], R(B[:]), R(A[:]), start=True, stop=True), reads=[A, B], writes=[Ap])
                            Bp = None
                            if lev < NLEV:
                                Bp = bk[1]
                                mk.op("pe", lambda e, Bp=Bp, A=A, B=B: e.matmul(Bp[:], R(A[:]), R(B[:]), start=True, stop=True), reads=[A, B], writes=[Bp])
                            pend[wi] = (Ap, Bp, bk)
                        for wi, d, n, si in cds:
                            Ap, Bp, bk = pend[wi]
                            ApI = wb["ApI"][wi]
                            mk.op("dve", lambda e, ApI=ApI, Ap=Ap: e.tensor_tensor(R(ApI[:]), Ap[:], ident[:], op=ALU.add), reads=[Ap, ident], writes=[ApI])
                            if lev < NLEV:
                                An = wb["A" + nxt][wi]; Bn = wb["B" + nxt][wi]
                                mk.op("act", lambda e, An=An, Ap=Ap: e.copy(R(An[:]), Ap[:]), reads=[Ap], writes=[An])
                                mk.op("act", lambda e, Bn=Bn, Bp=Bp: e.copy(R(Bn[:]), Bp[:]), reads=[Bp], writes=[Bn])
                                P[wi][0] = An; P[wi][1] = Bn
                        pend2 = {}
                        for wi, d, n, si in cds:
                            Pm = P[wi][2]; ApI = wb["ApI"][wi]
                            Pp = pend[wi][2][2]
                            mk.op("pe", lambda e, Pp=Pp, ApI=ApI, Pm=Pm: e.matmul(Pp[:], R(ApI[:]), R(Pm[:]), start=True, stop=True), reads=[ApI, Pm], writes=[Pp])
                            pend2[wi] = Pp
                        for wi, d, n, si in cds:
                            Pn = wb["P" + nxt][wi]
                            Pp = pend2[wi]
                            if wi % 2 == 0:
                                mk.op("dve", lambda e, Pn=Pn, Pp=Pp: e.tensor_copy(R(Pn[:]), Pp[:]), reads=[Pp], writes=[Pn])
                            else:
                                mk.op("act", lambda e, Pn=Pn, Pp=Pp: e.copy(R(Pn[:]), Pp[:]), reads=[Pp], writes=[Pn])
                            P[wi][2] = Pn
                    for wi, d, n, si in cds:
                        Dr = P[wi][2]; Dl = wb["dec2"][wi]
                        bk = nbank()
                        mk.op("pe", lambda e, bk=bk, Dr=Dr: e.transpose(bk[0][:], Dr[:], ident[:]), reads=[Dr, ident], writes=[bk[0]])
                        mk.op("act", lambda e, bk=bk, Dl=Dl: e.copy(R(Dl[:]), bk[0][:]), reads=[bk[0]], writes=[Dl])
                    for bl in range(3):
                        Ms = msk[bl]
                        pend = {}
                        for wi, d, n, si in cds:
                            Dr = P[wi][2]; AM = wb["eGb"][wi]; Af = wb["Af"][wi]
                            mk.op("pool", lambda e, AM=AM, Af=Af, Ms=Ms: e.tensor_tensor(R(AM[:]), Af[:], Ms[:], op=ALU.mult), reads=[Af, Ms], writes=[AM])
                            bk = nbank()
                            mk.op("pe", lambda e, bk=bk, AM=AM, Dr=Dr: e.matmul(bk[0][:], R(AM[:]), R(Dr[:]), start=True, stop=True), reads=[AM, Dr], writes=[bk[0]])
                            pend[wi] = bk
                        for wi, d, n, si in cds:
                            bk = pend[wi]; Ysb = wb["dec"][wi]
                            mk.op("act", lambda e, bk=bk, Ysb=Ysb: e.copy(R(Ysb[:]), bk[0][:]), reads=[bk[0]], writes=[Ysb])
                        for wi, d, n, si in cds:
                            bk = pend[wi]; Ysb = wb["dec"][wi]; Dl = wb["dec2"][wi]
                            mk.op("pe", lambda e, bk=bk, Dl=Dl, Ysb=Ysb: e.matmul(bk[1][:], R(Dl[:]), R(Ysb[:]), start=True, stop=True), reads=[Dl, Ysb], writes=[bk[1]])
                        for wi, d, n, si in cds:
                            bk = pend[wi]; Dr = P[wi][2]
                            Dn = wb["P1"][wi] if Dr is wb["P0"][wi] else wb["P0"][wi]
                            mk.op("dve", lambda e, bk=bk, Dr=Dr, Dn=Dn: e.tensor_tensor(R(Dn[:]), Dr[:], bk[1][:], op=ALU.subtract), reads=[Dr, bk[1]], writes=[Dn])
                            P[wi][2] = Dn
                        if bl < 2:
                            for wi, d, n, si in cds:
                                bk = pend[wi]; Dn = P[wi][2]; Dl = wb["dec2"][wi]
                                mk.op("pe", lambda e, bk=bk, Dn=Dn: e.transpose(bk[2][:], Dn[:], ident[:]), reads=[Dn, ident], writes=[bk[2]])
                                mk.op("act", lambda e, bk=bk, Dl=Dl: e.copy(R(Dl[:]), bk[2][:]), reads=[bk[2]], writes=[Dl])
                    if PREP_STOP <= 3:
                        return
                    for wi, d, n, si in cds:
                        TT = P[wi][2]
                        bc = small["bc"][d]; kbs = small["kbs"][d]
                        vb = wb["vb"][wi]; kbg = wb["kbg"][wi]
                        mk.op("dve", lambda e, vb=vb, n=n, bc=bc: e.tensor_scalar(R(vb[:]), vtok[:, n, :], bc[:, n:n + 1], None, op0=ALU.mult), reads=[vtok, bc], writes=[vb])
                        mk.op("dve", lambda e, kbg=kbg, n=n, kbs=kbs: e.tensor_scalar(R(kbg[:]), ktok[:, n, :], kbs[:, n:n + 1], None, op0=ALU.mult), reads=[ktok, kbs], writes=[kbg])
                        bk = nbank(); up = bk[0]; wp = bk[1]
                        mk.op("pe", lambda e, up=up, TT=TT, vb=vb: e.matmul(up[:], R(TT[:]), R(vb[:]), start=True, stop=True), reads=[TT, vb], writes=[up])
                        mk.op("pe", lambda e, wp=wp, TT=TT, kbg=kbg: e.matmul(wp[:], R(kbg[:]), R(TT[:]), start=True, stop=True), reads=[TT, kbg], writes=[wp])
                        mk.op("act", lambda e, si=si, up=up: e.copy(uV[si][:], up[:]), reads=[up], writes=[uV[si]])
                        mk.op("dve", lambda e, si=si, wp=wp: e.tensor_scalar(R(wV[si][:]), wp[:], -1.0, None, op0=ALU.mult), reads=[wp], writes=[wV[si]])

                def scan(wave):
                    for wi, (d, n) in enumerate(wave):
                        si = (wave_base + wi) % NS
                        kds = small["kds"][d]; eGl = small["eGl"][d]
                        Sc = Sst[d][spos[d] % 2]; Sn = Sst[d][(spos[d] + 1) % 2]; spos[d] += 1
                        kd = kdec[si % 4]; vn = vnew[si % 4]
                        mk.op("act", lambda e, kd=kd, n=n, kds=kds: e.activation(R(kd[:]), ktok[:, n, :], AF.Copy, scale=kds[:, n:n + 1]), reads=[ktok, kds], writes=[kd])
                        bk = nbank(); vp = bk[0]; sp_ = bk[1]; bk2 = nbank()
                        mk.op("pe", lambda e, vp=vp, si=si, Sc=Sc: e.matmul(vp[:], R(wV[si][:]), R(Sc[:]), start=True, stop=True), reads=[wV[si], Sc], writes=[vp])
                        mk.op("dve", lambda e, vn=vn, vp=vp, si=si: e.tensor_tensor(R(vn[:]), vp[:], uV[si][:], op=ALU.add), reads=[vp, uV[si]], writes=[vn])
                        if n < 16:
                            op_ = bk2[0]
                            mk.op("pe", lambda e, op_=op_, Sc=Sc, si=si: e.matmul(op_[:], R(Sc[:]), R(qV[si][:]), start=True, stop=False), reads=[Sc, qV[si]], writes=[op_])
                            mk.op("pe", lambda e, op_=op_, vn=vn, si=si: e.matmul(op_[:], R(vn[:]), R(aV[si][:]), start=False, stop=True), reads=[vn, aV[si]], writes=[op_])
                            if n not in oinit:
                                oinit.add(n)
                                mk.op("act", lambda e, op_=op_, n=n: e.copy(oacc[:, n * 128:(n + 1) * 128], op_[:]), reads=[op_], writes=[oaccB[n]])
                            else:
                                mk.op("dve", lambda e, op_=op_, n=n: e.tensor_tensor(oacc[:, n * 128:(n + 1) * 128], op_[:], oacc[:, n * 128:(n + 1) * 128], op=ALU.add), reads=[op_, oaccB[n]], writes=[oaccB[n]])
                        mk.op("pe", lambda e, sp_=sp_, kd=kd, vn=vn: e.matmul(sp_[:], R(kd[:]), R(vn[:]), start=True, stop=True), reads=[kd, vn], writes=[sp_])
                        mk.op("dve", lambda e, Sn=Sn, Sc=Sc, eGl=eGl, n=n, sp_=sp_: e.scalar_tensor_tensor(R(Sn[:]), Sc[:], eGl[:, n:n + 1], sp_[:], op0=ALU.mult, op1=ALU.add), reads=[Sc, eGl, sp_], writes=[Sn])

                waves = [seq[i_:i_ + W] for i_ in range(0, len(seq), W)]
                if DN_STOP <= 4:
                    break
                for wv_i, wave in enumerate(waves):
                    wave_base = wv_i * W
                    prep(wave)
                    if DN_STOP <= 5:
                        break
                    scan(wave)
                    if DN_STOP <= 6:
                        break
                if DN_STOP <= 6:
                    break
                mk.op("act", lambda e: e.activation(sqb[:, :L], oacc[:], AF.Square), reads=oaccB, writes=[sqb])
                for (t0, w) in TOK5[:4]:
                    p = nwide()
                    mk.op("pe", lambda e, p=p, t0=t0, w=w: e.matmul(p[:, :w], ones[:], sqb[:, t0:t0 + w], start=True, stop=True), reads=[ones, sqb], writes=[p])
                    mk.op("act", lambda e, p=p, t0=t0, w=w: e.activation(rsb[:, t0:t0 + w], p[:, :w], AF.Ln, bias=epsT[:, 0:1], scale=1.0 / 128.0), reads=[p, epsT], writes=[rsb])
                mk.op("act", lambda e: e.activation(rsb[:, :L], rsb[:, :L], AF.Exp, scale=-0.5), reads=[rsb], writes=[rsb])
                mk.op("dve", lambda e: e.tensor_tensor(sqb[:, :L], oacc[:], rsb[:, :L], op=ALU.mult), reads=oaccB + [rsb], writes=[sqb])
                mk.op("dve", lambda e: e.scalar_tensor_tensor(rsb[:, :1024].bitcast(BF16), sqb[:, :L], hw[:, 0:1], zt[:, :L], op0=ALU.mult, op1=ALU.mult), reads=[sqb, hw, zt], writes=[rsb])
                mk.dma("pool", S.odn.t[h], rsb[:, :1024].bitcast(BF16), reads=[rsb], writes=[S.odn])
        mk.barrier()

    g.phase_dn = phase_dn

    def phase_na(heads=range(16)):
        with ExitStack() as ps:
            pb = [pst(ps, "na%d" % i, [128, 512]) for i in range(8)]
            cnt = {"s": 0, "g": 0, "bb": 0}

            def sbank():
                cnt["s"] += 1
                return pb[cnt["s"] % 4]
            ropec = sbt(ps, "ropec", [128, L]); ropes = sbt(ps, "ropes", [128, L])
            mk.dma("sp", ropec[:], I.ropec, writes=[ropec]); mk.dma("sp", ropes[:], I.ropes, writes=[ropes])
            qraws = [sbt(ps, "qraw%d" % i, [128, L]) for i in range(2)]; kraws = [sbt(ps, "kraw%d" % i, [128, T]) for i in range(2)]
            sq = sbt(ps, "nsq", [128, T]); rs = sbt(ps, "nrs", [128, T])
            t1 = sbt(ps, "t1", [128, T]); qbf = sbt(ps, "qbf", [128, L], BF16); kbf = sbt(ps, "kbf", [128, T], BF16)
            vts = [sbt(ps, "vt%d" % i, [128, NCH, 128], BF16) for i in range(2)]
            bias = [sbt(ps, "bias%d" % i, [128, 512]) for i in range(8)]
            et = [sbt(ps, "et%d" % i, [128, 512]) for i in range(2)]
            pT = [sbt(ps, "pT%d" % i, [128, 512], BF16) for i in range(3)]
            t2 = [sbt(ps, "t2_%d" % i, [128, 512]) for i in range(2)]
            ost = sbt(ps, "nost", [128, L], BF16); rden = sbt(ps, "rden", [128, 512])
            rot = K["rot"]
            qbf2 = sbt(ps, "qbf2", [128, L], BF16); kbf2 = sbt(ps, "kbf2", [128, T], BF16)
            qbfs = [qbf, qbf2]; kbfs = [kbf, kbf2]

            def prep_stages(hi_, h):
                slot = hi_ % 2
                qraw = qraws[slot]; kraw = kraws[slot]; vt = vts[slot]

                def loads():
                    mk.dma("sp", qraw[:], S.nq.t[h], reads=[S.nq], writes=[qraw])
                    mk.dma("sp", kraw[:], S.nk.t[h], reads=[S.nk], writes=[kraw])
                    mk.dma("sp", vt[:], S.nv.t.rearrange("(n p) c -> p n c", p=128)[:, :, h * 128:(h + 1) * 128], reads=[S.nv], writes=[vt])

                def normA(raw, Wd):
                    mk.op("act", lambda e, raw=raw, Wd=Wd: e.activation(sq[:, :Wd], raw[:, :Wd], AF.Square), reads=[raw], writes=[sq])
                    for (t0, w) in TOK5:
                        if t0 >= Wd:
                            continue
                        p = sbank()
                        mk.op("pe", lambda e, p=p, t0=t0, w=w: e.matmul(p[:, :w], ones[:], sq[:, t0:t0 + w], start=True, stop=True), reads=[ones, sq], writes=[p])
                        mk.op("act", lambda e, p=p, t0=t0, w=w: e.activation(rs[:, t0:t0 + w], p[:, :w], AF.Ln, bias=epsT[:, 0:1], scale=1.0 / 128.0), reads=[p, epsT], writes=[rs])
                    mk.op("act", lambda e, Wd=Wd: e.activation(rs[:, :Wd], rs[:, :Wd], AF.Exp, scale=-0.5), reads=[rs], writes=[rs])

                def normB(raw, Wd, wc, scl, obf_):
                    mk.op("dve", lambda e, raw=raw, Wd=Wd: e.tensor_tensor(t1[:, :Wd], raw[:, :Wd], rs[:, :Wd], op=ALU.mult), reads=[raw, rs], writes=[t1])
                    mk.op("dve", lambda e, Wd=Wd, wc=wc, scl=scl: e.tensor_scalar(t1[:, :Wd], t1[:, :Wd], hw[:, wc:wc + 1], scl, op0=ALU.mult, op1=ALU.mult), reads=[t1, hw], writes=[t1])
                    for ti in range(4):
                        t0 = ti * 512
                        p = sbank(); tt = t2[ti % 2]
                        mk.op("pe", lambda e, p=p, t0=t0: e.matmul(p[:, :512], rot[:], t1[:, t0:t0 + 512], start=True, stop=True), reads=[rot, t1], writes=[p])
                        mk.op("dve", lambda e, p=p, t0=t0, tt=tt: e.tensor_tensor(tt[:], p[:, :512], ropes[:, t0:t0 + 512], op=ALU.mult), reads=[p, ropes], writes=[tt])
                        mk.op("pool", lambda e, t0=t0: e.tensor_tensor(sq[:, t0:t0 + 512], t1[:, t0:t0 + 512], ropec[:, t0:t0 + 512], op=ALU.mult), reads=[t1, ropec], writes=[sq])
                        mk.op("pool", lambda e, t0=t0, tt=tt, obf_=obf_: e.tensor_tensor(obf_[:, t0:t0 + 512], sq[:, t0:t0 + 512], tt[:], op=ALU.add), reads=[sq, tt], writes=[obf_])
                    if Wd > L:
                        mk.op("act", lambda e, obf_=obf_: e.copy(obf_[:, L:T], t1[:, L:T]), reads=[t1], writes=[obf_])

                def s0():
                    loads(); normA(qraw, L)

                def s1():
                    normB(qraw, L, 1, 128.0 ** -0.5, qbfs[slot])

                def s2():
                    normA(kraw, T)

                def s3():
                    normB(kraw, T, 2, 1.0, kbfs[slot])
                return [s0, s1, s2, s3]

            for st_ in prep_stages(0, heads[0]):
                st_()
            for hi_, h in enumerate(heads):
                slot = hi_ % 2
                qbf = qbfs[slot]; kbf = kbfs[slot]; vt = vts[slot]
                nxt = prep_stages(hi_ + 1, heads[hi_ + 1]) if hi_ + 1 < len(heads) else [None] * 4
                bi = 0
                for m, (lo, nt) in enumerate(NA_BLOCKS):
                    qs = qbf[:, m * 512:(m + 1) * 512]
                    obank = pb[4 + m % 2]; dbank = pb[6 + m % 2]
                    tiles = [("l", lo + 2 * j) for j in range(nt)] + [("c", 0), ("c", 1)]
                    for ti, (kind, kr0) in enumerate(tiles):
                        sb_ = sbank()
                        if kind == "l":
                            keys = kbf[:, kr0 * 64:kr0 * 64 + 128]; V = vt[:, kr0 // 2, :]
                        else:
                            keys = kbf[:, L + kr0 * 128:L + (kr0 + 1) * 128]; V = vt[:, 16 + kr0, :]
                        mk.op("pe", lambda e, sb_=sb_, keys=keys, qs=qs: e.matmul(sb_[:, :512], keys, qs, start=True, stop=True), reads=[kbf, qbf], writes=[sb_])
                        cnt["g"] += 1
                        pt_ = pT[cnt["g"] % 3]
                        if kind == "l":
                            bt = bias[cnt["bb"] % 8]; cnt["bb"] += 1; e_ = et[cnt["g"] % 2]
                            mk.dma("sp", bt[:], I.nab[h, bi], writes=[bt])
                            bi += 1
                            mk.op("dve", lambda e, e_=e_, sb_=sb_, bt=bt: e.tensor_tensor(e_[:], sb_[:, :512], bt[:], op=ALU.add), reads=[sb_, bt], writes=[e_])
                            mk.op("act", lambda e, pt_=pt_, e_=e_: e.activation(pt_[:], e_[:], AF.Exp), reads=[e_], writes=[pt_])
                        else:
                            mk.op("act", lambda e, pt_=pt_, sb_=sb_: e.activation(pt_[:], sb_[:, :512], AF.Exp), reads=[sb_], writes=[pt_])
                        first = (ti == 0); last = (ti == len(tiles) - 1)
                        mk.op("pe", lambda e, obank=obank, V=V, pt_=pt_, first=first, last=last: e.matmul(obank[:, :512], V, pt_[:], start=first, stop=last), reads=[vt, pt_], writes=[obank])
                        mk.op("pe", lambda e, dbank=dbank, pt_=pt_, first=first, last=last: e.matmul(dbank[:, :512], onesb[:], pt_[:], start=first, stop=last), reads=[onesb, pt_], writes=[dbank])
                    mk.op("act", lambda e, dbank=dbank: e.activation(rden[:], dbank[:, :512], AF.Ln), reads=[dbank], writes=[rden])
                    mk.op("act", lambda e: e.activation(rden[:], rden[:], AF.Exp, scale=-1.0), reads=[rden], writes=[rden])
                    mk.op("dve", lambda e, obank=obank, m=m: e.tensor_tensor(ost[:, m * 512:(m + 1) * 512], obank[:, :512], rden[:], op=ALU.mult), reads=[obank, rden], writes=[ost])
                    if nxt[m] is not None:
                        nxt[m]()
                mk.dma("pool", S.ona.t[h], ost[:], reads=[ost], writes=[S.ona])
        mk.barrier()

    g.phase_na = phase_na
    a2 = X["a2"]; norm_mod = X["norm_mod"]

    def make_wloader(ps, nbuf=4):
        stg = [sbt(ps, "stg%d" % i, [128, 4096]) for i in range(2)]
        wbf = [sbt(ps, "wld%d" % i, [128, 4096], BF16) for i in range(nbuf)]
        st = {"i": 0}

        def load(view, a, b):
            i = st["i"]; st["i"] += 1
            s_ = stg[i % 2]; w_ = wbf[i % nbuf]
            mk.dma("sp", s_[:].rearrange("p (a b) -> p a b", b=b), view, writes=[s_])
            mk.op("act", lambda e: e.copy(w_[:, 0:2048], s_[:, 0:2048]), reads=[s_], writes=[w_])
            mk.op("dve", lambda e: e.tensor_copy(w_[:, 2048:4096], s_[:, 2048:4096]), reads=[s_], writes=[w_])
            return w_
        return load

    def pipelined(n, loadfn, computefn):
        cur = loadfn(0)
        for i in range(n):
            nxt = loadfn(i + 1) if i + 1 < n else None
            computefn(i, cur)
            cur = nxt

    def phase_merge():
        with ExitStack() as ps:
            pb = [pst(ps, "mg%d" % i, [128, 512]) for i in range(8)]
            cnt = {"b": 0}

            def bank():
                cnt["b"] += 1
                return pb[cnt["b"] % 8]
            load = make_wloader(ps, nbuf=6)
            odn = sbt(ps, "odn", [128, 16, 1024], BF16); ona = sbt(ps, "ona", [128, 16, 1024], BF16)
            sa = [sbt(ps, "sa%d" % i, [128, 1024]) for i in range(2)]; sb_ = [sbt(ps, "sbb%d" % i, [128, 1024]) for i in range(2)]
            ta = [sbt(ps, "ta%d" % i, [128, 512]) for i in range(2)]; tb = [sbt(ps, "tb%d" % i, [128, 512]) for i in range(2)]
            yst = [sbt(ps, "yst%d" % i, [128, 1024], BF16) for i in range(2)]
            wav = I.w_a.rearrange("(k p) n -> p k n", p=128); wbv = I.w_b.rearrange("(k p) n -> p k n", p=128)
            for half in range(2):
                th0 = half * 1024
                mk.dma("sp", odn[:], S.odn.t.rearrange("h p t -> p h t")[:, :, th0:th0 + 1024], reads=[S.odn], writes=[odn])
                mk.dma("sp", ona[:], S.ona.t.rearrange("h p t -> p h t")[:, :, th0:th0 + 1024], reads=[S.ona], writes=[ona])

                def ld(cb):
                    return (load(wav[:, :, cb * 256:(cb + 1) * 256], 16, 256), load(wbv[:, :, cb * 256:(cb + 1) * 256], 16, 256))

                def comp(cb, ws):
                    wa_, wb_ = ws
                    for ct in range(2):
                        dt = cb * 2 + ct
                        sA = sa[dt % 2]; sB = sb_[dt % 2]; ys = yst[dt % 2]
                        mk.dma("sp", sA[:], S.ga.t[dt][:, th0:th0 + 1024], reads=[S.ga], writes=[sA])
                        mk.dma("sp", sB[:], S.gb.t[dt][:, th0:th0 + 1024], reads=[S.gb], writes=[sB])
                        for tq in range(2):
                            t0 = tq * 512
                            pA = bank(); pB = bank()
                            for k in range(16):
                                mk.op("pe", lambda e, pA=pA, k=k, ct=ct, t0=t0: e.matmul(pA[:, :512], wa_[:, k * 256 + ct * 128:k * 256 + (ct + 1) * 128], odn[:, k, t0:t0 + 512], start=(k == 0), stop=(k == 15)), reads=[wa_, odn], writes=[pA])
                            for k in range(16):
                                mk.op("pe", lambda e, pB=pB, k=k, ct=ct, t0=t0: e.matmul(pB[:, :512], wb_[:, k * 256 + ct * 128:k * 256 + (ct + 1) * 128], ona[:, k, t0:t0 + 512], start=(k == 0), stop=(k == 15)), reads=[wb_, ona], writes=[pB])
                            a_ = ta[tq]; b_ = tb[tq]
                            mk.op("dve", lambda e, a_=a_, pA=pA, sA=sA, t0=t0: e.tensor_tensor(a_[:], pA[:, :512], sA[:, t0:t0 + 512], op=ALU.mult), reads=[pA, sA], writes=[a_])
                            mk.op("dve", lambda e, b_=b_, pB=pB, sB=sB, t0=t0: e.tensor_tensor(b_[:], pB[:, :512], sB[:, t0:t0 + 512], op=ALU.mult), reads=[pB, sB], writes=[b_])
                            mk.op("pool", lambda e, a_=a_, b_=b_, ys=ys, t0=t0: e.tensor_tensor(ys[:, t0:t0 + 512], a_[:], b_[:], op=ALU.add), reads=[a_, b_], writes=[ys])
                        mk.dma("sp", S.y.t[dt][:, th0:th0 + 1024], ys[:], reads=[ys], writes=[S.y])
                pipelined(8, ld, comp)
            mk.barrier()
            yT = odn
            xt = [sbt(ps, "mxt%d" % i, [128, 1024]) for i in range(2)]
            xst = [sbt(ps, "mxs%d" % i, [128, 1024]) for i in range(2)]
            wov = I.w_out.rearrange("(k p) n -> p k n", p=128)
            for half in range(2):
                th0 = half * 1024
                mk.dma("sp", yT[:], S.y.t.rearrange("h p t -> p h t")[:, :, th0:th0 + 1024], reads=[S.y], writes=[yT])

                def ld2(cb):
                    return load(wov[:, :, cb * 256:(cb + 1) * 256], 16, 256)

                def comp2(cb, wo_):
                    for ct in range(2):
                        dt = cb * 2 + ct
                        x_ = xt[dt % 2]; xs = xst[dt % 2]
                        mk.dma("sp", x_[:], I.xT[dt][:, th0:th0 + 1024], writes=[x_])
                        for tq in range(2):
                            t0 = tq * 512
                            p = bank()
                            for k in range(16):
                                mk.op("pe", lambda e, p=p, k=k, ct=ct, t0=t0: e.matmul(p[:, :512], wo_[:, k * 256 + ct * 128:k * 256 + (ct + 1) * 128], yT[:, k, t0:t0 + 512], start=(k == 0), stop=(k == 15)), reads=[wo_, yT], writes=[p])
                            mk.op("dve", lambda e, p=p, xs=xs, x_=x_, dt=dt, t0=t0: e.scalar_tensor_tensor(xs[:, t0:t0 + 512], p[:, :512], modsb[:, 32 + dt, 0:1], x_[:, t0:t0 + 512], op0=ALU.mult, op1=ALU.add), reads=[p, modsb, x_], writes=[xs])
                        mk.dma("sp", S.x1.t[dt][:, th0:th0 + 1024], xs[:], reads=[xs], writes=[S.x1])
                pipelined(8, ld2, comp2)
        mk.barrier()

    g.phase_merge = phase_merge

    def phase_moe():
        with ExitStack() as ps:
            pb = [pst(ps, "me%d" % i, [128, 512]) for i in range(6)]
            pbt = [pst(ps, "met%d" % i, [128, 1024], BF16) for i in range(2)]
            cnt = {"b": 0, "t": 0}

            def bank():
                cnt["b"] += 1
                return pb[cnt["b"] % 6]
            h2tok = sbt(ps, "h2tok", [128, 16, D], BF16)
            logits = sbt(ps, "logits", [128, 16, 16])
            wr = sbt(ps, "wr", [128, 16, 16])
            iota = sbt(ps, "iota", [128, 258])
            mk.dma("sp", wr[:], I.w_r.rearrange("(k p) e -> p k e", p=128), writes=[wr])
            mk.dma("sp", iota[:], I.iota, writes=[iota])
            affT = sbt(ps, "affT", [16, L]); work = sbt(ps, "work", [16, L]); rkmT = sbt(ps, "rkmT", [16, L]); gwT = sbt(ps, "gwT", [16, L])
            rkm = sbt(ps, "rkm", [128, 16, 16])
            with ExitStack() as p1:
                xts = [sbt(p1, "mx%d" % i, [128, 16, 512]) for i in range(1)]
                sq = sbt(p1, "msq", [128, 16, 512]); rs = sbt(p1, "mrs", [128, 512])
                tmp2 = [sbt(p1, "mtmp%d" % i, [128, 512]) for i in range(2)]
                h2f = sq; h2b = sbt(p1, "h2b", [128, 16, 512], BF16)
                xv = S.x1.t.rearrange("k p t -> p k t")
                for tt in range(4):
                    t0 = tt * 512
                    xt = xts[0]
                    mk.dma("sp", xt[:], xv[:, :, t0:t0 + 512], reads=[S.x1], writes=[xt])
                    norm_mod(bank(), xt, sq, rs, tmp2, 512, a2, (lambda k: modsb[:, 48 + k, 0:1]), lambda k: (h2f[:, k, :], h2f))
                    for tc in range(4):
                        p = bank()
                        for k in range(16):
                            mk.op("pe", lambda e, p=p, k=k, tc=tc: e.matmul(p[:, :16], h2f[:, k, tc * 128:(tc + 1) * 128], wr[:, k, :], start=(k == 0), stop=(k == 15)), reads=[h2f, wr], writes=[p])
                        mk.op("dve", lambda e, p=p, tt=tt, tc=tc: e.tensor_copy(logits[:, tt * 4 + tc, :], p[:, :16]), reads=[p], writes=[logits])
                    mk.op("act", lambda e: e.copy(h2b[:], h2f[:]), reads=[h2f], writes=[h2b])
                    for tc in range(4):
                        for kg in range(2):
                            cnt["t"] += 1
                            pt = pbt[cnt["t"] % 2]
                            for kk in range(8):
                                k = kg * 8 + kk
                                mk.op("pe", lambda e, pt=pt, kk=kk, k=k, tc=tc: e.transpose(pt[:, kk * 128:(kk + 1) * 128], h2b[:, k, tc * 128:(tc + 1) * 128], identb[:]), reads=[h2b, identb], writes=[pt])
                            eng = "dve" if (tc + kg) % 2 == 0 else "act"
                            dst = h2tok[:, tt * 4 + tc, kg * 1024:(kg + 1) * 1024]
                            if eng == "dve":
                                mk.op("dve", lambda e, pt=pt, dst=dst: e.tensor_copy(dst, pt[:]), reads=[pt], writes=[h2tok])
                            else:
                                mk.op("act", lambda e, pt=pt, dst=dst: e.copy(dst, pt[:]), reads=[pt], writes=[h2tok])
                mx = sbt(p1, "mx_", [128, 16]); sm = sbt(p1, "sm_", [128, 16])
                for tc in range(16):
                    mk.op("dve", lambda e, tc=tc: e.reduce_max(mx[:, tc:tc + 1], logits[:, tc, :], axis=AX.X), reads=[logits], writes=[mx])
                mk.op("dve", lambda e: e.tensor_scalar(mx[:], mx[:], -1.0, None, op0=ALU.mult), reads=[mx], writes=[mx])
                for tc in range(16):
                    mk.op("act", lambda e, tc=tc: e.activation(logits[:, tc, :], logits[:, tc, :], AF.Exp, bias=mx[:, tc:tc + 1], scale=1.0, accum_out=sm[:, tc:tc + 1]), reads=[logits, mx], writes=[logits, sm])
                mk.op("dve", lambda e: e.reciprocal(sm[:], sm[:]), reads=[sm], writes=[sm])
                for tc in range(16):
                    mk.op("dve", lambda e, tc=tc: e.tensor_scalar(logits[:, tc, :], logits[:, tc, :], sm[:, tc:tc + 1], None, op0=ALU.mult), reads=[logits, sm], writes=[logits])
                for g4 in range(4):
                    p = bank()
                    for q_ in range(4):
                        tc = g4 * 4 + q_
                        mk.op("pe", lambda e, p=p, q_=q_, tc=tc: e.transpose(p[0:16, q_ * 128:(q_ + 1) * 128], logits[:, tc, :], ident[:]), reads=[logits, ident], writes=[p])
                    mk.op("dve", lambda e, p=p, g4=g4: e.tensor_copy(affT[:, g4 * 512:(g4 + 1) * 512], p[0:16, :512]), reads=[p], writes=[affT])
                mk.op("dve", lambda e: e.tensor_copy(work[:], affT[:]), reads=[affT], writes=[work])
                m8 = sbt(p1, "m8", [16, 8])
                for r_ in range(32):
                    mk.op("dve", lambda e: e.max(m8[:], work[:]), reads=[work], writes=[m8])
                    if r_ < 31:
                        mk.op("dve", lambda e: e.match_replace(work[:], m8[:], work[:], -1.0), reads=[work, m8], writes=[work])
                mk.op("dve", lambda e: e.tensor_scalar(work[:], affT[:], m8[:, 7:8], None, op0=ALU.is_ge), reads=[affT, m8], writes=[work])
                mk.op("dve", lambda e: e.tensor_tensor(gwT[:], affT[:], work[:], op=ALU.mult), reads=[affT, work], writes=[gwT])
                onesr = sbt(p1, "onesr", [16, L])
                mk.op("pool", lambda e: e.memset(onesr[:], 1.0), writes=[onesr])
                mk.op("dve", lambda e: e.tensor_tensor_scan(rkmT[:], onesr[:], work[:], 0.0, op0=ALU.mult, op1=ALU.add), reads=[onesr, work], writes=[rkmT])
                mk.op("dve", lambda e: e.tensor_tensor(rkmT[:], rkmT[:], work[:], op=ALU.mult), reads=[rkmT, work], writes=[rkmT])
                mk.op("dve", lambda e: e.tensor_scalar(rkmT[:], rkmT[:], -1.0, None, op0=ALU.add), reads=[rkmT], writes=[rkmT])
                for g4 in range(4):
                    p = bank()
                    for q_ in range(4):
                        tc = g4 * 4 + q_
                        mk.op("pe", lambda e, p=p, q_=q_, tc=tc: e.transpose(p[:, q_ * 16:(q_ + 1) * 16], rkmT[:, tc * 128:(tc + 1) * 128], ident[0:16, 0:16]), reads=[rkmT, ident], writes=[p])
                    mk.op("dve", lambda e, p=p, g4=g4: e.tensor_copy(rkm[:, g4 * 4:(g4 + 1) * 4, :], p[:, :64].rearrange("p (a b) -> p a b", b=16)), reads=[p], writes=[rkm])
            mk.barrier()
            with ExitStack() as p2:
                load = make_wloader(p2, nbuf=4)
                sel = [sbt(p2, "sel%d" % i, [128, 16, 256], BF16) for i in range(2)]
                xg = sbt(p2, "xg", [128, 16, 256], BF16)
                hid = sbt(p2, "hid", [128, 8, 256], BF16)
                st_ = [sbt(p2, "sil%d" % i, [128, 256]) for i in range(2)]
                yest = [sbt(p2, "yest%d" % i, [128, D], BF16) for i in range(2)]
                for ex in range(16):
                    sl_ = sel[ex % 2]
                    for tc in range(16):
                        eng = "dve"
                        mk.op(eng, lambda e, sl_=sl_, tc=tc, ex=ex: e.tensor_scalar(sl_[:, tc, :], iota[:, 0:256], rkm[:, tc, ex:ex + 1], None, op0=ALU.is_equal), reads=[iota, rkm], writes=[sl_])
                    for Dc in range(16):
                        p = bank()
                        for tc in range(16):
                            mk.op("pe", lambda e, p=p, tc=tc, Dc=Dc, sl_=sl_: e.matmul(p[:, :256], h2tok[:, tc, Dc * 128:(Dc + 1) * 128], sl_[:, tc, :], start=(tc == 0), stop=(tc == 15)), reads=[h2tok, sl_], writes=[p])
                        if Dc % 2 == 0:
                            mk.op("dve", lambda e, p=p, Dc=Dc: e.tensor_copy(xg[:, Dc, :], p[:, :256]), reads=[p], writes=[xg])
                        else:
                            mk.op("act", lambda e, p=p, Dc=Dc: e.copy(xg[:, Dc, :], p[:, :256]), reads=[p], writes=[xg])
                    w1v = I.ew1[ex].rearrange("(k p) f -> p k f", p=128); w3v = I.ew3[ex].rearrange("(k p) f -> p k f", p=128)

                    def ld(fb, w1v=w1v, w3v=w3v):
                        return (load(w1v[:, :, fb * 256:(fb + 1) * 256], 16, 256), load(w3v[:, :, fb * 256:(fb + 1) * 256], 16, 256))

                    def comp(fb, ws):
                        w1_, w3_ = ws
                        for ft in range(2):
                            p1_ = bank(); p3_ = bank()
                            for k in range(16):
                                mk.op("pe", lambda e, p1_=p1_, k=k, ft=ft: e.matmul(p1_[:, :256], w1_[:, k * 256 + ft * 128:k * 256 + (ft + 1) * 128], xg[:, k, :], start=(k == 0), stop=(k == 15)), reads=[w1_, xg], writes=[p1_])
                            for k in range(16):
                                mk.op("pe", lambda e, p3_=p3_, k=k, ft=ft: e.matmul(p3_[:, :256], w3_[:, k * 256 + ft * 128:k * 256 + (ft + 1) * 128], xg[:, k, :], start=(k == 0), stop=(k == 15)), reads=[w3_, xg], writes=[p3_])
                            s_ = st_[ft]
                            mk.op("act", lambda e, s_=s_, p1_=p1_: e.activation(s_[:], p1_[:, :256], AF.Silu), reads=[p1_], writes=[s_])
                            mk.op("dve", lambda e, s_=s_, p3_=p3_, fb=fb, ft=ft: e.tensor_tensor(hid[:, fb * 2 + ft, :], s_[:], p3_[:, :256], op=ALU.mult), reads=[s_, p3_], writes=[hid])
                    pipelined(4, ld, comp)
                    w2v = I.ew2[ex].rearrange("(k p) n -> p k n", p=128)

                    def ld2(nb, w2v=w2v):
                        return load(w2v[:, :, nb * 512:(nb + 1) * 512], 8, 512)

                    def comp2(nb, w2_):
                        for s2 in range(2):
                            p = bank(); ys = yest[s2]
                            for f in range(8):
                                mk.op("pe", lambda e, p=p, f=f, s2=s2: e.matmul(p[:, :512], hid[:, f, s2 * 128:(s2 + 1) * 128], w2_[:, f * 512:(f + 1) * 512], start=(f == 0), stop=(f == 7)), reads=[hid, w2_], writes=[p])
                            if s2 == 0:
                                mk.op("dve", lambda e, p=p, ys=ys, nb=nb: e.tensor_copy(ys[:, nb * 512:(nb + 1) * 512], p[:, :512]), reads=[p], writes=[ys])
                            else:
                                mk.op("act", lambda e, p=p, ys=ys, nb=nb: e.copy(ys[:, nb * 512:(nb + 1) * 512], p[:, :512]), reads=[p], writes=[ys])
                    pipelined(4, ld2, comp2)
                    for s2 in range(2):
                        mk.dma("sp", S.ye.t[ex * 2 + s2], yest[s2][:], reads=[yest[s2]], writes=[S.ye])
            mk.barrier()
            with ExitStack() as p3:
                selT = sbt(p3, "selT", [16, 16, 128])
                for ex in range(16):
                    mk.op("dve", lambda e, ex=ex: e.tensor_copy(selT[:, ex, :], ident[0:16, ex:ex + 1].to_broadcast([16, 128])), reads=[ident], writes=[selT])
                SGT = sbt(p3, "SGT", [128, 32, 512], BF16)
                gwB = [sbt(p3, "gwB%d" % i, [128, 512]) for i in range(2)]
                yeD = [sbt(p3, "yeD%d" % i, [128, 32, 128], BF16) for i in range(2)]
                x1t = [sbt(p3, "x1t%d" % i, [128, 512]) for i in range(2)]
                ot = [sbt(p3, "ot%d" % i, [128, 512]) for i in range(2)]
                yev = S.ye.t.rearrange("q p d -> p q d")
                for tt in range(4):
                    t0 = tt * 512
                    for ex in range(16):
                        pR = bank(); pG = bank(); gb_ = gwB[ex % 2]
                        mk.op("pe", lambda e, pR=pR, ex=ex, t0=t0: e.matmul(pR[:, :512], selT[:, ex, :], rkmT[:, t0:t0 + 512], start=True, stop=True), reads=[selT, rkmT], writes=[pR])
                        mk.op("pe", lambda e, pG=pG, ex=ex, t0=t0: e.matmul(pG[:, :512], selT[:, ex, :], gwT[:, t0:t0 + 512], start=True, stop=True), reads=[selT, gwT], writes=[pG])
                        mk.op("act", lambda e, gb_=gb_, pG=pG: e.copy(gb_[:], pG[:, :512]), reads=[pG], writes=[gb_])
                        for s2 in range(2):
                            mk.op("dve", lambda e, pR=pR, gb_=gb_, ex=ex, s2=s2: e.scalar_tensor_tensor(SGT[:, ex * 2 + s2, :], pR[:, :512], iota[:, 256 + s2:257 + s2], gb_[:], op0=ALU.is_equal, op1=ALU.mult), reads=[pR, iota, gb_], writes=[SGT])
                    for Dc in range(16):
                        yd = yeD[Dc % 2]; x_ = x1t[Dc % 2]; o_ = ot[Dc % 2]
                        mk.dma("sp", yd[:], yev[:, :, Dc * 128:(Dc + 1) * 128], reads=[S.ye], writes=[yd])
                        mk.dma("sp", x_[:], S.x1.t[Dc][:, t0:t0 + 512], reads=[S.x1], writes=[x_])
                        p = bank()
                        for q_ in range(32):
                            mk.op("pe", lambda e, p=p, q_=q_, yd=yd: e.matmul(p[:, :512], yd[:, q_, :], SGT[:, q_, :], start=(q_ == 0), stop=(q_ == 31)), reads=[yd, SGT], writes=[p])
                        mk.op("dve", lambda e, p=p, o_=o_, x_=x_, Dc=Dc: e.scalar_tensor_tensor(o_[:], p[:, :512], modsb[:, 80 + Dc, 0:1], x_[:], op0=ALU.mult, op1=ALU.add), reads=[p, modsb, x_], writes=[o_])
                        mk.dma("sp", outT.t[Dc][:, t0:t0 + 512], o_[:], reads=[o_], writes=[outT])
        mk.barrier()

    g.phase_moe = phase_moe


def prep_shared(inp):
    f = np.float32
    sh = {}
    sh["ada_w"] = np.ascontiguousarray(inp["ada_w"][0], f)
    sh["ada_b"] = np.ascontiguousarray(inp["ada_b"][0].reshape(96, 128).T, f)
    sh["n1w"] = np.ascontiguousarray(inp["norm1_w"][0].reshape(16, 128).T, f)
    sh["n2w"] = np.ascontiguousarray(inp["norm2_w"][0].reshape(16, 128).T, f)
    sh["w_in"] = np.ascontiguousarray(inp["w_in"][0], f)
    sh["conv_w"] = np.ascontiguousarray(inp["conv_w"][0].T.reshape(48, 128, 5).transpose(1, 0, 2), f)
    sc = np.concatenate([inp["dn_a_log"][0].reshape(-1), inp["dn_dt_bias"][0].reshape(-1)]).astype(f)
    sh["dn_sc"] = np.ascontiguousarray(np.broadcast_to(sc[None, :], (128, 64)), f)
    sh["hw"] = np.ascontiguousarray(np.stack([inp["dn_norm_w"][0], inp["na_q_norm_w"][0], inp["na_k_norm_w"][0]], axis=1), f)
    c = _consts()
    sh["consts"] = np.ascontiguousarray(np.stack([c[n] for n in CONST_NAMES]), f)
    sh["ropec"], sh["ropes"] = _rope_tables()
    sh["nab"] = _na_bias_table(np.asarray(inp["na_rpb"][0], f))
    io = np.zeros((128, 258), f)
    io[:, :256] = np.arange(256, dtype=f)[None, :]
    io[:, 256] = np.arange(128, dtype=f)
    io[:, 257] = np.arange(128, dtype=f) + 128
    sh["iota"] = io
    ii = np.arange(128)
    mks = [(ii[:, None] // 16 == ii[None, :] // 16)]
    for b_ in (16, 32, 64):
        same = (ii[:, None] // (2 * b_) == ii[None, :] // (2 * b_))
        hi = (ii % (2 * b_)) >= b_
        mks.append(same & (hi[:, None] != hi[None, :]))
    sh["dnmask"] = np.ascontiguousarray(np.stack(mks).astype(f))
    sh["w_a"] = np.ascontiguousarray(inp["w_branch_a"][0], f)
    sh["w_b"] = np.ascontiguousarray(inp["w_branch_b"][0], f)
    sh["w_out"] = np.ascontiguousarray(inp["w_out"][0], f)
    sh["w_r"] = np.ascontiguousarray(inp["w_router"][0], f)
    sh["ew1"] = np.ascontiguousarray(inp["expert_w1"][0], f)
    sh["ew3"] = np.ascontiguousarray(inp["expert_w3"][0], f)
    sh["ew2"] = np.ascontiguousarray(inp["expert_w2"][0], f)
    return sh


def prep_core(inp, b):
    f = np.float32
    m = {}
    xc = np.concatenate([inp["x"][b], inp["ctx"][b]], axis=0)
    m["xT"] = np.ascontiguousarray(xc.T.reshape(16, 128, T), f)
    cs = np.stack([inp["c"][b].reshape(16, 128), inp["c_ctx"].reshape(16, 128)], axis=-1)
    m["cs"] = np.ascontiguousarray(cs.transpose(1, 0, 2).reshape(128, 32), f)
    return m


_CACHE = {}


def kernel(**inputs):
    inp = {k: np.asarray(v) for k, v in inputs.items()}
    if "nc" not in _CACHE:
        _CACHE["nc"] = build_nc()[0]
    nc = _CACHE["nc"]
    sh = prep_shared(inp)
    in_maps = []
    for b in range(8):
        m = dict(sh)
        m.update(prep_core(inp, b))
        in_maps.append(m)
    res = run_bass_kernel_spmd(nc, in_maps, core_ids=list(range(8)))
    out = np.stack([np.asarray(r["outT"], np.float32).reshape(D, L).T for r in res.results], axis=0)
    return np.ascontiguousarray(out, np.float32)
```
